# Optimizing a Trainium2 kernel written in Bass

```python
import math
import jax, jax.numpy as jnp
from jax import lax
import numpy as np

D_MODEL = 2048
BATCH = 4
SEQ = 8192
DEPTH = 1

MEM_LEN = 256
D_MIX = D_MODEL
HEAD_DIM = 64
NSA_WIDTH = D_MIX // 2
CONV_WIDTH = D_MIX - NSA_WIDTH
NSA_HEADS = NSA_WIDTH // HEAD_DIM
NSA_KV_HEADS = 4
NSA_GROUP = NSA_HEADS // NSA_KV_HEADS
CMP_BLOCK = 32
CMP_STRIDE = 16
CMP_HIDDEN = 256
SEL_BLOCK = 64
SEL_TOPK = 16
WIN = 512
Q_BLOCK = 128
CONV_K = 31
REL_BUCKETS = 32
REL_MAX_DIST = 128
X_HEADS = 4
X_HEAD_DIM = D_MODEL // X_HEADS
MOE_GROUPS = 4
MOE_EXPERTS_PER_GROUP = 8
MOE_EXPERTS = MOE_GROUPS * MOE_EXPERTS_PER_GROUP
MOE_TOPK = 2
MOE_HIDDEN = 512
MOE_BLOCK = 128
LN_EPS = 1e-5
DEEPNORM_ALPHA = (2 * DEPTH) ** 0.25
DEEPNORM_BETA = (8 * DEPTH) ** -0.25
IN_SIZES = (NSA_HEADS * HEAD_DIM,) + (NSA_KV_HEADS * HEAD_DIM,) * 6 + (NSA_HEADS * 3, 2 * CONV_WIDTH)
IN_COLS = sum(IN_SIZES)

kernel_name = "hybrid_nsa_conformer_hmoe_deepnorm"


def layer_norm(x, g, b):
    xf = x.astype(jnp.float32)
    mu = jnp.mean(xf, axis=-1, keepdims=True)
    var = jnp.mean(jnp.square(xf - mu), axis=-1, keepdims=True)
    return ((xf - mu) * lax.rsqrt(var + LN_EPS) * g + b).astype(x.dtype)


def masked_softmax(s, mask):
    s = jnp.where(mask, s, -jnp.inf)
    m = jnp.max(s, axis=-1, keepdims=True)
    m = jnp.where(jnp.isfinite(m), m, 0.0)
    p = jnp.exp(s - m)
    return p / jnp.maximum(jnp.sum(p, axis=-1, keepdims=True), 1e-30)


def t5_bucket(dist):
    n = jnp.maximum(dist, 0)
    max_exact = REL_BUCKETS // 2
    nf = jnp.maximum(n, 1).astype(jnp.float32)
    large = max_exact + (jnp.log(nf / max_exact) / math.log(REL_MAX_DIST / max_exact)
                         * (REL_BUCKETS - max_exact)).astype(jnp.int32)
    large = jnp.minimum(large, REL_BUCKETS - 1)
    return jnp.where(n < max_exact, n, large)


def compress(k, pe, w1, w2):
    B, Hk, S, dh = k.shape
    ch = k.reshape(B, Hk, S // CMP_STRIDE, CMP_STRIDE, dh)
    blk = jnp.concatenate([ch[:, :, :-1], ch[:, :, 1:]], axis=3) + pe
    n_cmp = blk.shape[2]
    h = jax.nn.gelu(blk.reshape(B, Hk, n_cmp, CMP_BLOCK * dh) @ w1)
    return h @ w2


def cmp_to_sel_overlap(n_cmp, n_sel):
    cs = np.arange(n_cmp) * CMP_STRIDE
    ce = cs + CMP_BLOCK
    ss = np.arange(n_sel) * SEL_BLOCK
    se = ss + SEL_BLOCK
    ov = np.clip(np.minimum(ce[:, None], se[None, :]) - np.maximum(cs[:, None], ss[None, :]), 0, None)
    return jnp.asarray(ov / CMP_BLOCK, dtype=jnp.float32)


def nsa_attention(q, kc, vc, ks, vs, kw, vw, gates, rel_table):
    B, Hk, G, S, dh = q.shape
    n_q = S // Q_BLOCK
    n_sel = S // SEL_BLOCK
    n_top = min(SEL_TOPK, n_sel)
    n_cmp = kc.shape[2]
    cmp_end = jnp.arange(n_cmp, dtype=jnp.int32) * CMP_STRIDE + CMP_BLOCK - 1
    overlap = cmp_to_sel_overlap(n_cmp, n_sel)
    ks_blocks = ks.reshape(B, Hk, n_sel, SEL_BLOCK, dh)
    vs_blocks = vs.reshape(B, Hk, n_sel, SEL_BLOCK, dh)
    kw_pad = jnp.pad(kw, ((0, 0), (0, 0), (WIN, 0), (0, 0)))
    vw_pad = jnp.pad(vw, ((0, 0), (0, 0), (WIN, 0), (0, 0)))
    tab = rel_table.reshape(REL_BUCKETS, Hk, G).transpose(1, 2, 0)
    h_ix = jnp.arange(Hk)[None, :, None, None, None]
    g_ix = jnp.arange(G)[None, None, :, None, None]
    b_ix = jnp.arange(B)[:, None, None, None]
    hk_ix = jnp.arange(Hk)[None, :, None, None]
    sel_j = jnp.arange(n_sel, dtype=jnp.int32)
    scale = HEAD_DIM ** -0.5

    def block_fn(qi):
        q0 = qi * Q_BLOCK
        qb = lax.dynamic_slice_in_dim(q, q0, Q_BLOCK, axis=3) * scale
        gb = lax.dynamic_slice_in_dim(gates, q0, Q_BLOCK, axis=3)
        t = q0 + jnp.arange(Q_BLOCK, dtype=jnp.int32)

        dist_c = t[:, None] - cmp_end[None, :]
        s_c = jnp.einsum('bhgqd,bhnd->bhgqn', qb, kc, preferred_element_type=jnp.float32)
        s_c = s_c + tab[:, :, t5_bucket(dist_c)]
        p_c = masked_softmax(s_c, dist_c >= 0)
        o_c = jnp.einsum('bhgqn,bhnd->bhgqd', p_c.astype(vc.dtype), vc)

        imp = jnp.einsum('bhgqn,nj->bhqj', p_c, overlap)
        cur = t // SEL_BLOCK
        forced = (sel_j[None, :] == 0) | (sel_j[None, :] == cur[:, None]) | (sel_j[None, :] == cur[:, None] - 1)
        valid = sel_j[None, :] * SEL_BLOCK <= t[:, None]
        score = jnp.where(forced, jnp.inf, jnp.where(valid, imp, -jnp.inf))
        _, idx = lax.top_k(score, n_top)

        k_sel = ks_blocks[b_ix, hk_ix, idx].reshape(B, Hk, Q_BLOCK, n_top * SEL_BLOCK, dh)
        v_sel = vs_blocks[b_ix, hk_ix, idx].reshape(B, Hk, Q_BLOCK, n_top * SEL_BLOCK, dh)
        pos_s = (idx[..., None] * SEL_BLOCK + jnp.arange(SEL_BLOCK, dtype=jnp.int32)).reshape(
            B, Hk, Q_BLOCK, n_top * SEL_BLOCK)
        dist_s = t[:, None] - pos_s
        s_s = jnp.einsum('bhgqd,bhqkd->bhgqk', qb, k_sel, preferred_element_type=jnp.float32)
        s_s = s_s + tab[h_ix, g_ix, t5_bucket(dist_s)[:, :, None]]
        p_s = masked_softmax(s_s, (dist_s >= 0)[:, :, None])
        o_s = jnp.einsum('bhgqk,bhqkd->bhgqd', p_s.astype(v_sel.dtype), v_sel)

        kwb = lax.dynamic_slice_in_dim(kw_pad, q0, Q_BLOCK + WIN, axis=2)
        vwb = lax.dynamic_slice_in_dim(vw_pad, q0, Q_BLOCK + WIN, axis=2)
        pos_w = q0 - WIN + jnp.arange(Q_BLOCK + WIN, dtype=jnp.int32)
        dist_w = t[:, None] - pos_w[None, :]
        mask_w = (dist_w >= 0) & (dist_w < WIN) & (pos_w[None, :] >= 0)
        s_w = jnp.einsum('bhgqd,bhkd->bhgqk', qb, kwb, preferred_element_type=jnp.float32)
        s_w = s_w + tab[:, :, t5_bucket(dist_w)]
        p_w = masked_softmax(s_w, mask_w)
        o_w = jnp.einsum('bhgqk,bhkd->bhgqd', p_w.astype(vwb.dtype), vwb)

        return gb[..., 0:1] * o_c + gb[..., 1:2] * o_s + gb[..., 2:3] * o_w

    out = lax.map(block_fn, jnp.arange(n_q, dtype=jnp.int32))
    return out.transpose(1, 0, 4, 2, 3, 5).reshape(B, S, Hk * G * dh)


def conformer_conv(u, dw_w, dw_b, ln_g, ln_b):
    a, gate = jnp.split(u, 2, axis=-1)
    h = a * jax.nn.sigmoid(gate)
    h = lax.conv_general_dilated(h, dw_w, window_strides=(1,), padding=[(CONV_K - 1, 0)],
                                 dimension_numbers=('NWC', 'WIO', 'NWC'),
                                 feature_group_count=CONV_WIDTH) + dw_b
    return jax.nn.silu(layer_norm(h, ln_g, ln_b))


def memory_cross_attn(x, mem, mem_g, mem_b, wq, wkv, wo):
    B, S, D = x.shape
    m = layer_norm(mem, mem_g, mem_b)
    q = (x @ wq).reshape(B, S, X_HEADS, X_HEAD_DIM)
    k, v = jnp.split(m @ wkv, 2, axis=-1)
    k = k.reshape(B, -1, X_HEADS, X_HEAD_DIM)
    v = v.reshape(B, -1, X_HEADS, X_HEAD_DIM)
    s = jnp.einsum('bshd,bmhd->bhsm', q, k, preferred_element_type=jnp.float32) * (X_HEAD_DIM ** -0.5)
    p = jax.nn.softmax(s, axis=-1)
    o = jnp.einsum('bhsm,bmhd->bshd', p.astype(v.dtype), v).reshape(B, S, X_HEADS * X_HEAD_DIM)
    return o @ wo


def hier_moe(x2d, rg_w, rg_b, re_w, re_b, w_gate, w_up, w_down):
    T, D = x2d.shape
    xf = x2d.astype(jnp.float32)
    lg = xf @ rg_w.astype(jnp.float32) + rg_b.astype(jnp.float32)
    grp = jnp.argmax(lg, axis=-1).astype(jnp.int32)
    w_grp = jnp.take_along_axis(jax.nn.softmax(lg, axis=-1), grp[:, None], axis=-1)[:, 0]
    le = (xf @ re_w.astype(jnp.float32) + re_b.astype(jnp.float32)).reshape(T, MOE_GROUPS, MOE_EXPERTS_PER_GROUP)
    le_g = jnp.take_along_axis(le, grp[:, None, None], axis=1)[:, 0]
    top_v, top_i = lax.top_k(le_g, MOE_TOPK)
    w_tok = w_grp[:, None] * jax.nn.softmax(top_v, axis=-1)
    expert = grp[:, None] * MOE_EXPERTS_PER_GROUP + top_i

    A = T * MOE_TOPK
    e_flat = expert.reshape(A)
    tok_flat = jnp.repeat(jnp.arange(T, dtype=jnp.int32), MOE_TOPK)
    w_flat = w_tok.reshape(A)
    order = jnp.argsort(e_flat)
    e_s, tok_s, w_s = e_flat[order], tok_flat[order], w_flat[order]
    counts = jnp.zeros((MOE_EXPERTS,), jnp.int32).at[e_flat].add(1)
    starts = jnp.cumsum(counts) - counts
    padded = (counts + MOE_BLOCK - 1) // MOE_BLOCK * MOE_BLOCK
    pends = jnp.cumsum(padded)
    pstarts = pends - padded
    dest = pstarts[e_s] + (jnp.arange(A, dtype=jnp.int32) - starts[e_s])
    P = A + MOE_EXPERTS * MOE_BLOCK
    n_blk = P // MOE_BLOCK
    tok_buf = jnp.full((P,), T, jnp.int32).at[dest].set(tok_s)
    w_buf = jnp.zeros((P,), x2d.dtype).at[dest].set(w_s.astype(x2d.dtype))
    blk_e = jnp.minimum(jnp.searchsorted(pends, jnp.arange(n_blk, dtype=jnp.int32) * MOE_BLOCK, side='right'),
                        MOE_EXPERTS - 1).astype(jnp.int32)
    x_pad = jnp.concatenate([x2d, jnp.zeros((1, D), x2d.dtype)], axis=0)

    def run_block(args):
        tok, wb, e = args
        xb = x_pad[tok]
        h = jax.nn.silu(xb @ w_gate[e]) * (xb @ w_up[e])
        return (h @ w_down[e]) * wb[:, None]

    y = lax.map(run_block, (tok_buf.reshape(n_blk, MOE_BLOCK), w_buf.reshape(n_blk, MOE_BLOCK), blk_e))
    out = jnp.zeros((T + 1, D), x2d.dtype).at[tok_buf].add(y.reshape(P, D))
    return out[:T]


def hybrid_layer(x, mem, w_in, cmp_pe_k, cmp_w1_k, cmp_w2_k, cmp_pe_v, cmp_w1_v, cmp_w2_v, rel_table,
                 conv_dw_w, conv_dw_b, conv_ln_g, conv_ln_b, w_out, ln1_g, ln1_b,
                 mem_ln_g, mem_ln_b, xa_wq, xa_wkv, xa_wo, ln2_g, ln2_b,
                 rg_w, rg_b, re_w, re_b, w_gate, w_up, w_down, ln3_g, ln3_b):
    B, S, D = x.shape
    proj = x @ w_in
    cuts = [int(c) for c in np.cumsum(IN_SIZES)[:-1]]
    q, k_c, v_c, k_s, v_s, k_w, v_w, g, u = jnp.split(proj, cuts, axis=-1)

    def kv_heads(t):
        return t.reshape(B, S, NSA_KV_HEADS, HEAD_DIM).transpose(0, 2, 1, 3)

    q = q.reshape(B, S, NSA_KV_HEADS, NSA_GROUP, HEAD_DIM).transpose(0, 2, 3, 1, 4)
    gates = jax.nn.sigmoid(g).reshape(B, S, NSA_KV_HEADS, NSA_GROUP, 3).transpose(0, 2, 3, 1, 4)
    kc = compress(kv_heads(k_c), cmp_pe_k, cmp_w1_k, cmp_w2_k)
    vc = compress(kv_heads(v_c), cmp_pe_v, cmp_w1_v, cmp_w2_v)
    o_nsa = nsa_attention(q, kc, vc, kv_heads(k_s), kv_heads(v_s), kv_heads(k_w), kv_heads(v_w), gates, rel_table)
    o_conv = conformer_conv(u, conv_dw_w, conv_dw_b, conv_ln_g, conv_ln_b)
    mix = jnp.concatenate([o_nsa, o_conv], axis=-1) @ w_out
    x = layer_norm(DEEPNORM_ALPHA * x + mix, ln1_g, ln1_b)

    xa = memory_cross_attn(x, mem, mem_ln_g, mem_ln_b, xa_wq, xa_wkv, xa_wo)
    x = layer_norm(DEEPNORM_ALPHA * x + xa, ln2_g, ln2_b)

    moe = hier_moe(x.reshape(B * S, D), rg_w, rg_b, re_w, re_b, w_gate, w_up, w_down).reshape(B, S, D)
    return layer_norm(DEEPNORM_ALPHA * x + moe, ln3_g, ln3_b)


def setup_inputs(seed: int = 0) -> dict:
    key = jax.random.key(seed)
    keys = iter(jax.random.split(key, 48))

    def nrm(shape, scale):
        return jax.random.normal(next(keys), shape, jnp.float32) * scale

    def gain(shape):
        return 1.0 + nrm(shape, 0.02)

    L = DEPTH
    D = D_MODEL
    return {
        "x": nrm((BATCH, SEQ, D), 1.0),
        "mem": nrm((BATCH, MEM_LEN, D), 1.0),
        "ln_in_g": gain((D,)),
        "ln_in_b": nrm((D,), 0.02),
        "w_in": nrm((L, D, IN_COLS), D ** -0.5),
        "cmp_pe_k": nrm((L, CMP_BLOCK, HEAD_DIM), 0.1),
        "cmp_w1_k": nrm((L, CMP_BLOCK * HEAD_DIM, CMP_HIDDEN), (CMP_BLOCK * HEAD_DIM) ** -0.5),
        "cmp_w2_k": nrm((L, CMP_HIDDEN, HEAD_DIM), CMP_HIDDEN ** -0.5),
        "cmp_pe_v": nrm((L, CMP_BLOCK, HEAD_DIM), 0.1),
        "cmp_w1_v": nrm((L, CMP_BLOCK * HEAD_DIM, CMP_HIDDEN), (CMP_BLOCK * HEAD_DIM) ** -0.5),
        "cmp_w2_v": nrm((L, CMP_HIDDEN, HEAD_DIM), CMP_HIDDEN ** -0.5),
        "rel_table": nrm((REL_BUCKETS, NSA_HEADS), 0.2),
        "conv_dw_w": nrm((L, CONV_K, 1, CONV_WIDTH), CONV_K ** -0.5),
        "conv_dw_b": nrm((L, CONV_WIDTH), 0.02),
        "conv_ln_g": gain((L, CONV_WIDTH)),
        "conv_ln_b": nrm((L, CONV_WIDTH), 0.02),
        "w_out": nrm((L, D_MIX, D), D_MIX ** -0.5 * DEEPNORM_BETA),
        "ln1_g": gain((L, D)),
        "ln1_b": nrm((L, D), 0.02),
        "mem_ln_g": gain((L, D)),
        "mem_ln_b": nrm((L, D), 0.02),
        "xa_wq": nrm((L, D, X_HEADS * X_HEAD_DIM), D ** -0.5),
        "xa_wkv": nrm((L, D, 2 * X_HEADS * X_HEAD_DIM), D ** -0.5),
        "xa_wo": nrm((L, X_HEADS * X_HEAD_DIM, D), (X_HEADS * X_HEAD_DIM) ** -0.5 * DEEPNORM_BETA),
        "ln2_g": gain((L, D)),
        "ln2_b": nrm((L, D), 0.02),
        "router_group_w": nrm((L, D, MOE_GROUPS), D ** -0.5),
        "router_group_b": nrm((L, MOE_GROUPS), 0.01),
        "router_expert_w": nrm((L, D, MOE_EXPERTS), D ** -0.5),
        "router_expert_b": nrm((L, MOE_EXPERTS), 0.01),
        "moe_w_gate": nrm((L, MOE_EXPERTS, D, MOE_HIDDEN), D ** -0.5),
        "moe_w_up": nrm((L, MOE_EXPERTS, D, MOE_HIDDEN), D ** -0.5),
        "moe_w_down": nrm((L, MOE_EXPERTS, MOE_HIDDEN, D), MOE_HIDDEN ** -0.5 * DEEPNORM_BETA),
        "ln3_g": gain((L, D)),
        "ln3_b": nrm((L, D), 0.02),
    }


def reference(x, mem, ln_in_g, ln_in_b, w_in, cmp_pe_k, cmp_w1_k, cmp_w2_k, cmp_pe_v, cmp_w1_v, cmp_w2_v,
              rel_table, conv_dw_w, conv_dw_b, conv_ln_g, conv_ln_b, w_out, ln1_g, ln1_b,
              mem_ln_g, mem_ln_b, xa_wq, xa_wkv, xa_wo, ln2_g, ln2_b,
              router_group_w, router_group_b, router_expert_w, router_expert_b,
              moe_w_gate, moe_w_up, moe_w_down, ln3_g, ln3_b):
    h = layer_norm(x, ln_in_g, ln_in_b)
    for l in range(DEPTH):
        h = hybrid_layer(h, mem, w_in[l], cmp_pe_k[l], cmp_w1_k[l], cmp_w2_k[l], cmp_pe_v[l], cmp_w1_v[l],
                         cmp_w2_v[l], rel_table, conv_dw_w[l], conv_dw_b[l], conv_ln_g[l], conv_ln_b[l],
                         w_out[l], ln1_g[l], ln1_b[l], mem_ln_g[l], mem_ln_b[l], xa_wq[l], xa_wkv[l], xa_wo[l],
                         ln2_g[l], ln2_b[l], router_group_w[l], router_group_b[l], router_expert_w[l],
                         router_expert_b[l], moe_w_gate[l], moe_w_up[l], moe_w_down[l], ln3_g[l], ln3_b[l])
    return h
```

```python
import os
import math
from contextlib import ExitStack
import numpy as np
import concourse.bass as bass
import concourse.mybir as mybir
from concourse.bass_utils import run_bass_kernel_spmd

F32 = mybir.dt.float32
BF16 = mybir.dt.bfloat16
I32 = mybir.dt.int32
AF = mybir.ActivationFunctionType
ALU = mybir.AluOpType
AX = mybir.AxisListType

SEM_LIMIT = 30000
N_DMA_SLOTS = 12

D = 2048
KC = 16
SV = 8192
OWN = 4096
NT = 32
ALPHA = 2 ** 0.25
EPS = 1e-5
NEG = -30000.0
CAP = 512
NE = 32
O_KF, O_VT, O_Q, O_G, O_U = 0, 1024, 1536, 2560, 2608
PH_ALL = "A1,A2,B,C,D,E1,M,E2,E3,F,G"


class MK:
    ENG = ("pe", "act", "dve", "pool", "sp")

    def __init__(self, nc):
        self.nc = nc
        self.streams = {e: [] for e in self.ENG}
        self.cur_sem = {}
        self.cur_cnt = {}
        for e in ("pe", "act", "dve", "pool"):
            self.cur_sem[e] = nc.alloc_semaphore("s_" + e + "0")
            self.cur_cnt[e] = 0
        self.nsem = 4
        self.slots = {}
        self.slot_rr = {}
        for q in ("sp", "pool", "act"):
            self.slots[q] = [[nc.alloc_semaphore("d_%s%d" % (q, i)), 0] for i in range(N_DMA_SLOTS)]
            self.slot_rr[q] = 0
        self.seen = {e: {} for e in self.ENG}
        self.state = {}
        self.all_events = {}
        self.n_inst = {e: 0 for e in self.ENG}

    def _need(self, eng, reads, writes):
        need = {}

        def add(ev):
            if ev is None:
                return
            sem, val = ev
            k = id(sem)
            if k not in need or need[k][1] < val:
                need[k] = (sem, val)

        for k in reads:
            st = self.state.get(k)
            if st:
                add(st[0])
        for k in writes:
            st = self.state.get(k)
            if st:
                add(st[0])
                for ev in st[1].values():
                    add(ev)
        out = []
        for k, (sem, val) in need.items():
            if eng == "pe" and sem is self.cur_sem["pe"]:
                continue
            if self.seen[eng].get(k, 0) < val:
                self.seen[eng][k] = val
                out.append((sem, val))
        return out

    def _emit_waits(self, eng, waits):
        for sem, val in waits:
            self.streams[eng].append(lambda e, sem=sem, val=val: e.wait_ge(sem, val))

    def _commit(self, ev, reads, writes):
        sem, val = ev
        self.all_events[id(sem)] = ev
        for k in reads:
            st = self.state.setdefault(k, [None, {}])
            st[1][id(sem)] = ev
        for k in writes:
            self.state[k] = [ev, {}]

    def op(self, eng, fn, reads=(), writes=()):
        waits = self._need(eng, reads, writes)
        self._emit_waits(eng, waits)
        if self.cur_cnt[eng] >= SEM_LIMIT:
            self.cur_sem[eng] = self.nc.alloc_semaphore("s_%s%d" % (eng, self.nsem))
            self.nsem += 1
            self.cur_cnt[eng] = 0
        self.cur_cnt[eng] += 1
        sem, val = self.cur_sem[eng], self.cur_cnt[eng]
        self.streams[eng].append(lambda e, sem=sem: fn(e).then_inc(sem, 1))
        self.n_inst[eng] += 1
        self._commit((sem, val), reads, writes)

    def dma(self, q, fn, reads=(), writes=()):
        slot = self.slots[q][self.slot_rr[q]]
        self.slot_rr[q] = (self.slot_rr[q] + 1) % N_DMA_SLOTS
        sem, prev = slot
        waits = self._need(q, reads, writes)
        k = id(sem)
        if self.seen[q].get(k, 0) < prev:
            self.seen[q][k] = prev
            waits.append((sem, prev))
        self._emit_waits(q, waits)
        val = prev + 16
        slot[1] = val
        self.streams[q].append(lambda e, sem=sem: fn(e).then_inc(sem, 16))
        self.n_inst[q] += 1
        self._commit((sem, val), reads, writes)

    def barrier(self):
        evs = list(self.all_events.values())
        for eng in self.ENG:
            for sem, val in evs:
                k = id(sem)
                if eng == "pe" and sem is self.cur_sem["pe"]:
                    continue
                if self.seen[eng].get(k, 0) < val:
                    self.seen[eng][k] = val
                    self.streams[eng].append(lambda e, sem=sem, val=val: e.wait_ge(sem, val))
        self.state = {}

    def finish(self):
        self.barrier()
        nc = self.nc
        with nc.Block() as block:
            @block.tensor
            def _(e):
                for f in self.streams["pe"]:
                    f(e)

            @block.scalar
            def _(e):
                for f in self.streams["act"]:
                    f(e)

            @block.vector
            def _(e):
                for f in self.streams["dve"]:
                    f(e)

            @block.gpsimd
            def _(e):
                for f in self.streams["pool"]:
                    f(e)

            @block.sync
            def _(e):
                for f in self.streams["sp"]:
                    f(e)

    def mm(self, out, lhsT, rhs, start, stop, r, w, skip=False):
        self.op("pe", lambda e: e.matmul(out, lhsT=lhsT, rhs=rhs, start=start, stop=stop,
                                         skip_group_check=skip), r, w)

    def tr(self, out, in_, ident, r, w):
        self.op("pe", lambda e: e.transpose(out, in_, ident), r, w)

    def act(self, out, in_, func, r, w, bias=None, scale=None, accum=None):
        kw = {}
        if bias is not None:
            kw["bias"] = bias
        if scale is not None:
            kw["scale"] = scale
        if accum is not None:
            kw["accum_out"] = accum
        self.op("act", lambda e: e.activation(out=out, in_=in_, func=func, **kw), r, w)

    def tt(self, eng, out, in0, in1, op, r, w):
        self.op(eng, lambda e: e.tensor_tensor(out=out, in0=in0, in1=in1, op=op), r, w)

    def ts(self, eng, out, in0, s1, s2, op0, op1, r, w):
        if op1 is None:
            self.op(eng, lambda e: e.tensor_scalar(out=out, in0=in0, scalar1=s1, scalar2=None, op0=op0), r, w)
        else:
            self.op(eng, lambda e: e.tensor_scalar(out=out, in0=in0, scalar1=s1, scalar2=s2, op0=op0, op1=op1), r, w)

    def stt(self, out, in0, scalar, in1, op0, op1, r, w):
        self.op("dve", lambda e: e.scalar_tensor_tensor(out=out, in0=in0, scalar=scalar, in1=in1, op0=op0, op1=op1), r, w)

    def cp(self, eng, out, in_, r, w):
        if eng == "act":
            self.op("act", lambda e: e.activation(out=out, in_=in_, func=AF.Copy), r, w)
        else:
            self.op(eng, lambda e: e.tensor_copy(out=out, in_=in_), r, w)

    def ld(self, q, out, in_, r, w):
        self.dma(q, lambda e: e.dma_start(out=out, in_=in_), r, w)


def build(phases=PH_ALL, debug=()):
    phases = phases.split(",")
    nc = bass.Bass("TRN2", target_bir_lowering=False)
    m = MK(nc)

    def din(name, shape, dt=F32):
        return nc.dram_tensor(name, list(shape), dt, kind="ExternalInput").ap()

    def dscr(name, shape, dt):
        return nc.dram_tensor(name, list(shape), dt, kind="ExternalOutput" if name in debug else "Internal").ap()

    xv = din("xv", [SV, D])
    memb = din("memb", [256, D])
    lnv = din("lnv", [10, D])
    w_inp = din("w_inp", [D, 4656])
    w1k = din("w1k", [64, 32 * 256]); w1v = din("w1v", [64, 32 * 256])
    pek = din("pek", [64, 32]); pev = din("pev", [64, 32])
    w2k = din("w2k", [256, 64]); w2v = din("w2v", [256, 64])
    diag_raw = din("diag_raw", [128, 16 * 128]); sub1_raw = din("sub1_raw", [128, 16 * 128])
    band_raw = din("band_raw", [16, 16 * 128]); r31 = din("r31", [128, 16])
    diag_mask = din("diag_mask", [128, 128]); anti_mask = din("anti_mask", [128, 128]); band_mask = din("band_mask", [16, 128])
    dw_c = din("dw_c", [16, 272]); ident_c = din("ident_c", [128, 128]); ut_c = din("ut_c", [128, 128])
    ov_c = din("ov_c", [512, 128]); ec_c = din("ec_c", [128, 32]); f0_c = din("f0_c", [128, 128]); band3_c = din("band3_c", [128, 3])
    asel_c = din("asel_c", [64, SV]); keybias_c = din("keybias_c", [128, 64]); cmpbias_c = din("cmpbias_c", [128, 4]); halo_c = din("halo_c", [128, 1])
    dww = din("dww", [128, 8 * 31]); cvec = din("cvec", [128, 24])
    w_out = din("w_out", [D, D]); xa_wq = din("xa_wq", [D, D]); xa_wkv = din("xa_wkv", [D, 2 * D]); xa_wo = din("xa_wo", [D, D])
    rw = din("rw", [D, 36]); rb = din("rb", [1, 36])
    mwg = din("mwg", [NE, D, 512]); mwu = din("mwu", [NE, D, 512]); mwd = din("mwd", [NE, 512, D])
    out = nc.dram_tensor("out", [OWN, D], F32, kind="ExternalOutput").ap()

    kcmpT_d = dscr("kcmpT_d", [4, 64, SV], BF16); vcmpT_d = dscr("vcmpT_d", [4, 64, SV], BF16)
    kslcT_d = dscr("kslcT_d", [4, 64, SV], BF16); kwinT_d = dscr("kwinT_d", [4, 64, SV], BF16)
    vslc_d = dscr("vslc_d", [SV, 256], BF16); vwin_d = dscr("vwin_d", [SV, 256], BF16)
    qT_d = dscr("qT_d", [16, 64, OWN], BF16)
    gates_d = dscr("gates_d", [OWN, 48], F32)
    hconvT_d = dscr("hconvT_d", [8, 128, 128 + OWN], BF16)
    o_d = dscr("o_d", [OWN, 1024], BF16)
    oconvT_d = dscr("oconvT_d", [8, 128, OWN], BF16)
    x1_d = dscr("x1_d", [OWN, D], F32); x1T_d = dscr("x1T_d", [KC, 128, OWN], BF16)
    oxT_d = dscr("oxT_d", [KC, 128, OWN], BF16)
    x2_d = dscr("x2_d", [OWN, D], F32)
    xs_d = dscr("xs_d", [NE * CAP, D], BF16)
    ys_d = dscr("ys_d", [NE * CAP, D], F32)
    rt_d = dscr("rt_d", [OWN, 4], F32)

    ident_f = nc.alloc_sbuf_tensor("ident_f", [128, 128], F32)
    ident_b = nc.alloc_sbuf_tensor("ident_b", [128, 128], BF16)
    ones_b = nc.alloc_sbuf_tensor("ones_b", [128, 128], BF16)
    ones_f = nc.alloc_sbuf_tensor("ones_f", [128, 128], F32)
    lnrep = nc.alloc_sbuf_tensor("lnrep", [128, 2, D], F32)
    PS = [nc.alloc_psum_tensor("ps%d" % i, [128, 512], F32) for i in range(6)]
    PB = [nc.alloc_psum_tensor("pb%d" % i, [128, 1024], BF16) for i in range(2)]
    PSK = ["ps%d" % i for i in range(6)]
    PBK = ["pb%d" % i for i in range(2)]

    m.ld("sp", ident_f[:], ident_c, [], ["ident_f"])
    m.ld("pool", ident_b[:], ident_c, [], ["ident_b"])
    m.op("dve", lambda e: e.memset(ones_b[:], 1.0), [], ["ones_b"])
    m.op("dve", lambda e: e.memset(ones_f[:], 1.0), [], ["ones_f"])

    def load_ln(row):
        m.ld("sp", lnrep[:, 0, :], lnv[row:row + 1, :].broadcast_to([128, D]), [], ["lnrep"])
        m.ld("sp", lnrep[:, 1, :], lnv[row + 1:row + 2, :].broadcast_to([128, D]), [], ["lnrep"])

    uid = {"n": 0}

    class Phase:
        def __init__(self):
            self.es = ExitStack()
            self.n = 0

        def sb(self, name, shape, dt):
            uid["n"] += 1
            return self.es.enter_context(nc.sbuf_tensor("%s_%d" % (name, uid["n"]), list(shape), dt))

        def close(self):
            m.barrier()
            self.es.close()

    rr = {"i": 0}

    def layer_norm(ph_tmp, src, src_keys, dst, dst_keys, np_=128, last_eng="pool"):
        st, mv, rs, nb = ph_tmp
        for c in range(4):
            m.op("dve", lambda e, c=c: e.bn_stats(out=st[:np_, c, :], in_=src[:, c * 512:(c + 1) * 512]), src_keys, ["ln_st"])
        m.op("dve", lambda e: e.bn_aggr(out=mv[:np_, :], in_=st[:np_, :, :]), ["ln_st"], ["ln_mv"])
        m.act(rs[:np_, :], mv[:np_, 1:2], AF.Sqrt, ["ln_mv"], ["ln_rs"], bias=EPS)
        m.op("dve", lambda e: e.reciprocal(out=rs[:np_, :], in_=rs[:np_, :]), ["ln_rs"], ["ln_rs"])
        m.stt(nb[:np_, :], mv[:np_, 0:1], -1.0, rs[:np_, :], ALU.mult, ALU.mult, ["ln_mv", "ln_rs"], ["ln_nb"])
        m.act(src, src, AF.Identity, src_keys + ["ln_rs", "ln_nb"], src_keys, bias=nb[:np_, :], scale=rs[:np_, :])
        m.tt("dve", src, src, lnrep[:np_, 0, :], ALU.mult, src_keys + ["lnrep"], src_keys)
        m.tt(last_eng, dst, src, lnrep[:np_, 1, :], ALU.add, src_keys + ["lnrep"], dst_keys)

    def ln_tmps(ph):
        return (ph.sb("ln_st", [128, 4, 6], F32), ph.sb("ln_mv", [128, 2], F32),
                ph.sb("ln_rs", [128, 1], F32), ph.sb("ln_nb", [128, 1], F32))

    def transpose_to(hT, hT_key, src_bf, src_keys, ntile_idx, nk=KC, col0=0, only=None):
        for k0 in range(0, nk, 8):
            pi = rr["i"] % 2
            rr["i"] += 1
            pb = PB[pi]
            n8 = min(8, nk - k0)
            for kk in range(n8):
                m.tr(pb[:, kk * 128:(kk + 1) * 128], src_bf[:, (k0 + kk) * 128:(k0 + kk + 1) * 128], ident_b[:],
                     src_keys + ["ident_b"], [PBK[pi]])
            eng = only or ("dve" if (rr["i"] % 2) else "act")
            m.cp(eng, hT[:, k0:k0 + n8, col0 + ntile_idx * 128: col0 + (ntile_idx + 1) * 128],
                 pb[:, 0:n8 * 128].rearrange("p (k t) -> p k t", k=n8), [PBK[pi]], [hT_key])

    def phase_A(own_pass):
        ph = Phase()
        tm = ln_tmps(ph)
        load_ln(0)
        if not own_pass:
            wkf = ph.sb("wkf", [128, KC, 1024], BF16)
            wvt = ph.sb("wvt", [128, KC, 512], BF16)
            for k in range(KC):
                m.ld("pool", wkf[:, k, :], w_inp[k * 128:(k + 1) * 128, O_KF:O_KF + 1024], [], ["wkf"])
                m.ld("pool", wvt[:, k, :], w_inp[k * 128:(k + 1) * 128, O_VT:O_VT + 512], [], ["wvt"])
        else:
            wq = ph.sb("wq", [128, KC, 1024], BF16)
            wg = ph.sb("wg", [128, KC, 48], BF16)
            wu = ph.sb("wu", [128, KC, 2048], BF16)
            hal = ph.sb("hal", [128, 1], F32)
            m.ld("sp", hal[:], halo_c, [], ["hal"])
            for k in range(KC):
                m.ld("pool", wq[:, k, :], w_inp[k * 128:(k + 1) * 128, O_Q:O_Q + 1024], [], ["wq"])
                m.ld("pool", wg[:, k, :], w_inp[k * 128:(k + 1) * 128, O_G:O_G + 48], [], ["wg"])
                m.ld("pool", wu[:, k, :], w_inp[k * 128:(k + 1) * 128, O_U:O_U + 2048], [], ["wu"])
        xt = [ph.sb("xt%d" % i, [128, D], F32) for i in range(2)]
        hb = [ph.sb("hb%d" % i, [128, D], BF16) for i in range(4)]
        hT = [ph.sb("hT%d" % i, [128, KC, 512], BF16) for i in range(2)]
        stg = [ph.sb("stg%d" % i, [128, 4, 512], BF16) for i in range(2)]
        vst = [ph.sb("vst%d" % i, [128, 512], BF16) for i in range(2)]
        sig = [ph.sb("sig%d" % i, [128, 512], F32) for i in range(2)]
        gst = ph.sb("gst", [128, 48], F32)
        psi = {"i": 0}

        def nextps():
            i = psi["i"] % 6
            psi["i"] += 1
            return PS[i], PSK[i]

        if not own_pass:
            blocks = [(b * 512, 4) for b in range(16)]
        else:
            blocks = [(OWN - 128, 1)] + [(OWN + b * 512, 4) for b in range(8)]

        def ln_tile(bi, ti):
            tok0 = blocks[bi][0]
            x_ = xt[ti % 2]; xk = "xt%d" % (ti % 2)
            m.ld("sp", x_[:], xv[tok0 + ti * 128: tok0 + (ti + 1) * 128, :], [], [xk])
            layer_norm(tm, x_[:], [xk], hb[ti][:], ["hb%d" % ti])

        def tr_block(bi):
            for ti in range(blocks[bi][1]):
                transpose_to(hT[bi % 2], "hT%d" % (bi % 2), hb[ti], ["hb%d" % ti], ti)

        def groups(bi):
            tok0, ntile = blocks[bi]
            hTb = hT[bi % 2]; hk = "hT%d" % (bi % 2)
            ntok = ntile * 128
            gl = []
            if not own_pass:
                for kind, dst in enumerate((kcmpT_d, vcmpT_d, kslcT_d, kwinT_d)):
                    for pp in range(2):
                        def g_(kind=kind, dst=dst, pp=pp):
                            sg = stg[kind % 2]; sk = "stg%d" % (kind % 2)
                            ps, pk = nextps()
                            c0 = kind * 256 + pp * 128
                            for k in range(KC):
                                m.mm(ps[:, 0:ntok], wkf[:, k, c0:c0 + 128], hTb[:, k, 0:ntok], k == 0, k == KC - 1,
                                     ["wkf", hk], [pk])
                            m.cp("act" if pp % 2 else "dve", sg[:, pp, 0:ntok], ps[:, 0:ntok], [pk], [sk])
                            if pp == 1:
                                m.ld("pool", dst.rearrange("h d t -> (h d) t").rearrange("(a p) t -> p a t", p=128)[:, :, tok0:tok0 + ntok],
                                     sg[:, 0:2, 0:ntok], [sk], [])
                        gl.append(g_)
                for ti in range(ntile):
                    def g_(ti=ti):
                        ps, pk = nextps()
                        for k in range(KC):
                            m.mm(ps[:, :], hTb[:, k, ti * 128:(ti + 1) * 128], wvt[:, k, :], k == 0, k == KC - 1, ["wvt", hk], [pk])
                        v_ = vst[ti % 2]; vk = "vst%d" % (ti % 2)
                        m.cp("act" if ti % 2 else "dve", v_[:], ps[:, :], [pk], [vk])
                        r0 = tok0 + ti * 128
                        m.ld("pool", vslc_d[r0:r0 + 128, :], v_[:, 0:256], [vk], [])
                        m.ld("pool", vwin_d[r0:r0 + 128, :], v_[:, 256:512], [vk], [])
                    gl.append(g_)
            else:
                halo = (bi == 0)
                if not halo:
                    o0 = tok0 - OWN
                    for hg in range(4):
                        for pp in range(2):
                            def g_(hg=hg, pp=pp):
                                sg = stg[hg % 2]; sk = "stg%d" % (hg % 2)
                                c0 = (hg * 4 + 2 * pp) * 64
                                ps, pk = nextps()
                                for k in range(KC):
                                    m.mm(ps[:, 0:ntok], wq[:, k, c0:c0 + 128], hTb[:, k, 0:ntok], k == 0, k == KC - 1,
                                         ["wq", hk], [pk])
                                m.act(sg[:, pp, 0:ntok], ps[:, 0:ntok], AF.Copy, [pk], [sk], scale=0.125)
                                if pp == 1:
                                    m.ld("pool", qT_d[hg * 4:(hg + 1) * 4].rearrange("h d t -> (h d) t").rearrange("(a p) t -> p a t", p=128)[:, :, o0:o0 + ntok],
                                         sg[:, 0:2, 0:ntok], [sk], [])
                            gl.append(g_)
                    for ti in range(ntile):
                        def g_(ti=ti):
                            ps, pk = nextps()
                            for k in range(KC):
                                m.mm(ps[:, 0:48], hTb[:, k, ti * 128:(ti + 1) * 128], wg[:, k, :], k == 0, k == KC - 1, ["wg", hk], [pk])
                            m.act(gst[:], ps[:, 0:48], AF.Sigmoid, [pk], ["gst"])
                            m.ld("pool", gates_d[o0 + ti * 128:o0 + (ti + 1) * 128, :], gst[:], ["gst"], [])
                        gl.append(g_)
                for cc in range(8):
                    def g_(cc=cc):
                        pa, pak = nextps()
                        pg, pgk = nextps()
                        for k in range(KC):
                            m.mm(pa[:, 0:ntok], wu[:, k, cc * 128:(cc + 1) * 128], hTb[:, k, 0:ntok], k == 0, k == KC - 1, ["wu", hk], [pak])
                        for k in range(KC):
                            m.mm(pg[:, 0:ntok], wu[:, k, 1024 + cc * 128:1024 + (cc + 1) * 128], hTb[:, k, 0:ntok], k == 0, k == KC - 1, ["wu", hk], [pgk])
                        s_ = sig[cc % 2]; sk2 = "sig%d" % (cc % 2)
                        v_ = vst[cc % 2]; vk = "vst%d" % (cc % 2)
                        m.act(s_[:, 0:ntok], pg[:, 0:ntok], AF.Sigmoid, [pgk], [sk2])
                        if halo:
                            m.ts("dve", s_[:, 0:ntok], s_[:, 0:ntok], hal[:, 0:1], None, ALU.mult, None, [sk2, "hal"], [sk2])
                        m.tt("dve", v_[:, 0:ntok], pa[:, 0:ntok], s_[:, 0:ntok], ALU.mult, [pak, sk2], [vk])
                        c0 = 0 if halo else 128 + (tok0 - OWN)
                        m.ld("pool", hconvT_d[cc, :, c0:c0 + ntok], v_[:, 0:ntok], [vk], [])
                    gl.append(g_)
            return gl

        for ti in range(blocks[0][1]):
            ln_tile(0, ti)
        tr_block(0)
        for bi in range(len(blocks)):
            gs = groups(bi)
            nxt = bi + 1 < len(blocks)
            nt_n = blocks[bi + 1][1] if nxt else 0
            npart = nt_n + 1
            bounds = [len(gs) * p // npart for p in range(npart + 1)]
            for p in range(npart):
                for g_ in gs[bounds[p]:bounds[p + 1]]:
                    g_()
                if p < nt_n:
                    ln_tile(bi + 1, p)
            if nxt:
                tr_block(bi + 1)
        ph.close()

    if "A1" in phases:
        phase_A(False)
    if "A2" in phases:
        phase_A(True)

    def phase_BC():
        ph = Phase()
        kcT = ph.sb("kcT", [128, 4, 512], BF16)
        vcx = ph.sb("vcx", [128, 4, 4, 193], BF16)
        m.op("dve", lambda e: e.memset(kcT[:], 0.0), [], ["kcT"])
        m.op("dve", lambda e: e.memset(vcx[:], 0.0), [], ["vcx"])
        for h in range(4):
            m.op("dve", lambda e, h=h: e.memset(vcx[:, :, h, 64:65], 1.0), [], ["vcx"])
            m.ld("pool", vcx[:, :, h, 65:193], ov_c.rearrange("(t p) j -> p t j", p=128), [], ["vcx"])
        pb_ = Phase()
        w1s = pb_.sb("w1s", [64, 32, 256], BF16)
        w2s = pb_.sb("w2s", [128, 2, 64], BF16)
        pes = pb_.sb("pes", [64, 32], BF16)
        cj = pb_.sb("cj", [128, 2], F32)
        Tin = [pb_.sb("Tin%d" % i, [64, SV], BF16) for i in range(2)]
        T2 = [pb_.sb("T2_%d" % i, [64, 16, 512], BF16) for i in range(2)]
        gu = pb_.sb("gu", [128, 512], F32); g2 = pb_.sb("g2", [128, 512], F32)
        gT = pb_.sb("gT", [128, 2, 512], BF16)
        m.op("dve", lambda e: e.memset(gT[:], 0.0), [], ["gT"])
        for kv in range(2):
            m.ld("pool", w1s[:], (w1k, w1v)[kv].rearrange("d (r j) -> d r j", r=32), [], ["w1s"])
            m.ld("pool", w2s[:], (w2k, w2v)[kv].rearrange("(c p) d -> p c d", p=128), [], ["w2s"])
            m.ld("pool", pes[:], (pek, pev)[kv], [], ["pes"])
            for jc in range(2):
                for r in range(32):
                    m.mm(PS[0][:, jc * 2:jc * 2 + 2], w1s[:, r, jc * 128:(jc + 1) * 128], pes[:, r:r + 1].broadcast_to([64, 2]), r == 0, r == 31,
                         ["w1s", "pes"], ["ps0"], skip=True)
            m.cp("dve", cj[:], PS[0][:, 0:4:2], ["ps0"], ["cj"])
            for h in range(4):
                T_ = Tin[h % 2]; tk = "Tin%d" % (h % 2)
                m.ld("sp", T_[:], (kcmpT_d, vcmpT_d)[kv][h], [], [tk])
                t2 = T2[h % 2]; t2k = "T2_%d" % (h % 2)
                for r8 in range(0, 16, 8):
                    m.cp("dve" if r8 else "pool", t2[:, r8:r8 + 8, :], T_[:].rearrange("d (n r) -> d r n", r=16)[:, r8:r8 + 8, :], [tk], [t2k])
                for jc in range(2):
                    ps, pk = PS[1 + jc], PSK[1 + jc]
                    for r in range(32):
                        m.mm(ps[:, 0:511], w1s[:, r, jc * 128:(jc + 1) * 128], t2[:, r % 16, r // 16:r // 16 + 511], r == 0, r == 31,
                             ["w1s", t2k], [pk])
                    m.act(gu[:, 0:511], ps[:, 0:511], AF.Identity, [pk, "cj"], ["gu"], bias=cj[:, jc:jc + 1])
                    m.tt("dve", g2[:, 0:511], gu[:, 0:511], gu[:, 0:511], ALU.mult, ["gu"], ["g2"])
                    m.ts("dve", g2[:, 0:511], g2[:, 0:511], 0.044715, 1.0, ALU.mult, ALU.add, ["g2"], ["g2"])
                    m.tt("dve", g2[:, 0:511], g2[:, 0:511], gu[:, 0:511], ALU.mult, ["g2", "gu"], ["g2"])
                    m.act(g2[:, 0:511], g2[:, 0:511], AF.Tanh, ["g2"], ["g2"], scale=0.7978845608028654)
                    m.ts("dve", g2[:, 0:511], g2[:, 0:511], 1.0, 0.5, ALU.add, ALU.mult, ["g2"], ["g2"])
                    m.tt("dve", gT[:, jc, 0:511], g2[:, 0:511], gu[:, 0:511], ALU.mult, ["g2", "gu"], ["gT"])
                if kv == 0:
                    for jc in range(2):
                        m.mm(PS[3][0:64, 0:512], w2s[:, jc, :], gT[:, jc, :], jc == 0, jc == 1, ["w2s", "gT"], ["ps3"])
                    m.cp("dve", kcT[0:64, h, 0:511], PS[3][0:64, 0:511], ["ps3"], ["kcT"])
                else:
                    for bt in range(4):
                        for jc in range(2):
                            m.mm(PS[3][:, bt * 64:(bt + 1) * 64], gT[:, jc, bt * 128:(bt + 1) * 128], w2s[:, jc, :], jc == 0, jc == 1,
                                 ["w2s", "gT"], ["ps3"], skip=True)
                    m.cp("dve", vcx[:, :, h, 0:64], PS[3][:, 0:256].rearrange("p (t d) -> p t d", t=4), ["ps3"], ["vcx"])
        pb_.close()
        if "C" not in phases:
            ph.close()
            return
        tabs = ph.sb("tabs", [128, 2, 2048], BF16)
        anti = ph.sb("anti", [128, 512], BF16)
        bandt = ph.sb("bandt", [16, 2048], BF16)
        dwt = ph.sb("dwt", [16, 272], BF16)
        i4 = ph.sb("i4", [128, 512], BF16)
        f0 = ph.sb("f0", [128, 128], F32); b3 = ph.sb("b3", [128, 3], F32)
        kbias = ph.sb("kbias", [128, 64], F32); cbias = ph.sb("cbias", [128, 4], F32)
        with ExitStack() as es:
            raw = es.enter_context(nc.sbuf_tensor("raw_t", [128, 16, 128], F32))
            r31s = es.enter_context(nc.sbuf_tensor("r31s", [128, 16], F32))
            msk = es.enter_context(nc.sbuf_tensor("msk", [128, 128], F32))
            m.ld("sp", r31s[:], r31, [], ["r31s"])
            m.ld("sp", raw[:].rearrange("p h q -> p (h q)"), diag_raw, [], ["raw"])
            m.ld("sp", msk[:], diag_mask, [], ["msk"])
            for hd in range(16):
                m.stt(tabs[:, 0, hd * 128:(hd + 1) * 128], raw[:, hd, :], r31s[:, hd:hd + 1], msk[:], ALU.subtract, ALU.add, ["raw", "r31s", "msk"], ["tabs"])
            m.ld("sp", raw[:].rearrange("p h q -> p (h q)"), sub1_raw, [], ["raw"])
            for hd in range(16):
                m.ts("dve", tabs[:, 1, hd * 128:(hd + 1) * 128], raw[:, hd, :], r31s[:, hd:hd + 1], None, ALU.subtract, None, ["raw", "r31s"], ["tabs"])
            m.ld("sp", raw[0:16].rearrange("p h q -> p (h q)"), band_raw, [], ["raw"])
            m.ld("sp", msk[0:16, :], band_mask, [], ["msk"])
            for hd in range(16):
                m.stt(bandt[:, hd * 128:(hd + 1) * 128], raw[0:16, hd, :], r31s[0:16, hd:hd + 1], msk[0:16, :], ALU.subtract, ALU.add, ["raw", "r31s", "msk"], ["bandt"])
            m.ld("sp", msk[:], anti_mask, [], ["msk"])
            for g in range(4):
                m.cp("dve", anti[:, g * 128:(g + 1) * 128], msk[:], ["msk"], ["anti"])
                m.cp("dve", i4[:, g * 128:(g + 1) * 128], ident_f[:], ["ident_f"], ["i4"])
            m.barrier()
        m.ld("pool", dwt[:], dw_c, [], ["dwt"])
        m.ld("sp", f0[:], f0_c, [], ["f0"]); m.ld("sp", b3[:], band3_c, [], ["b3"])
        m.ld("sp", kbias[:], keybias_c, [], ["kbias"]); m.ld("sp", cbias[:], cmpbias_c, [], ["cbias"])

        kslc = ph.sb("kslc", [128, SV], BF16)
        kwin = ph.sb("kwin", [128, OWN + 512], BF16)
        vslc = ph.sb("vslc", [128, 64, 65], BF16)
        vwin = ph.sb("vwin", [128, 36, 65], BF16)
        qT = ph.sb("qT", [64, NT, 512], BF16)
        gts = ph.sb("gts", [128, NT, 12], F32)
        pT = [ph.sb("pT%d" % i, [128, 512], BF16) for i in range(6)]
        mneg = [ph.sb("mneg%d" % i, [128, 192], BF16) for i in range(2)]
        QM = [[ph.sb("QM%d%d" % (i, c), [128, 512], BF16) for c in range(2)] for i in range(3)]
        for i in range(2):
            m.op("pool", lambda e, i=i: e.memset(mneg[i][:], 0.0), [], ["mneg%d" % i])
        for i in range(3):
            for c in range(2):
                m.op("pool", lambda e, i=i, c=c: e.memset(QM[i][c][:], 0.0), [], ["QM%d" % i])
        m.op("pool", lambda e: e.memset(kwin[64:128, :], 0.0), [], ["kwin_hi"])
        for c4 in range(4):
            m.ld("pool", kslc[64:128, c4 * 2048:(c4 + 1) * 2048], asel_c[:, c4 * 2048:(c4 + 1) * 2048], [], ["kslc_hi"])
        sc = ph.sb("sc", [128, 128], F32); sc2 = ph.sb("sc2", [128, 128], F32)
        mx = ph.sb("mx", [128, 16], F32)
        acc = [ph.sb("acc%d" % i, [128, 3, 4, 65], F32) for i in range(2)]
        rinv = [ph.sb("rinv%d" % i, [128, 12], F32) for i in range(2)]
        ob = [ph.sb("ob%d" % i, [128, 256], BF16) for i in range(2)]
        of = ph.sb("of", [128, 4, 64], F32)
        m.op("dve", lambda e: e.memset(vslc[:, :, 64:65], 1.0), [], ["vslc"])
        m.op("dve", lambda e: e.memset(vwin[:, :, 64:65], 1.0), [], ["vwin"])
        SCB = [PS[0][:], PS[1][:], PS[2][:], PB[1][:].bitcast(F32)]
        SCK = ["ps0", "ps1", "ps2", "pb1"]
        WACC = PB[0][:].bitcast(F32)
        sidx = {"i": 0}
        pidx = {"i": 0}
        DEPTH = 3
        pend = []
        delayed = []

        def run_delayed():
            while delayed:
                delayed.pop(0)[1]()

        def flush_one():
            u = pend.pop(0)
            u[0]()
            for p in u[1]:
                p()

        def add_unit(kT_ap, kkeys, rows, qap, extra, bias_ap, bias_keys, accps, acck, v_ap, vkeys, ncol, first, last):
            si = sidx["i"] % 4; sidx["i"] += 1
            ps, pk = SCB[si], SCK[si]
            n_ex = len(extra)
            m.mm(ps[0:rows, :], kT_ap, qap, True, n_ex == 0, kkeys + ["qT"], [pk])
            for xi, (l_ap, r_ap, ks) in enumerate(extra):
                m.mm(ps[0:rows, :], l_ap, r_ap, False, xi == n_ex - 1, ks, [pk])

            def pv():
                pi = pidx["i"] % 6; pidx["i"] += 1
                p_, ppk = pT[pi], "pT%d" % pi
                m.act(p_[0:rows, :], ps[0:rows, :], AF.Exp, [pk] + bias_keys, [ppk], bias=bias_ap)
                for g in range(4):
                    m.mm(accps[:, g * ncol:(g + 1) * ncol] if ncol == 65 else accps[g // 2][:, (g % 2) * ncol:(g % 2 + 1) * ncol],
                         p_[0:rows, g * 128:(g + 1) * 128], v_ap, first and (g == 0 or (ncol != 65 and g == 2)), last and g == 3,
                         [ppk] + vkeys, acck, skip=True)

            pend.append((pv, []))
            if len(pend) > DEPTH:
                flush_one()
            for dl in list(delayed):
                dl[0] -= 1
                if dl[0] <= 0:
                    delayed.remove(dl)
                    dl[1]()

        def add_post(fn):
            if pend:
                pend[-1][1].append(fn)
            else:
                fn()

        for h in range(4):
            for g in range(4):
                m.ld("sp", qT[:].rearrange("d l (g q) -> d l g q", g=4)[:, :, g, :],
                     qT_d[h * 4 + g].rearrange("d (l q) -> d l q", q=128), [], ["qT"])
            m.ld("sp", gts[:], gates_d[:, h * 12:(h + 1) * 12].rearrange("(t p) c -> p t c", p=128), [], ["gts"])
            m.ld("sp", kwin[0:64, :], kwinT_d[h][:, OWN - 512:SV], [], ["kwin"])
            for t8 in range(4):
                m.ld("sp", vwin[:, t8 * 9:(t8 + 1) * 9, 0:64],
                     vwin_d[OWN - 512 + t8 * 1152:OWN - 512 + (t8 + 1) * 1152, h * 64:(h + 1) * 64].rearrange("(t p) d -> p t d", p=128), [], ["vwin"])
            m.ld("sp", kslc[0:64, :], kslcT_d[h], [], ["kslc"])
            for t8 in range(8):
                m.ld("sp", vslc[:, t8 * 8:(t8 + 1) * 8, 0:64],
                     vslc_d[t8 * 1024:(t8 + 1) * 1024, h * 64:(h + 1) * 64].rearrange("(t p) d -> p t d", p=128), [], ["vslc"])

            def emit_cmp(lt, h=h):
                t = 32 + lt
                qmk = "QM%d" % (lt % 3)
                for l2 in ([0, 1] if lt == 0 else [lt + 1]):
                    if l2 < NT:
                        for c in range(2):
                            m.cp("pool", QM[l2 % 3][c][0:64, :], qT[:, l2, :], ["qT"], ["QM%d" % (l2 % 3)])
                qap = QM[lt % 3][0][:]
                a_ = acc[lt % 2]; ak = "acc%d" % (lt % 2)
                rv = rinv[lt % 2]; rvk = "rinv%d" % (lt % 2)
                n_c = 8 * t + 7
                ntile = (n_c + 127) // 128
                b0 = 8 * t - 9
                jb, off = b0 // 128, b0 % 128
                for J in range(ntile):
                    rows = min(128, n_c - 128 * J)
                    extra = []
                    if J == jb:
                        extra.append((dwt[:, 128 - off:128 - off + rows], bandt[:, h * 512:(h + 1) * 512], ["dwt", "bandt"]))
                    elif J == jb + 1:
                        extra.append((dwt[:, 256 - off:256 - off + rows], bandt[:, h * 512:(h + 1) * 512], ["dwt", "bandt"]))
                    add_unit(kcT[:, h, J * 128:J * 128 + rows], ["kcT", qmk], rows, qap, extra, cbias[0:rows, J:J + 1], ["cbias"],
                             (PS[4], PS[5]), ["ps4", "ps5"], vcx[0:rows, J, h, :], ["vcx"], 193, J == 0, J == ntile - 1)

                def post():
                    for half in range(2):
                        pc = PS[4 + half]
                        m.cp("act", a_[:, 0, half * 2:half * 2 + 2, :], pc[:, 0:386].rearrange("p (g c) -> p g c", g=2)[:, :, 0:65],
                             ["ps%d" % (4 + half)], [ak])
                    m.ts("dve", rv[:, 0:4], a_[:, 0, :, 64], 1e-30, None, ALU.max, None, [ak], [rvk])
                    m.op("dve", lambda e: e.reciprocal(out=rv[:, 0:4], in_=rv[:, 0:4]), [rvk], [rvk])
                    for g in range(4):
                        pc = PS[4 + g // 2]
                        u_ap = pc[:, (g % 2) * 193 + 65:(g % 2) * 193 + 193]
                        if g == 0:
                            m.stt(sc[:], u_ap, rv[:, 0:1], f0[:], ALU.mult, ALU.add, ["ps4", rvk, "f0"], ["sc"])
                        else:
                            m.stt(sc[:], u_ap, rv[:, g:g + 1], sc[:], ALU.mult, ALU.add, ["ps%d" % (4 + g // 2), rvk, "sc"], ["sc"])
                    ncol = 2 * t + 2
                    m.tt("dve", sc[:, 2 * t - 1:2 * t + 2], sc[:, 2 * t - 1:2 * t + 2], b3[:], ALU.add, ["sc", "b3"], ["sc"])
                    m.op("dve", lambda e: e.max(out=mx[:, 0:8], in_=sc[:, 0:ncol]), ["sc"], ["mx"])
                    m.op("dve", lambda e: e.match_replace(out=sc2[:, 0:ncol], in_to_replace=mx[:, 0:8], in_values=sc[:, 0:ncol], imm_value=-1e9),
                         ["sc", "mx"], ["sc2"])
                    m.op("dve", lambda e: e.max(out=mx[:, 8:16], in_=sc2[:, 0:ncol]), ["sc2"], ["mx"])
                    mn = mneg[lt % 2]; mk_ = "mneg%d" % (lt % 2)
                    m.ts("dve", sc2[:, 0:ncol], sc[:, 0:ncol], mx[:, 15:16], -NEG, ALU.is_ge, ALU.mult, ["sc", "mx"], ["sc2"])
                    m.ts("dve", mn[:, 64:64 + ncol], sc2[:, 0:ncol], NEG, None, ALU.add, None, ["sc2"], [mk_])

                    def post_b():
                        for c in range(2):
                            m.mm(PS[4 + c][:, :], mn[:, 64 * c:64 * c + 128], i4[:], True, True, [mk_, "i4"], ["ps%d" % (4 + c)])
                            m.cp("dve", QM[lt % 3][c][64:128, :], PS[4 + c][64:128, :], ["ps%d" % (4 + c)], [qmk])
                    delayed.append([10, post_b])
                add_post(post)

            def emit_win(lt, h=h):
                t = 32 + lt
                qmk = "QM%d" % (lt % 3)
                qap = QM[lt % 3][0][:]
                a_ = acc[lt % 2]; ak = "acc%d" % (lt % 2)
                for j in range(t - 4, t + 1):
                    extra = []
                    if j == t:
                        extra.append((ident_b[:], tabs[:, 0, h * 512:(h + 1) * 512], ["ident_b", "tabs"]))
                    elif j == t - 1:
                        extra.append((ident_b[:], tabs[:, 1, h * 512:(h + 1) * 512], ["ident_b", "tabs"]))
                    elif j == t - 4:
                        extra.append((ident_b[:], anti[:], ["ident_b", "anti"]))
                    jl = j - 28
                    add_unit(kwin[:, jl * 128:(jl + 1) * 128], ["kwin", "kwin_hi", qmk], 128, qap, extra, kbias[:, j:j + 1], ["kbias"],
                             WACC, ["pb0"], vwin[:, jl, :], ["vwin"], 65, j == t - 4, j == t)
                add_post(lambda: m.cp("act", a_[:, 2, :, :], WACC[:, 0:260].rearrange("p (g c) -> p g c", g=4), ["pb0"], [ak]))

            def emit_sel(lt, h=h):
                t = 32 + lt
                qmk = "QM%d" % (lt % 3)
                a_ = acc[lt % 2]; ak = "acc%d" % (lt % 2)
                rv = rinv[lt % 2]; rvk = "rinv%d" % (lt % 2)
                if lt == 0:
                    while pend:
                        flush_one()
                    run_delayed()
                for j in range(t + 1):
                    qap = QM[lt % 3][j // 32][:]
                    extra = []
                    if j == t:
                        extra.append((ident_b[:], tabs[:, 0, h * 512:(h + 1) * 512], ["ident_b", "tabs"]))
                    elif j == t - 1:
                        extra.append((ident_b[:], tabs[:, 1, h * 512:(h + 1) * 512], ["ident_b", "tabs"]))
                    add_unit(kslc[:, j * 128:(j + 1) * 128], ["kslc", "kslc_hi", qmk], 128, qap, extra, kbias[:, j:j + 1], ["kbias"],
                             PS[3], ["ps3"], vslc[:, j, :], ["vslc"], 65, j == 0, j == t)

                def post():
                    m.cp("act", a_[:, 1, :, :], PS[3][:, 0:260].rearrange("p (g c) -> p g c", g=4), ["ps3"], [ak])
                    m.ts("dve", rv[:, 4:12].rearrange("p (b g) -> p b g", b=2), a_[:, 1:3, :, 64], 1e-30, None, ALU.max, None, [ak], [rvk])
                    m.op("dve", lambda e: e.reciprocal(out=rv[:, 4:12], in_=rv[:, 4:12]), [rvk], [rvk])
                    m.tt("dve", rv[:].rearrange("p (b g) -> p b g", b=3), rv[:].rearrange("p (b g) -> p b g", b=3),
                         gts[:, lt, :].rearrange("p (g b) -> p b g", b=3), ALU.mult, [rvk, "gts"], [rvk])
                    o_ = ob[lt % 2]; ok = "ob%d" % (lt % 2)
                    for g in range(4):
                        m.ts("dve", of[:, g, :], a_[:, 0, g, 0:64], rv[:, g:g + 1], None, ALU.mult, None, [ak, rvk], ["of"])
                        m.stt(of[:, g, :], a_[:, 1, g, 0:64], rv[:, 4 + g:5 + g], of[:, g, :], ALU.mult, ALU.add, [ak, rvk, "of"], ["of"])
                        m.stt(o_[:, g * 64:(g + 1) * 64], a_[:, 2, g, 0:64], rv[:, 8 + g:9 + g], of[:, g, :], ALU.mult, ALU.add, [ak, rvk, "of"], [ok])
                    m.ld("sp", o_d[lt * 128:(lt + 1) * 128, h * 256:(h + 1) * 256], o_[:], [ok], [])
                add_post(post)

            emit_cmp(0)
            for lt in range(NT):
                emit_win(lt)
                if lt + 1 < NT:
                    emit_cmp(lt + 1)
                emit_sel(lt)
            while pend:
                flush_one()
            run_delayed()
        ph.close()

    if "B" in phases:
        phase_BC()

    def phase_D():
        ph = Phase()
        dg = ph.sb("dg", [128, 8, 31, 128], BF16)
        wc = ph.sb("wc", [128, 8, 31], F32)
        cv = ph.sb("cv", [128, 24], F32)
        hc = ph.sb("hc", [128, 8, 128 + OWN], BF16)
        yb = ph.sb("yb", [128, 8, 512], F32)
        ysq = ph.sb("ysq", [128, 512], F32)
        mean = ph.sb("mean", [128, 512], F32); rstd = ph.sb("rstd", [128, 512], F32)
        z = [ph.sb("z%d" % i, [128, 512], F32) for i in range(4)]
        zo = [ph.sb("zo%d" % i, [128, 512], BF16) for i in range(4)]
        m.ld("sp", wc[:].rearrange("p c k -> p (c k)"), dww, [], ["wc"])
        m.ld("sp", cv[:], cvec, [], ["cv"])
        for cc in range(8):
            m.ld("sp", hc[:, cc, :], hconvT_d[cc], [], ["hc"])
            for k in range(31):
                if k % 2:
                    m.ts("dve", dg[:, cc, k, :], ident_f[:], wc[:, cc, k:k + 1], None, ALU.mult, None, ["ident_f", "wc"], ["dg"])
                else:
                    m.act(dg[:, cc, k, :], ident_f[:], AF.Copy, ["ident_f", "wc"], ["dg"], scale=wc[:, cc, k:k + 1])
        for blk in range(8):
            for cc in range(8):
                ps, pk = PS[cc % 4], PSK[cc % 4]
                for k in range(31):
                    c0 = 128 + blk * 512 - 30 + k
                    m.mm(ps[:, :], dg[:, cc, k, :], hc[:, cc, c0:c0 + 512], k == 0, k == 30, ["dg", "hc"], [pk])
                m.act(yb[:, cc, :], ps[:, :], AF.Identity, [pk, "cv"], ["yb%d" % cc], bias=cv[:, cc:cc + 1])
                m.act(ysq[:], yb[:, cc, :], AF.Square, ["yb%d" % cc], ["ysq"])
                m.mm(PS[4][:, :], ones_f[:], yb[:, cc, :], cc == 0, cc == 7, ["ones_f", "yb%d" % cc], ["ps4"])
                m.mm(PS[5][:, :], ones_f[:], ysq[:], cc == 0, cc == 7, ["ones_f", "ysq"], ["ps5"])
            m.act(mean[:], PS[4][:, :], AF.Copy, ["ps4"], ["mean"], scale=1.0 / 1024)
            m.tt("dve", rstd[:], mean[:], mean[:], ALU.mult, ["mean"], ["rstd"])
            m.stt(rstd[:], PS[5][:, :], 1.0 / 1024, rstd[:], ALU.mult, ALU.subtract, ["ps5", "rstd"], ["rstd"])
            m.act(rstd[:], rstd[:], AF.Sqrt, ["rstd"], ["rstd"], bias=EPS)
            m.op("dve", lambda e: e.reciprocal(out=rstd[:], in_=rstd[:]), ["rstd"], ["rstd"])
            for cc in range(8):
                z_ = z[cc % 4]; zk = "z%d" % (cc % 4)
                zo_ = zo[cc % 4]; zok = "zo%d" % (cc % 4)
                m.tt("dve", z_[:], yb[:, cc, :], mean[:], ALU.subtract, ["yb%d" % cc, "mean"], [zk])
                m.tt("pool", z_[:], z_[:], rstd[:], ALU.mult, [zk, "rstd"], [zk])
                m.act(zo_[:], z_[:], AF.Silu, [zk, "cv"], [zok], bias=cv[:, 16 + cc:17 + cc], scale=cv[:, 8 + cc:9 + cc])
                m.ld("sp", oconvT_d[cc, :, blk * 512:(blk + 1) * 512], zo_[:], [zok], [])
        ph.close()

    if "D" in phases:
        phase_D()

    def load_w_bf(ph, name, src, ncols, q="pool"):
        wt = ph.sb(name, [128, KC, ncols], BF16)
        for k4 in range(0, KC, 4):
            m.ld(q, wt[:, k4:k4 + 4, :], src[k4 * 128:(k4 + 4) * 128, :].rearrange("(k p) n -> p k n", p=128), [], [name])
        return wt

    def phase_E1():
        ph = Phase()
        tm = ln_tmps(ph)
        wo_ = load_w_bf(ph, "w_o", w_out, D)
        lnrep2 = ph.sb("lnrep2", [128, 2, D], F32)
        m.ld("sp", lnrep2[:, 0, :], lnv[2:3, :].broadcast_to([128, D]), [], ["lnrep2"])
        m.ld("sp", lnrep2[:, 1, :], lnv[3:4, :].broadcast_to([128, D]), [], ["lnrep2"])
        load_ln(0)
        ot = [ph.sb("ot%d" % i, [128, 1024], BF16) for i in range(2)]
        mixT = [ph.sb("mixT%d" % i, [128, KC, 128], BF16) for i in range(2)]
        xt = [ph.sb("e1x%d" % i, [128, D], F32) for i in range(2)]
        y = [ph.sb("e1y%d" % i, [128, D], F32) for i in range(2)]
        yb_ = [ph.sb("e1yb%d" % i, [128, D], BF16) for i in range(2)]
        xT = [ph.sb("e1xT%d" % i, [128, KC, 128], BF16) for i in range(2)]
        def e1_part2(lt):
            i2 = lt % 2
            transpose_to(xT[i2], "e1xT%d" % i2, yb_[i2], ["e1yb%d" % i2], 0)
            m.ld("sp", x1T_d.rearrange("c p t -> p c t")[:, :, lt * 128:(lt + 1) * 128], xT[i2][:], ["e1xT%d" % i2], [])

        def e1_stage0(lt):
            i2 = lt % 2
            m.ld("sp", ot[i2][:], o_d[lt * 128:(lt + 1) * 128, :], [], ["ot%d" % i2])
            transpose_to(mixT[i2], "mixT%d" % i2, ot[i2], ["ot%d" % i2], 0, nk=8)
            m.ld("sp", mixT[i2][:, 8:16, :], oconvT_d.rearrange("c p t -> p c t")[:, :, lt * 128:(lt + 1) * 128], [], ["mixT%d" % i2])
            m.ld("sp", xt[i2][:], xv[OWN + lt * 128:OWN + (lt + 1) * 128, :], [], ["e1x%d" % i2])
            layer_norm(tm, xt[i2][:], ["e1x%d" % i2], xt[i2][:], ["e1x%d" % i2])

        e1_stage0(0)
        for lt in range(NT):
            i2 = lt % 2
            if lt + 1 < NT:
                e1_stage0(lt + 1)
            for oc in range(4):
                ps, pk = PS[oc], PSK[oc]
                for k in range(KC):
                    m.mm(ps[:, :], mixT[i2][:, k, :], wo_[:, k, oc * 512:(oc + 1) * 512], k == 0, k == KC - 1, ["mixT%d" % i2, "w_o"], [pk])
                m.stt(y[i2][:, oc * 512:(oc + 1) * 512], xt[i2][:, oc * 512:(oc + 1) * 512], ALPHA, ps[:, :], ALU.mult, ALU.add,
                      ["e1x%d" % i2, pk], ["e1y%d" % i2])
            ln_generic(tm, y[i2][:], ["e1y%d" % i2], lnrep2, "lnrep2")
            m.ld("sp", x1_d[lt * 128:(lt + 1) * 128, :], y[i2][:], ["e1y%d" % i2], [])
            m.cp("pool", yb_[i2][:], y[i2][:], ["e1y%d" % i2], ["e1yb%d" % i2])
            if lt > 0:
                e1_part2(lt - 1)
        e1_part2(NT - 1)
        ph.close()

    def ln_generic(tm, src, src_keys, rep, repk, np_=128, last_eng="pool"):
        st, mv, rs, nb = tm
        for c in range(4):
            m.op("dve", lambda e, c=c: e.bn_stats(out=st[:np_, c, :], in_=src[:, c * 512:(c + 1) * 512]), src_keys, ["ln_st"])
        m.op("dve", lambda e: e.bn_aggr(out=mv[:np_, :], in_=st[:np_, :, :]), ["ln_st"], ["ln_mv"])
        m.act(rs[:np_, :], mv[:np_, 1:2], AF.Sqrt, ["ln_mv"], ["ln_rs"], bias=EPS)
        m.op("dve", lambda e: e.reciprocal(out=rs[:np_, :], in_=rs[:np_, :]), ["ln_rs"], ["ln_rs"])
        m.stt(nb[:np_, :], mv[:np_, 0:1], -1.0, rs[:np_, :], ALU.mult, ALU.mult, ["ln_mv", "ln_rs"], ["ln_nb"])
        m.act(src, src, AF.Identity, src_keys + ["ln_rs", "ln_nb"], src_keys, bias=nb[:np_, :], scale=rs[:np_, :])
        m.tt("dve", src, src, rep[:np_, 0, :], ALU.mult, src_keys + [repk], src_keys)
        m.tt(last_eng, src, src, rep[:np_, 1, :], ALU.add, src_keys + [repk], src_keys)

    if "E1" in phases:
        phase_E1()

    def phase_E2():
        ph = Phase()
        tm = ln_tmps(ph)
        KmT = ph.sb("KmT", [128, KC, 256], BF16)
        Vm = ph.sb("Vm", [128, 2, D], BF16)
        load_ln(4)
        pm = Phase()
        mt_ = pm.sb("mt_", [128, D], F32); mb_ = pm.sb("mb_", [128, D], BF16)
        mT = pm.sb("mT", [128, KC, 256], BF16)
        wkv = [pm.sb("wkv%d" % i, [128, KC, 512], BF16) for i in range(2)]
        wkvf = pm.sb("wkvf", [128, KC, 512], F32)
        for ti in range(2):
            m.ld("sp", mt_[:], memb[ti * 128:(ti + 1) * 128, :], [], ["mt_"])
            layer_norm(tm, mt_[:], ["mt_"], mb_[:], ["mb_"])
            transpose_to(mT, "mT", mb_, ["mb_"], ti)
        for cb in range(8):
            w_ = wkv[cb % 2]; wk_ = "wkv%d" % (cb % 2)
            if cb % 2 == 0:
                for k4 in range(0, KC, 4):
                    m.ld("pool", w_[:, k4:k4 + 4, :],
                         xa_wkv[k4 * 128:(k4 + 4) * 128, cb * 512:(cb + 1) * 512].rearrange("(k p) n -> p k n", p=128), [], [wk_])
            else:
                for k4 in range(0, KC, 4):
                    m.ld("sp", wkvf[:, k4:k4 + 4, :],
                         xa_wkv[k4 * 128:(k4 + 4) * 128, cb * 512:(cb + 1) * 512].rearrange("(k p) n -> p k n", p=128), [], ["wkvf%d" % k4])
                    m.cp("dve" if (k4 // 4) % 2 else "act", w_[:, k4:k4 + 4, :], wkvf[:, k4:k4 + 4, :], ["wkvf%d" % k4], [wk_])
            if cb < 4:
                for sub in range(4):
                    ps, pk = PS[sub], PSK[sub]
                    for k in range(KC):
                        m.mm(ps[:, 0:256], w_[:, k, sub * 128:(sub + 1) * 128], mT[:, k, :], k == 0, k == KC - 1, [wk_, "mT"], [pk])
                    m.cp("dve" if sub % 2 else "act", KmT[:, cb * 4 + sub, :], ps[:, 0:256], [pk], ["KmT"])
            else:
                for ti in range(2):
                    ps, pk = PS[ti], PSK[ti]
                    for k in range(KC):
                        m.mm(ps[:, :], mT[:, k, ti * 128:(ti + 1) * 128], w_[:, k, :], k == 0, k == KC - 1, [wk_, "mT"], [pk])
                    m.cp("dve" if ti else "act", Vm[:, ti, (cb - 4) * 512:(cb - 3) * 512], ps[:, :], [pk], ["Vm"])
        pm.close()
        wq_ = load_w_bf(ph, "xwq", xa_wq, D)
        xT = [ph.sb("e2xT%d" % i, [128, KC, 512], BF16) for i in range(2)]
        qx = ph.sb("qx", [128, KC, 512], BF16)
        pT = [ph.sb("e2pT%d" % i, [128, 2, 512], BF16) for i in range(2)]
        rinv = ph.sb("e2rinv", [128, 512], F32)
        ox = [ph.sb("e2ox0", [128, KC, 512], BF16)] * 2
        sc_ = 512 ** -0.5
        for gi in range(8):
            i2 = gi % 2
            xk = "e2xT%d" % i2
            m.ld("sp", xT[i2][:], x1T_d.rearrange("c p t -> p c t")[:, :, gi * 512:(gi + 1) * 512], [], [xk])
            for oc in range(KC):
                ps, pk = PS[oc % 4], PSK[oc % 4]
                for k in range(KC):
                    m.mm(ps[:, :], wq_[:, k, oc * 128:(oc + 1) * 128], xT[i2][:, k, :], k == 0, k == KC - 1, ["xwq", xk], [pk])
                m.cp("dve" if oc % 2 else "act", qx[:, oc, :], ps[:, :], [pk], ["qx%d" % oc])
            for hd in range(4):
                p_ = pT[hd % 2]; ppk = "e2pT%d" % (hd % 2)
                for mt in range(2):
                    ps, pk = PS[mt], PSK[mt]
                    for kk in range(4):
                        m.mm(ps[:, :], KmT[:, hd * 4 + kk, mt * 128:(mt + 1) * 128], qx[:, hd * 4 + kk, :], kk == 0, kk == 3,
                             ["KmT", "qx%d" % (hd * 4 + kk)], [pk])
                    m.act(p_[:, mt, :], ps[:, :], AF.Exp, [pk], [ppk], scale=sc_)
                for mt in range(2):
                    m.mm(PS[2][:, :], ones_b[:], p_[:, mt, :], mt == 0, mt == 1, ["ones_b", ppk], ["ps2"])
                m.op("dve", lambda e: e.reciprocal(out=rinv[:], in_=PS[2][:, :]), ["ps2"], ["e2rinv"])
                for dc in range(4):
                    ps, pk = PS[3 + dc % 3], PSK[3 + dc % 3]
                    for mt in range(2):
                        m.mm(ps[:, :], Vm[:, mt, (hd * 4 + dc) * 128:(hd * 4 + dc + 1) * 128], p_[:, mt, :], mt == 0, mt == 1, ["Vm", ppk], [pk])
                    m.tt("dve", ox[i2][:, hd * 4 + dc, :], ps[:, :], rinv[:], ALU.mult, [pk, "e2rinv"], ["e2ox0"])
            m.ld("pool", oxT_d.rearrange("c p t -> p c t")[:, :, gi * 512:(gi + 1) * 512], ox[i2][:], ["e2ox0"], [])
        ph.close()

    if "E2" in phases:
        phase_E2()

    def phase_E3():
        ph = Phase()
        tm = ln_tmps(ph)
        wo_ = load_w_bf(ph, "xwo", xa_wo, D)
        load_ln(6)
        rws = ph.sb("rws", [128, KC, 36], F32)
        rbs = ph.sb("rbs", [128, 36], F32)
        ut = ph.sb("ut", [128, 128], BF16)
        ecs = ph.sb("ecs", [128, 32], F32)
        carry = ph.sb("carry", [128, 32], F32)
        m.ld("sp", rws[:], rw.rearrange("(c p) j -> p c j", p=128), [], ["rws"])
        m.ld("sp", rbs[:], rb.broadcast_to([128, 36]), [], ["rbs"])
        m.ld("pool", ut[:], ut_c, [], ["ut"])
        m.ld("sp", ecs[:], ec_c, [], ["ecs"])
        m.op("dve", lambda e: e.memset(carry[:], 0.0), [], ["carry"])
        oT = [ph.sb("e3oT%d" % i, [128, KC, 128], BF16) for i in range(2)]
        x1 = [ph.sb("e3x1%d" % i, [128, D], F32) for i in range(2)]
        y = [ph.sb("e3y%d" % i, [128, D], F32) for i in range(2)]
        yb_ = [ph.sb("e3yb%d" % i, [128, D], BF16) for i in range(2)]
        yT = ph.sb("e3yT", [128, KC, 128], F32)
        lg = ph.sb("lg", [128, 36], F32)
        sm = ph.sb("sm", [128, 16], F32)
        oh = ph.sb("oh", [128, 4, 32], F32)
        ohb = ph.sb("ohb", [128, 32], BF16)
        lem = ph.sb("lem", [128, 32], F32)
        mx8 = ph.sb("mx8", [128, 8], F32)
        rk = ph.sb("rk", [128, 32], F32)
        rto = [ph.sb("rto%d" % i, [128, 4], F32) for i in range(2)]
        dsti = [ph.sb("dsti%d" % i, [128, 2], I32) for i in range(2)]
        def e3_part2a(lt):
            i2 = lt % 2
            yk = "e3y%d" % i2
            for k0 in range(0, KC, 4):
                ps, pk = PS[4 + (k0 // 4) % 2], PSK[4 + (k0 // 4) % 2]
                for kk in range(4):
                    m.tr(ps[:, kk * 128:(kk + 1) * 128], y[i2][:, (k0 + kk) * 128:(k0 + kk + 1) * 128], ident_f[:], [yk, "ident_f"], [pk])
                m.cp("act", yT[:, k0:k0 + 4, :], ps[:, :].rearrange("p (k t) -> p k t", k=4), [pk], ["e3yT"])

        def e3_part2(lt):
            i2 = lt % 2
            yk = "e3y%d" % i2
            for k in range(KC):
                m.mm(PS[4][:, 0:36], yT[:, k, :], rws[:, k, :], k == 0, k == KC - 1, ["e3yT", "rws"], ["ps4"])
            m.tt("dve", lg[:], PS[4][:, 0:36], rbs[:], ALU.add, ["ps4", "rbs"], ["lg"])
            m.op("dve", lambda e: e.tensor_reduce(out=sm[:, 0:1], in_=lg[:, 0:4], axis=AX.X, op=ALU.max), ["lg"], ["sm"])
            m.ts("dve", sm[:, 1:2], sm[:, 0:1], -1.0, None, ALU.mult, None, ["sm"], ["sm"])
            m.act(sm[:, 4:8], lg[:, 0:4], AF.Exp, ["lg", "sm"], ["sm"], bias=sm[:, 1:2], accum=sm[:, 2:3])
            m.op("dve", lambda e: e.reciprocal(out=sm[:, 3:4], in_=sm[:, 2:3]), ["sm"], ["sm"])
            m.ts("dve", sm[:, 8:12], lg[:, 0:4], sm[:, 0:1], None, ALU.is_ge, None, ["lg", "sm"], ["sm"])
            m.ts("dve", oh[:, 0, :].rearrange("p (g e) -> p g e", g=4), sm[:, 8:12].unsqueeze(2).broadcast_to([128, 4, 8]), 1.0, 1e4,
                 ALU.subtract, ALU.mult, ["sm"], ["oh"])
            m.tt("dve", lem[:], lg[:, 4:36], oh[:, 0, :], ALU.add, ["lg", "oh"], ["lem"])
            m.op("dve", lambda e: e.max(out=mx8[:], in_=lem[:]), ["lem"], ["mx8"])
            m.ts("dve", oh[:, 1, :], lem[:], mx8[:, 0:1], None, ALU.is_ge, None, ["lem", "mx8"], ["oh"])
            m.ts("dve", oh[:, 3, :], lem[:], mx8[:, 1:2], None, ALU.is_ge, None, ["lem", "mx8"], ["oh"])
            m.tt("dve", oh[:, 2, :], oh[:, 3, :], oh[:, 1, :], ALU.subtract, ["oh"], ["oh"])
            m.cp("dve", ohb[:], oh[:, 3, :], ["oh"], ["ohb"])
            m.tt("dve", sm[:, 12:13], mx8[:, 1:2], mx8[:, 0:1], ALU.subtract, ["mx8"], ["sm"])
            m.act(sm[:, 13:14], sm[:, 12:13], AF.Exp, ["sm"], ["sm"])
            m.ts("dve", sm[:, 14:15], sm[:, 13:14], 1.0, None, ALU.add, None, ["sm"], ["sm"])
            m.op("dve", lambda e: e.reciprocal(out=sm[:, 14:15], in_=sm[:, 14:15]), ["sm"], ["sm"])
            r_ = rto[i2]; rk_ = "rto%d" % i2
            m.tt("dve", r_[:, 2:3], sm[:, 14:15], sm[:, 3:4], ALU.mult, ["sm"], [rk_])
            m.tt("dve", r_[:, 3:4], sm[:, 13:14], r_[:, 2:3], ALU.mult, ["sm", rk_], [rk_])
            m.mm(PS[5][:, 0:32], ut[:], ohb[:], True, True, ["ut", "ohb"], ["ps5"])
            m.tt("dve", rk[:], PS[5][:, 0:32], carry[:], ALU.add, ["ps5", "carry"], ["rk"])
            m.tt("dve", rk[:], rk[:], ecs[:], ALU.add, ["rk", "ecs"], ["rk"])
            m.mm(PS[5][:, 32:64], ones_b[:], ohb[:], True, True, ["ones_b", "ohb"], ["ps5"])
            m.tt("dve", carry[:], carry[:], PS[5][:, 32:64], ALU.add, ["ps5", "carry"], ["carry"])
            for kq in range(2):
                m.tt("dve", oh[:, 0, :], oh[:, 1 + kq, :], rk[:], ALU.mult, ["oh", "rk"], ["oh"])
                m.op("dve", lambda e, kq=kq, r_=r_: e.tensor_reduce(out=r_[:, kq:kq + 1], in_=oh[:, 0, :], axis=AX.X, op=ALU.add), ["oh"], [rk_])
            d_ = dsti[i2]; dk = "dsti%d" % i2
            m.cp("dve", d_[:], r_[:, 0:2], [rk_], [dk])
            m.ld("sp", rt_d[lt * 128:(lt + 1) * 128, :], r_[:], [rk_], [])
            for kq in range(2):
                m.dma("pool", lambda e, kq=kq, d_=d_, yy=yb_[i2]: e.indirect_dma_start(
                    out=xs_d, out_offset=bass.IndirectOffsetOnAxis(ap=d_[:, kq:kq + 1], axis=0), in_=yy[:], in_offset=None),
                    ["e3yb%d" % i2, dk], ["xs_d"])

        for lt in range(NT):
            i2 = lt % 2
            m.ld("sp", oT[i2][:], oxT_d.rearrange("c p t -> p c t")[:, :, lt * 128:(lt + 1) * 128], [], ["e3oT%d" % i2])
            m.ld("sp", x1[i2][:], x1_d[lt * 128:(lt + 1) * 128, :], [], ["e3x1%d" % i2])
            if lt > 0:
                e3_part2a(lt - 1)
            for oc in range(4):
                ps, pk = PS[oc], PSK[oc]
                for k in range(KC):
                    m.mm(ps[:, :], oT[i2][:, k, :], wo_[:, k, oc * 512:(oc + 1) * 512], k == 0, k == KC - 1, ["e3oT%d" % i2, "xwo"], [pk])
                m.stt(y[i2][:, oc * 512:(oc + 1) * 512], x1[i2][:, oc * 512:(oc + 1) * 512], ALPHA, ps[:, :], ALU.mult, ALU.add,
                      ["e3x1%d" % i2, pk], ["e3y%d" % i2])
            yk = "e3y%d" % i2
            ln_generic(tm, y[i2][:], [yk], lnrep, "lnrep", last_eng="dve")
            m.ld("sp", x2_d[lt * 128:(lt + 1) * 128, :], y[i2][:], [yk], [])
            m.cp("dve", yb_[i2][:], y[i2][:], [yk], ["e3yb%d" % i2])
            if lt > 0:
                e3_part2(lt - 1)
        e3_part2a(NT - 1)
        e3_part2(NT - 1)
        ph.close()

    if "E3" in phases:
        phase_E3()

    def phase_F():
        ph = Phase()
        wg_ = [ph.sb("fwg%d" % i, [128, KC, 512], BF16) for i in range(2)]
        wu_ = [ph.sb("fwu%d" % i, [128, KC, 512], BF16) for i in range(2)]
        wd_ = [ph.sb("fwd%d" % i, [128, 4, D], BF16) for i in range(2)]
        xr = [ph.sb("fxr%d" % i, [128, D], BF16) for i in range(2)]
        xsT = [ph.sb("fxsT%d" % i, [128, KC, CAP], BF16) for i in range(2)]
        hT = ph.sb("fhT", [128, 4, CAP], BF16)
        sg = [ph.sb("fsg%d" % i, [128, CAP], F32) for i in range(2)]
        yo = [ph.sb("fyo%d" % i, [128, D], F32) for i in range(2)]
        wdf = ph.sb("fwdf", [128, 2, D], F32)
        cnt = {"i": 0}

        def f_loadw(e_):
            i2 = e_ % 2
            for k8 in range(0, KC, 8):
                m.ld("pool", wg_[i2][:, k8:k8 + 8, :], mwg[e_, k8 * 128:(k8 + 8) * 128, :].rearrange("(k p) n -> p k n", p=128), [], ["fwg%d" % i2])
            for k8 in range(0, KC, 8):
                m.ld("pool", wu_[i2][:, k8:k8 + 8, :], mwu[e_, k8 * 128:(k8 + 8) * 128, :].rearrange("(k p) n -> p k n", p=128), [], ["fwu%d" % i2])
            for k2 in range(0, 4, 2):
                for kk in range(2):
                    m.ld("sp", wdf[:, kk, :], mwd[e_, (k2 + kk) * 128:(k2 + kk + 1) * 128, :], [], ["fwdf%d" % kk])
                    m.cp("pool", wd_[i2][:, k2 + kk, :], wdf[:, kk, :], ["fwdf%d" % kk], ["fwd%d" % i2])

        def f_xsT(e_):
            i2 = e_ % 2
            for rt in range(4):
                x_ = xr[rt % 2]; xk = "fxr%d" % (rt % 2)
                m.ld("sp", x_[:], xs_d[e_ * CAP + rt * 128:e_ * CAP + (rt + 1) * 128, :], ["xs_d"], [xk])
                transpose_to(xsT[i2], "fxsT%d" % i2, x_, [xk], rt, only="dve")

        f_loadw(0)
        f_xsT(0)
        for e_ in range(NE):
            i2 = e_ % 2
            if e_ + 1 < NE:
                f_loadw(e_ + 1)
            for hc in range(4):
                pg, pgk = PS[(hc % 2) * 2], PSK[(hc % 2) * 2]
                pu, puk = PS[(hc % 2) * 2 + 1], PSK[(hc % 2) * 2 + 1]
                for k in range(KC):
                    m.mm(pg[:, :], wg_[i2][:, k, hc * 128:(hc + 1) * 128], xsT[i2][:, k, :], k == 0, k == KC - 1, ["fwg%d" % i2, "fxsT%d" % i2], [pgk])
                for k in range(KC):
                    m.mm(pu[:, :], wu_[i2][:, k, hc * 128:(hc + 1) * 128], xsT[i2][:, k, :], k == 0, k == KC - 1, ["fwu%d" % i2, "fxsT%d" % i2], [puk])
                s_ = sg[hc % 2]; sk = "fsg%d" % (hc % 2)
                m.act(s_[:], pg[:, :], AF.Silu, [pgk], [sk])
                m.tt("dve", hT[:, hc, :], pu[:, :], s_[:], ALU.mult, [puk, sk], ["fhT%d" % hc])
            if e_ + 1 < NE:
                f_xsT(e_ + 1)
            for rt in range(4):
                y_ = yo[rt % 2]; yk = "fyo%d" % (rt % 2)
                for oc in range(4):
                    pi = 4 + cnt["i"] % 2; cnt["i"] += 1
                    ps, pk = PS[pi], PSK[pi]
                    for hc in range(4):
                        m.mm(ps[:, :], hT[:, hc, rt * 128:(rt + 1) * 128], wd_[i2][:, hc, oc * 512:(oc + 1) * 512], hc == 0, hc == 3,
                             ["fhT%d" % hc, "fwd%d" % i2], [pk])
                    m.cp("dve", y_[:, oc * 512:(oc + 1) * 512], ps[:, :], [pk], [yk])
                r0 = e_ * CAP + rt * 128
                m.ld("act", ys_d[r0:r0 + 128, :], y_[:], [yk], ["ys_d"])
        ph.close()

    if "F" in phases:
        phase_F()

    def phase_G():
        ph = Phase()
        tm = ln_tmps(ph)
        load_ln(8)
        NB = 4
        x2 = [ph.sb("gx%d" % i, [128, D], F32) for i in range(NB)]
        y1 = [ph.sb("gy1%d" % i, [128, D], F32) for i in range(NB)]
        y2 = [ph.sb("gy2%d" % i, [128, D], F32) for i in range(NB)]
        rto = [ph.sb("grt%d" % i, [128, 4], F32) for i in range(NB)]
        dsti = [ph.sb("gds%d" % i, [128, 2], I32) for i in range(NB)]

        def issue(lt):
            i2 = lt % NB
            m.ld("sp", x2[i2][:], x2_d[lt * 128:(lt + 1) * 128, :], [], ["gx%d" % i2])
            m.ld("sp", rto[i2][:], rt_d[lt * 128:(lt + 1) * 128, :], [], ["grt%d" % i2])
            m.cp("dve", dsti[i2][:], rto[i2][:, 0:2], ["grt%d" % i2], ["gds%d" % i2])
            for kq, yy in enumerate((y1[i2], y2[i2])):
                yk = "gy%d%d" % (kq + 1, i2)
                m.dma("pool", lambda e, kq=kq, yy=yy, d_=dsti[i2]: e.indirect_dma_start(
                    out=yy[:], out_offset=None, in_=ys_d, in_offset=bass.IndirectOffsetOnAxis(ap=d_[:, kq:kq + 1], axis=0)),
                    ["ys_d", "gds%d" % i2], [yk])

        issue(0)
        issue(1)
        for lt in range(NT):
            i2 = lt % NB
            if lt + 2 < NT:
                issue(lt + 2)
            xk = "gx%d" % i2
            m.act(x2[i2][:], x2[i2][:], AF.Copy, [xk], [xk], scale=ALPHA)
            m.stt(x2[i2][:], y1[i2][:], rto[i2][:, 2:3], x2[i2][:], ALU.mult, ALU.add, ["gy1%d" % i2, "grt%d" % i2, xk], [xk])
            m.stt(x2[i2][:], y2[i2][:], rto[i2][:, 3:4], x2[i2][:], ALU.mult, ALU.add, ["gy2%d" % i2, "grt%d" % i2, xk], [xk])
            ln_generic(tm, x2[i2][:], [xk], lnrep, "lnrep", last_eng="dve")
            m.ld("sp", out[lt * 128:(lt + 1) * 128, :], x2[i2][:], [xk], [])
        ph.close()

    if "G" in phases:
        phase_G()

    m.finish()
    return nc, m


def _t5_bucket(dist):
    n = np.maximum(dist, 0)
    nf = np.maximum(n, 1).astype(np.float32)
    large = 16 + (np.log(nf / np.float32(16)) / np.float32(math.log(8.0)) * np.float32(16)).astype(np.int32)
    large = np.minimum(large, 31)
    return np.where(n < 16, n, large).astype(np.int64)


def _prep(inputs):
    f = lambda a: np.ascontiguousarray(np.asarray(a, dtype=np.float32))
    x = f(inputs["x"]); mem = f(inputs["mem"])
    w_in = f(inputs["w_in"])[0]
    sizes = [1024] + [256] * 6 + [48, 2048]
    cuts = np.cumsum([0] + sizes)
    sec = [w_in[:, cuts[i]:cuts[i + 1]] for i in range(9)]
    q, k_c, v_c, k_s, v_s, k_w, v_w, g, u = sec
    w_inp = np.ascontiguousarray(np.concatenate([k_c, v_c, k_s, k_w, v_s, v_w, q, g, u], axis=1))
    lnv = np.stack([f(inputs["ln_in_g"]), f(inputs["ln_in_b"]), f(inputs["ln1_g"])[0], f(inputs["ln1_b"])[0],
                    f(inputs["mem_ln_g"])[0], f(inputs["mem_ln_b"])[0], f(inputs["ln2_g"])[0], f(inputs["ln2_b"])[0],
                    f(inputs["ln3_g"])[0], f(inputs["ln3_b"])[0]])

    def w1l(w):
        return np.ascontiguousarray(f(w)[0].reshape(32, 64, 256).transpose(1, 0, 2).reshape(64, 32 * 256))

    rel = f(inputs["rel_table"])
    kk = np.arange(128)[:, None]; qq = np.arange(128)[None, :]
    d_diag = qq - kk
    diag_raw = rel[_t5_bucket(d_diag)]
    diag_raw = np.ascontiguousarray(diag_raw.transpose(0, 2, 1).reshape(128, 16 * 128))
    diag_mask = np.where(d_diag >= 0, 0.0, NEG).astype(np.float32)
    sub1_raw = np.ascontiguousarray(rel[_t5_bucket(128 + qq - kk)].transpose(0, 2, 1).reshape(128, 16 * 128))
    ii = np.arange(16)[:, None]
    d_band = qq - 16 * ii + 113
    band_raw = np.ascontiguousarray(rel[_t5_bucket(d_band)].transpose(0, 2, 1).reshape(16, 16 * 128))
    band_mask = np.where(d_band >= 0, 0.0, NEG).astype(np.float32)
    anti_mask = np.where(kk > qq, 0.0, NEG).astype(np.float32)
    r31 = np.ascontiguousarray(np.broadcast_to(rel[31][None, :], (128, 16)))
    dw_c = np.zeros((16, 272), np.float32)
    dw_c[np.arange(16), 128 + np.arange(16)] = 1.0
    ident = np.eye(128, dtype=np.float32)
    ut = np.triu(np.ones((128, 128), np.float32), 1)
    cs = np.arange(512) * 16; ce = cs + 32
    ss = np.arange(128) * 64; se = ss + 64
    ov = (np.clip(np.minimum(ce[:, None], se[None, :]) - np.maximum(cs[:, None], ss[None, :]), 0, None) / 32.0).astype(np.float32)
    ov[511] = 0.0
    ec = np.ascontiguousarray(np.broadcast_to((np.arange(32) * CAP).astype(np.float32)[None, :], (128, 32)))
    asel = np.zeros((64, SV), np.float32)
    kcol = np.arange(SV)
    asel[(2 * (kcol // 128) + (kcol % 128) // 64) % 64, kcol] = 1.0
    band3 = np.zeros((128, 3), np.float32)
    band3[:64, 0] = 100.0; band3[:, 1] = 100.0; band3[64:, 2] = 100.0; band3[:64, 2] = -100.0
    dww = np.ascontiguousarray(f(inputs["conv_dw_w"])[0][:, 0, :].reshape(31, 8, 128).transpose(2, 1, 0).reshape(128, 8 * 31))
    cvec = np.concatenate([f(inputs["conv_dw_b"])[0].reshape(8, 128).T, f(inputs["conv_ln_g"])[0].reshape(8, 128).T,
                           f(inputs["conv_ln_b"])[0].reshape(8, 128).T], axis=1)
    cvec = np.ascontiguousarray(cvec)
    rwm = np.ascontiguousarray(np.concatenate([f(inputs["router_group_w"])[0], f(inputs["router_expert_w"])[0]], axis=1))
    rbm = np.concatenate([f(inputs["router_group_b"])[0], f(inputs["router_expert_b"])[0]])[None, :]
    common = dict(
        lnv=np.ascontiguousarray(lnv), w_inp=w_inp,
        w1k=w1l(inputs["cmp_w1_k"]), w1v=w1l(inputs["cmp_w1_v"]),
        pek=np.ascontiguousarray(f(inputs["cmp_pe_k"])[0].T), pev=np.ascontiguousarray(f(inputs["cmp_pe_v"])[0].T),
        w2k=f(inputs["cmp_w2_k"])[0], w2v=f(inputs["cmp_w2_v"])[0],
        diag_raw=diag_raw, sub1_raw=sub1_raw, band_raw=band_raw, r31=r31,
        diag_mask=diag_mask, anti_mask=anti_mask, band_mask=band_mask,
        dw_c=dw_c, asel_c=asel, ident_c=ident, ut_c=ut, ov_c=ov, ec_c=ec, band3_c=band3,
        dww=dww, cvec=cvec,
        w_out=f(inputs["w_out"])[0], xa_wq=f(inputs["xa_wq"])[0], xa_wkv=f(inputs["xa_wkv"])[0], xa_wo=f(inputs["xa_wo"])[0],
        rw=rwm, rb=np.ascontiguousarray(rbm),
        mwg=f(inputs["moe_w_gate"])[0], mwu=f(inputs["moe_w_up"])[0], mwd=f(inputs["moe_w_down"])[0],
    )
    in_maps = []
    for c in range(8):
        b, s = c // 2, c % 2
        if s == 1:
            xvv = x[b]
        else:
            xvv = np.ascontiguousarray(np.concatenate([x[b, OWN:], x[b, :OWN]], axis=0))
        keyb = np.zeros((128, 64), np.float32)
        cmpb = np.zeros((128, 4), np.float32)
        f0 = np.zeros((128, 128), np.float32)
        if s == 0:
            keyb[:, :32] = NEG
            cmpb[:, :2] = NEG
            f0[:, 64] = 100.0
        else:
            f0[:, 0] = 100.0
        cmpb[127, 3] = NEG
        halo = np.full((128, 1), float(s), np.float32)
        d = dict(common)
        d.update(xv=xvv, memb=mem[b], keybias_c=keyb, cmpbias_c=cmpb, f0_c=f0, halo_c=halo)
        in_maps.append(d)
    return in_maps


_CACHE = {}


def kernel(**inputs):
    in_maps = _prep(inputs)
    if "nc" not in _CACHE:
        _CACHE["nc"] = build()[0]
    res = run_bass_kernel_spmd(_CACHE["nc"], in_maps, core_ids=list(range(8)))
    outp = np.zeros((4, SV, D), np.float32)
    for c in range(8):
        b, s = c // 2, c % 2
        outp[b, s * OWN:(s + 1) * OWN] = res.results[c]["out"]
    return outp
```

```python
import os
import math
from contextlib import ExitStack
import numpy as np
import concourse.bass as bass
import concourse.mybir as mybir
from concourse.bass_utils import run_bass_kernel_spmd

F32 = mybir.dt.float32
BF16 = mybir.dt.bfloat16
I32 = mybir.dt.int32
AF = mybir.ActivationFunctionType
ALU = mybir.AluOpType
AX = mybir.AxisListType

SEM_LIMIT = 30000
N_DMA_SLOTS = 12

D = 2048
KC = 16
SV = 8192
OWN = 4096
NT = 32
ALPHA = 2 ** 0.25
EPS = 1e-5
NEG = -30000.0
CAP = 512
NE = 32
O_KF, O_VT, O_Q, O_G, O_U = 0, 1024, 1536, 2560, 2608
PH_ALL = "A1,A2,B,C,D,E1,M,E2,E3,F,G"


class MK:
    ENG = ("pe", "act", "dve", "pool", "sp")

    def __init__(self, nc):
        self.nc = nc
        self.streams = {e: [] for e in self.ENG}
        self.cur_sem = {}
        self.cur_cnt = {}
        for e in ("pe", "act", "dve", "pool"):
            self.cur_sem[e] = nc.alloc_semaphore("s_" + e + "0")
            self.cur_cnt[e] = 0
        self.nsem = 4
        self.slots = {}
        self.slot_rr = {}
        for q in ("sp", "pool", "act"):
            self.slots[q] = [[nc.alloc_semaphore("d_%s%d" % (q, i)), 0] for i in range(N_DMA_SLOTS)]
            self.slot_rr[q] = 0
        self.seen = {e: {} for e in self.ENG}
        self.state = {}
        self.all_events = {}
        self.n_inst = {e: 0 for e in self.ENG}

    def _need(self, eng, reads, writes):
        need = {}

        def add(ev):
            if ev is None:
                return
            sem, val = ev
            k = id(sem)
            if k not in need or need[k][1] < val:
                need[k] = (sem, val)

        for k in reads:
            st = self.state.get(k)
            if st:
                add(st[0])
        for k in writes:
            st = self.state.get(k)
            if st:
                add(st[0])
                for ev in st[1].values():
                    add(ev)
        out = []
        for k, (sem, val) in need.items():
            if eng == "pe" and sem is self.cur_sem["pe"]:
                continue
            if self.seen[eng].get(k, 0) < val:
                self.seen[eng][k] = val
                out.append((sem, val))
        return out

    def _emit_waits(self, eng, waits):
        for sem, val in waits:
            self.streams[eng].append(lambda e, sem=sem, val=val: e.wait_ge(sem, val))

    def _commit(self, ev, reads, writes):
        sem, val = ev
        self.all_events[id(sem)] = ev
        for k in reads:
            st = self.state.setdefault(k, [None, {}])
            st[1][id(sem)] = ev
        for k in writes:
            self.state[k] = [ev, {}]

    def op(self, eng, fn, reads=(), writes=()):
        waits = self._need(eng, reads, writes)
        self._emit_waits(eng, waits)
        if self.cur_cnt[eng] >= SEM_LIMIT:
            self.cur_sem[eng] = self.nc.alloc_semaphore("s_%s%d" % (eng, self.nsem))
            self.nsem += 1
            self.cur_cnt[eng] = 0
        self.cur_cnt[eng] += 1
        sem, val = self.cur_sem[eng], self.cur_cnt[eng]
        self.streams[eng].append(lambda e, sem=sem: fn(e).then_inc(sem, 1))
        self.n_inst[eng] += 1
        self._commit((sem, val), reads, writes)

    def dma(self, q, fn, reads=(), writes=()):
        slot = self.slots[q][self.slot_rr[q]]
        self.slot_rr[q] = (self.slot_rr[q] + 1) % N_DMA_SLOTS
        sem, prev = slot
        waits = self._need(q, reads, writes)
        k = id(sem)
        if self.seen[q].get(k, 0) < prev:
            self.seen[q][k] = prev
            waits.append((sem, prev))
        self._emit_waits(q, waits)
        val = prev + 16
        slot[1] = val
        self.streams[q].append(lambda e, sem=sem: fn(e).then_inc(sem, 16))
        self.n_inst[q] += 1
        self._commit((sem, val), reads, writes)

    def barrier(self):
        evs = list(self.all_events.values())
        for eng in self.ENG:
            for sem, val in evs:
                k = id(sem)
                if eng == "pe" and sem is self.cur_sem["pe"]:
                    continue
                if self.seen[eng].get(k, 0) < val:
                    self.seen[eng][k] = val
                    self.streams[eng].append(lambda e, sem=sem, val=val: e.wait_ge(sem, val))
        self.state = {}

    def finish(self):
        self.barrier()
        nc = self.nc
        with nc.Block() as block:
            @block.tensor
            def _(e):
                for f in self.streams["pe"]:
                    f(e)

            @block.scalar
            def _(e):
                for f in self.streams["act"]:
                    f(e)

            @block.vector
            def _(e):
                for f in self.streams["dve"]:
                    f(e)

            @block.gpsimd
            def _(e):
                for f in self.streams["pool"]:
                    f(e)

            @block.sync
            def _(e):
                for f in self.streams["sp"]:
                    f(e)

    def mm(self, out, lhsT, rhs, start, stop, r, w, skip=False):
        self.op("pe", lambda e: e.matmul(out, lhsT=lhsT, rhs=rhs, start=start, stop=stop,
                                         skip_group_check=skip), r, w)

    def tr(self, out, in_, ident, r, w):
        self.op("pe", lambda e: e.transpose(out, in_, ident), r, w)

    def act(self, out, in_, func, r, w, bias=None, scale=None, accum=None):
        kw = {}
        if bias is not None:
            kw["bias"] = bias
        if scale is not None:
            kw["scale"] = scale
        if accum is not None:
            kw["accum_out"] = accum
        self.op("act", lambda e: e.activation(out=out, in_=in_, func=func, **kw), r, w)

    def tt(self, eng, out, in0, in1, op, r, w):
        self.op(eng, lambda e: e.tensor_tensor(out=out, in0=in0, in1=in1, op=op), r, w)

    def ts(self, eng, out, in0, s1, s2, op0, op1, r, w):
        if op1 is None:
            self.op(eng, lambda e: e.tensor_scalar(out=out, in0=in0, scalar1=s1, scalar2=None, op0=op0), r, w)
        else:
            self.op(eng, lambda e: e.tensor_scalar(out=out, in0=in0, scalar1=s1, scalar2=s2, op0=op0, op1=op1), r, w)

    def stt(self, out, in0, scalar, in1, op0, op1, r, w):
        self.op("dve", lambda e: e.scalar_tensor_tensor(out=out, in0=in0, scalar=scalar, in1=in1, op0=op0, op1=op1), r, w)

    def cp(self, eng, out, in_, r, w):
        if eng == "act":
            self.op("act", lambda e: e.activation(out=out, in_=in_, func=AF.Copy), r, w)
        else:
            self.op(eng, lambda e: e.tensor_copy(out=out, in_=in_), r, w)

    def ld(self, q, out, in_, r, w):
        self.dma(q, lambda e: e.dma_start(out=out, in_=in_), r, w)


def build(phases=PH_ALL, debug=()):
    phases = phases.split(",")
    nc = bass.Bass("TRN2", target_bir_lowering=False)
    m = MK(nc)

    def din(name, shape, dt=F32):
        return nc.dram_tensor(name, list(shape), dt, kind="ExternalInput").ap()

    def dscr(name, shape, dt):
        return nc.dram_tensor(name, list(shape), dt, kind="ExternalOutput" if name in debug else "Internal").ap()

    xv = din("xv", [SV, D])
    memb = din("memb", [256, D])
    lnv = din("lnv", [10, D])
    w_inp = din("w_inp", [D, 4656])
    w1k = din("w1k", [64, 32 * 256]); w1v = din("w1v", [64, 32 * 256])
    pek = din("pek", [64, 32]); pev = din("pev", [64, 32])
    w2k = din("w2k", [256, 64]); w2v = din("w2v", [256, 64])
    diag_raw = din("diag_raw", [128, 16 * 128]); sub1_raw = din("sub1_raw", [128, 16 * 128])
    band_raw = din("band_raw", [16, 16 * 128]); r31 = din("r31", [128, 16])
    diag_mask = din("diag_mask", [128, 128]); anti_mask = din("anti_mask", [128, 128]); band_mask = din("band_mask", [16, 128])
    dw_c = din("dw_c", [16, 272]); ident_c = din("ident_c", [128, 128]); ut_c = din("ut_c", [128, 128])
    ov_c = din("ov_c", [512, 128]); ec_c = din("ec_c", [128, 32]); f0_c = din("f0_c", [128, 128]); band3_c = din("band3_c", [128, 3])
    asel_c = din("asel_c", [64, SV]); keybias_c = din("keybias_c", [128, 64]); cmpbias_c = din("cmpbias_c", [128, 4]); halo_c = din("halo_c", [128, 1])
    dww = din("dww", [128, 8 * 31]); cvec = din("cvec", [128, 24])
    w_out = din("w_out", [D, D]); xa_wq = din("xa_wq", [D, D]); xa_wkv = din("xa_wkv", [D, 2 * D]); xa_wo = din("xa_wo", [D, D])
    rw = din("rw", [D, 36]); rb = din("rb", [1, 36])
    mwg = din("mwg", [NE, D, 512]); mwu = din("mwu", [NE, D, 512]); mwd = din("mwd", [NE, 512, D])
    out = nc.dram_tensor("out", [OWN, D], F32, kind="ExternalOutput").ap()

    kcmpT_d = dscr("kcmpT_d", [4, 64, SV], BF16); vcmpT_d = dscr("vcmpT_d", [4, 64, SV], BF16)
    kslcT_d = dscr("kslcT_d", [4, 64, SV], BF16); kwinT_d = dscr("kwinT_d", [4, 64, SV], BF16)
    vslc_d = dscr("vslc_d", [SV, 256], BF16); vwin_d = dscr("vwin_d", [SV, 256], BF16)
    qT_d = dscr("qT_d", [16, 64, OWN], BF16)
    gates_d = dscr("gates_d", [OWN, 48], F32)
    hconvT_d = dscr("hconvT_d", [8, 128, 128 + OWN], BF16)
    o_d = dscr("o_d", [OWN, 1024], BF16)
    oconvT_d = dscr("oconvT_d", [8, 128, OWN], BF16)
    x1_d = dscr("x1_d", [OWN, D], F32); x1T_d = dscr("x1T_d", [KC, 128, OWN], BF16)
    oxT_d = dscr("oxT_d", [KC, 128, OWN], BF16)
    x2_d = dscr("x2_d", [OWN, D], F32)
    xs_d = dscr("xs_d", [NE * CAP, D], BF16)
    ys_d = dscr("ys_d", [NE * CAP, D], F32)
    rt_d = dscr("rt_d", [OWN, 4], F32)

    ident_f = nc.alloc_sbuf_tensor("ident_f", [128, 128], F32)
    ident_b = nc.alloc_sbuf_tensor("ident_b", [128, 128], BF16)
    ones_b = nc.alloc_sbuf_tensor("ones_b", [128, 128], BF16)
    ones_f = nc.alloc_sbuf_tensor("ones_f", [128, 128], F32)
    lnrep = nc.alloc_sbuf_tensor("lnrep", [128, 2, D], F32)
    PS = [nc.alloc_psum_tensor("ps%d" % i, [128, 512], F32) for i in range(6)]
    PB = [nc.alloc_psum_tensor("pb%d" % i, [128, 1024], BF16) for i in range(2)]
    PSK = ["ps%d" % i for i in range(6)]
    PBK = ["pb%d" % i for i in range(2)]

    m.ld("sp", ident_f[:], ident_c, [], ["ident_f"])
    m.ld("pool", ident_b[:], ident_c, [], ["ident_b"])
    m.op("dve", lambda e: e.memset(ones_b[:], 1.0), [], ["ones_b"])
    m.op("dve", lambda e: e.memset(ones_f[:], 1.0), [], ["ones_f"])

    def load_ln(row):
        m.ld("sp", lnrep[:, 0, :], lnv[row:row + 1, :].broadcast_to([128, D]), [], ["lnrep"])
        m.ld("sp", lnrep[:, 1, :], lnv[row + 1:row + 2, :].broadcast_to([128, D]), [], ["lnrep"])

    uid = {"n": 0}

    class Phase:
        def __init__(self):
            self.es = ExitStack()
            self.n = 0

        def sb(self, name, shape, dt):
            uid["n"] += 1
            return self.es.enter_context(nc.sbuf_tensor("%s_%d" % (name, uid["n"]), list(shape), dt))

        def close(self):
            m.barrier()
            self.es.close()

    rr = {"i": 0}

    def layer_norm(ph_tmp, src, src_keys, dst, dst_keys, np_=128, last_eng="pool"):
        st, mv, rs, nb = ph_tmp
        for c in range(4):
            m.op("dve", lambda e, c=c: e.bn_stats(out=st[:np_, c, :], in_=src[:, c * 512:(c + 1) * 512]), src_keys, ["ln_st"])
        m.op("dve", lambda e: e.bn_aggr(out=mv[:np_, :], in_=st[:np_, :, :]), ["ln_st"], ["ln_mv"])
        m.act(rs[:np_, :], mv[:np_, 1:2], AF.Sqrt, ["ln_mv"], ["ln_rs"], bias=EPS)
        m.op("dve", lambda e: e.reciprocal(out=rs[:np_, :], in_=rs[:np_, :]), ["ln_rs"], ["ln_rs"])
        m.stt(nb[:np_, :], mv[:np_, 0:1], -1.0, rs[:np_, :], ALU.mult, ALU.mult, ["ln_mv", "ln_rs"], ["ln_nb"])
        m.act(src, src, AF.Identity, src_keys + ["ln_rs", "ln_nb"], src_keys, bias=nb[:np_, :], scale=rs[:np_, :])
        m.tt("dve", src, src, lnrep[:np_, 0, :], ALU.mult, src_keys + ["lnrep"], src_keys)
        m.tt(last_eng, dst, src, lnrep[:np_, 1, :], ALU.add, src_keys + ["lnrep"], dst_keys)

    def ln_tmps(ph):
        return (ph.sb("ln_st", [128, 4, 6], F32), ph.sb("ln_mv", [128, 2], F32),
                ph.sb("ln_rs", [128, 1], F32), ph.sb("ln_nb", [128, 1], F32))

    def transpose_to(hT, hT_key, src_bf, src_keys, ntile_idx, nk=KC, col0=0, only=None):
        for k0 in range(0, nk, 8):
            pi = rr["i"] % 2
            rr["i"] += 1
            pb = PB[pi]
            n8 = min(8, nk - k0)
            for kk in range(n8):
                m.tr(pb[:, kk * 128:(kk + 1) * 128], src_bf[:, (k0 + kk) * 128:(k0 + kk + 1) * 128], ident_b[:],
                     src_keys + ["ident_b"], [PBK[pi]])
            eng = only or ("dve" if (rr["i"] % 2) else "act")
            m.cp(eng, hT[:, k0:k0 + n8, col0 + ntile_idx * 128: col0 + (ntile_idx + 1) * 128],
                 pb[:, 0:n8 * 128].rearrange("p (k t) -> p k t", k=n8), [PBK[pi]], [hT_key])

    def phase_A(own_pass):
        ph = Phase()
        tm = ln_tmps(ph)
        load_ln(0)
        if not own_pass:
            wkf = ph.sb("wkf", [128, KC, 1024], BF16)
            wvt = ph.sb("wvt", [128, KC, 512], BF16)
            for k in range(KC):
                m.ld("pool", wkf[:, k, :], w_inp[k * 128:(k + 1) * 128, O_KF:O_KF + 1024], [], ["wkf"])
                m.ld("pool", wvt[:, k, :], w_inp[k * 128:(k + 1) * 128, O_VT:O_VT + 512], [], ["wvt"])
        else:
            wq = ph.sb("wq", [128, KC, 1024], BF16)
            wg = ph.sb("wg", [128, KC, 48], BF16)
            wu = ph.sb("wu", [128, KC, 2048], BF16)
            hal = ph.sb("hal", [128, 1], F32)
            m.ld("sp", hal[:], halo_c, [], ["hal"])
            for k in range(KC):
                m.ld("pool", wq[:, k, :], w_inp[k * 128:(k + 1) * 128, O_Q:O_Q + 1024], [], ["wq"])
                m.ld("pool", wg[:, k, :], w_inp[k * 128:(k + 1) * 128, O_G:O_G + 48], [], ["wg"])
                m.ld("pool", wu[:, k, :], w_inp[k * 128:(k + 1) * 128, O_U:O_U + 2048], [], ["wu"])
        xt = [ph.sb("xt%d" % i, [128, D], F32) for i in range(2)]
        hb = [ph.sb("hb%d" % i, [128, D], BF16) for i in range(4)]
        hT = [ph.sb("hT%d" % i, [128, KC, 512], BF16) for i in range(2)]
        stg = [ph.sb("stg%d" % i, [128, 4, 512], BF16) for i in range(2)]
        vst = [ph.sb("vst%d" % i, [128, 512], BF16) for i in range(2)]
        sig = [ph.sb("sig%d" % i, [128, 512], F32) for i in range(2)]
        gst = ph.sb("gst", [128, 48], F32)
        psi = {"i": 0}

        def nextps():
            i = psi["i"] % 6
            psi["i"] += 1
            return PS[i], PSK[i]

        if not own_pass:
            blocks = [(b * 512, 4) for b in range(16)]
        else:
            blocks = [(OWN - 128, 1)] + [(OWN + b * 512, 4) for b in range(8)]

        def ln_tile(bi, ti):
            tok0 = blocks[bi][0]
            x_ = xt[ti % 2]; xk = "xt%d" % (ti % 2)
            m.ld("sp", x_[:], xv[tok0 + ti * 128: tok0 + (ti + 1) * 128, :], [], [xk])
            layer_norm(tm, x_[:], [xk], hb[ti][:], ["hb%d" % ti])

        def tr_block(bi):
            for ti in range(blocks[bi][1]):
                transpose_to(hT[bi % 2], "hT%d" % (bi % 2), hb[ti], ["hb%d" % ti], ti)

        def groups(bi):
            tok0, ntile = blocks[bi]
            hTb = hT[bi % 2]; hk = "hT%d" % (bi % 2)
            ntok = ntile * 128
            gl = []
            if not own_pass:
                for kind, dst in enumerate((kcmpT_d, vcmpT_d, kslcT_d, kwinT_d)):
                    for pp in range(2):
                        def g_(kind=kind, dst=dst, pp=pp):
                            sg = stg[kind % 2]; sk = "stg%d" % (kind % 2)
                            ps, pk = nextps()
                            c0 = kind * 256 + pp * 128
                            for k in range(KC):
                                m.mm(ps[:, 0:ntok], wkf[:, k, c0:c0 + 128], hTb[:, k, 0:ntok], k == 0, k == KC - 1,
                                     ["wkf", hk], [pk])
                            m.cp("act" if pp % 2 else "dve", sg[:, pp, 0:ntok], ps[:, 0:ntok], [pk], [sk])
                            if pp == 1:
                                m.ld("pool", dst.rearrange("h d t -> (h d) t").rearrange("(a p) t -> p a t", p=128)[:, :, tok0:tok0 + ntok],
                                     sg[:, 0:2, 0:ntok], [sk], [])
                        gl.append(g_)
                for ti in range(ntile):
                    def g_(ti=ti):
                        ps, pk = nextps()
                        for k in range(KC):
                            m.mm(ps[:, :], hTb[:, k, ti * 128:(ti + 1) * 128], wvt[:, k, :], k == 0, k == KC - 1, ["wvt", hk], [pk])
                        v_ = vst[ti % 2]; vk = "vst%d" % (ti % 2)
                        m.cp("act" if ti % 2 else "dve", v_[:], ps[:, :], [pk], [vk])
                        r0 = tok0 + ti * 128
                        m.ld("pool", vslc_d[r0:r0 + 128, :], v_[:, 0:256], [vk], [])
                        m.ld("pool", vwin_d[r0:r0 + 128, :], v_[:, 256:512], [vk], [])
                    gl.append(g_)
            else:
                halo = (bi == 0)
                if not halo:
                    o0 = tok0 - OWN
                    for hg in range(4):
                        for pp in range(2):
                            def g_(hg=hg, pp=pp):
                                sg = stg[hg % 2]; sk = "stg%d" % (hg % 2)
                                c0 = (hg * 4 + 2 * pp) * 64
                                ps, pk = nextps()
                                for k in range(KC):
                                    m.mm(ps[:, 0:ntok], wq[:, k, c0:c0 + 128], hTb[:, k, 0:ntok], k == 0, k == KC - 1,
                                         ["wq", hk], [pk])
                                m.act(sg[:, pp, 0:ntok], ps[:, 0:ntok], AF.Copy, [pk], [sk], scale=0.125)
                                if pp == 1:
                                    m.ld("pool", qT_d[hg * 4:(hg + 1) * 4].rearrange("h d t -> (h d) t").rearrange("(a p) t -> p a t", p=128)[:, :, o0:o0 + ntok],
                                         sg[:, 0:2, 0:ntok], [sk], [])
                            gl.append(g_)
                    for ti in range(ntile):
                        def g_(ti=ti):
                            ps, pk = nextps()
                            for k in range(KC):
                                m.mm(ps[:, 0:48], hTb[:, k, ti * 128:(ti + 1) * 128], wg[:, k, :], k == 0, k == KC - 1, ["wg", hk], [pk])
                            m.act(gst[:], ps[:, 0:48], AF.Sigmoid, [pk], ["gst"])
                            m.ld("pool", gates_d[o0 + ti * 128:o0 + (ti + 1) * 128, :], gst[:], ["gst"], [])
                        gl.append(g_)
                for cc in range(8):
                    def g_(cc=cc):
                        pa, pak = nextps()
                        pg, pgk = nextps()
                        for k in range(KC):
                            m.mm(pa[:, 0:ntok], wu[:, k, cc * 128:(cc + 1) * 128], hTb[:, k, 0:ntok], k == 0, k == KC - 1, ["wu", hk], [pak])
                        for k in range(KC):
                            m.mm(pg[:, 0:ntok], wu[:, k, 1024 + cc * 128:1024 + (cc + 1) * 128], hTb[:, k, 0:ntok], k == 0, k == KC - 1, ["wu", hk], [pgk])
                        s_ = sig[cc % 2]; sk2 = "sig%d" % (cc % 2)
                        v_ = vst[cc % 2]; vk = "vst%d" % (cc % 2)
                        m.act(s_[:, 0:ntok], pg[:, 0:ntok], AF.Sigmoid, [pgk], [sk2])
                        if halo:
                            m.ts("dve", s_[:, 0:ntok], s_[:, 0:ntok], hal[:, 0:1], None, ALU.mult, None, [sk2, "hal"], [sk2])
                        m.tt("dve", v_[:, 0:ntok], pa[:, 0:ntok], s_[:, 0:ntok], ALU.mult, [pak, sk2], [vk])
                        c0 = 0 if halo else 128 + (tok0 - OWN)
                        m.ld("pool", hconvT_d[cc, :, c0:c0 + ntok], v_[:, 0:ntok], [vk], [])
                    gl.append(g_)
            return gl

        for ti in range(blocks[0][1]):
            ln_tile(0, ti)
        tr_block(0)
        for bi in range(len(blocks)):
            gs = groups(bi)
            nxt = bi + 1 < len(blocks)
            nt_n = blocks[bi + 1][1] if nxt else 0
            npart = nt_n + 1
            bounds = [len(gs) * p // npart for p in range(npart + 1)]
            for p in range(npart):
                for g_ in gs[bounds[p]:bounds[p + 1]]:
                    g_()
                if p < nt_n:
                    ln_tile(bi + 1, p)
            if nxt:
                tr_block(bi + 1)
        ph.close()

    if "A1" in phases:
        phase_A(False)
    if "A2" in phases:
        phase_A(True)

    def phase_BC():
        ph = Phase()
        kcT = ph.sb("kcT", [128, 4, 512], BF16)
        vcx = ph.sb("vcx", [128, 4, 4, 193], BF16)
        m.op("dve", lambda e: e.memset(kcT[:], 0.0), [], ["kcT"])
        m.op("dve", lambda e: e.memset(vcx[:], 0.0), [], ["vcx"])
        for h in range(4):
            m.op("dve", lambda e, h=h: e.memset(vcx[:, :, h, 64:65], 1.0), [], ["vcx"])
            m.ld("pool", vcx[:, :, h, 65:193], ov_c.rearrange("(t p) j -> p t j", p=128), [], ["vcx"])
        pb_ = Phase()
        w1s = pb_.sb("w1s", [64, 32, 256], BF16)
        w2s = pb_.sb("w2s", [128, 2, 64], BF16)
        pes = pb_.sb("pes", [64, 32], BF16)
        cj = pb_.sb("cj", [128, 2], F32)
        Tin = [pb_.sb("Tin%d" % i, [64, SV], BF16) for i in range(2)]
        T2 = [pb_.sb("T2_%d" % i, [64, 16, 512], BF16) for i in range(2)]
        gu = pb_.sb("gu", [128, 512], F32); g2 = pb_.sb("g2", [128, 512], F32)
        gT = pb_.sb("gT", [128, 2, 512], BF16)
        m.op("dve", lambda e: e.memset(gT[:], 0.0), [], ["gT"])
        for kv in range(2):
            m.ld("pool", w1s[:], (w1k, w1v)[kv].rearrange("d (r j) -> d r j", r=32), [], ["w1s"])
            m.ld("pool", w2s[:], (w2k, w2v)[kv].rearrange("(c p) d -> p c d", p=128), [], ["w2s"])
            m.ld("pool", pes[:], (pek, pev)[kv], [], ["pes"])
            for jc in range(2):
                for r in range(32):
                    m.mm(PS[0][:, jc * 2:jc * 2 + 2], w1s[:, r, jc * 128:(jc + 1) * 128], pes[:, r:r + 1].broadcast_to([64, 2]), r == 0, r == 31,
                         ["w1s", "pes"], ["ps0"], skip=True)
            m.cp("dve", cj[:], PS[0][:, 0:4:2], ["ps0"], ["cj"])
            for h in range(4):
                T_ = Tin[h % 2]; tk = "Tin%d" % (h % 2)
                m.ld("sp", T_[:], (kcmpT_d, vcmpT_d)[kv][h], [], [tk])
                t2 = T2[h % 2]; t2k = "T2_%d" % (h % 2)
                for r8 in range(0, 16, 8):
                    m.cp("dve" if r8 else "pool", t2[:, r8:r8 + 8, :], T_[:].rearrange("d (n r) -> d r n", r=16)[:, r8:r8 + 8, :], [tk], [t2k])
                for jc in range(2):
                    ps, pk = PS[1 + jc], PSK[1 + jc]
                    for r in range(32):
                        m.mm(ps[:, 0:511], w1s[:, r, jc * 128:(jc + 1) * 128], t2[:, r % 16, r // 16:r // 16 + 511], r == 0, r == 31,
                             ["w1s", t2k], [pk])
                    m.act(gu[:, 0:511], ps[:, 0:511], AF.Identity, [pk, "cj"], ["gu"], bias=cj[:, jc:jc + 1])
                    m.tt("dve", g2[:, 0:511], gu[:, 0:511], gu[:, 0:511], ALU.mult, ["gu"], ["g2"])
                    m.ts("dve", g2[:, 0:511], g2[:, 0:511], 0.044715, 1.0, ALU.mult, ALU.add, ["g2"], ["g2"])
                    m.tt("dve", g2[:, 0:511], g2[:, 0:511], gu[:, 0:511], ALU.mult, ["g2", "gu"], ["g2"])
                    m.act(g2[:, 0:511], g2[:, 0:511], AF.Tanh, ["g2"], ["g2"], scale=0.7978845608028654)
                    m.ts("dve", g2[:, 0:511], g2[:, 0:511], 1.0, 0.5, ALU.add, ALU.mult, ["g2"], ["g2"])
                    m.tt("dve", gT[:, jc, 0:511], g2[:, 0:511], gu[:, 0:511], ALU.mult, ["g2", "gu"], ["gT"])
                if kv == 0:
                    for jc in range(2):
                        m.mm(PS[3][0:64, 0:512], w2s[:, jc, :], gT[:, jc, :], jc == 0, jc == 1, ["w2s", "gT"], ["ps3"])
                    m.cp("dve", kcT[0:64, h, 0:511], PS[3][0:64, 0:511], ["ps3"], ["kcT"])
                else:
                    for bt in range(4):
                        for jc in range(2):
                            m.mm(PS[3][:, bt * 64:(bt + 1) * 64], gT[:, jc, bt * 128:(bt + 1) * 128], w2s[:, jc, :], jc == 0, jc == 1,
                                 ["w2s", "gT"], ["ps3"], skip=True)
                    m.cp("dve", vcx[:, :, h, 0:64], PS[3][:, 0:256].rearrange("p (t d) -> p t d", t=4), ["ps3"], ["vcx"])
        pb_.close()
        if "C" not in phases:
            ph.close()
            return
        tabs = ph.sb("tabs", [128, 2, 2048], BF16)
        anti = ph.sb("anti", [128, 512], BF16)
        bandt = ph.sb("bandt", [16, 2048], BF16)
        dwt = ph.sb("dwt", [16, 272], BF16)
        i4 = ph.sb("i4", [128, 512], BF16)
        f0 = ph.sb("f0", [128, 128], F32); b3 = ph.sb("b3", [128, 3], F32)
        kbias = ph.sb("kbias", [128, 64], F32); cbias = ph.sb("cbias", [128, 4], F32)
        with ExitStack() as es:
            raw = es.enter_context(nc.sbuf_tensor("raw_t", [128, 16, 128], F32))
            r31s = es.enter_context(nc.sbuf_tensor("r31s", [128, 16], F32))
            msk = es.enter_context(nc.sbuf_tensor("msk", [128, 128], F32))
            m.ld("sp", r31s[:], r31, [], ["r31s"])
            m.ld("sp", raw[:].rearrange("p h q -> p (h q)"), diag_raw, [], ["raw"])
            m.ld("sp", msk[:], diag_mask, [], ["msk"])
            for hd in range(16):
                m.stt(tabs[:, 0, hd * 128:(hd + 1) * 128], raw[:, hd, :], r31s[:, hd:hd + 1], msk[:], ALU.subtract, ALU.add, ["raw", "r31s", "msk"], ["tabs"])
            m.ld("sp", raw[:].rearrange("p h q -> p (h q)"), sub1_raw, [], ["raw"])
            for hd in range(16):
                m.ts("dve", tabs[:, 1, hd * 128:(hd + 1) * 128], raw[:, hd, :], r31s[:, hd:hd + 1], None, ALU.subtract, None, ["raw", "r31s"], ["tabs"])
            m.ld("sp", raw[0:16].rearrange("p h q -> p (h q)"), band_raw, [], ["raw"])
            m.ld("sp", msk[0:16, :], band_mask, [], ["msk"])
            for hd in range(16):
                m.stt(bandt[:, hd * 128:(hd + 1) * 128], raw[0:16, hd, :], r31s[0:16, hd:hd + 1], msk[0:16, :], ALU.subtract, ALU.add, ["raw", "r31s", "msk"], ["bandt"])
            m.ld("sp", msk[:], anti_mask, [], ["msk"])
            for g in range(4):
                m.cp("dve", anti[:, g * 128:(g + 1) * 128], msk[:], ["msk"], ["anti"])
                m.cp("dve", i4[:, g * 128:(g + 1) * 128], ident_f[:], ["ident_f"], ["i4"])
            m.barrier()
        m.ld("pool", dwt[:], dw_c, [], ["dwt"])
        m.ld("sp", f0[:], f0_c, [], ["f0"]); m.ld("sp", b3[:], band3_c, [], ["b3"])
        m.ld("sp", kbias[:], keybias_c, [], ["kbias"]); m.ld("sp", cbias[:], cmpbias_c, [], ["cbias"])

        kslc = ph.sb("kslc", [128, SV], BF16)
        kwin = ph.sb("kwin", [128, OWN + 512], BF16)
        vslc = ph.sb("vslc", [128, 64, 65], BF16)
        vwin = ph.sb("vwin", [128, 36, 65], BF16)
        qT = ph.sb("qT", [64, NT, 512], BF16)
        gts = ph.sb("gts", [128, NT, 12], F32)
        pT = [ph.sb("pT%d" % i, [128, 512], BF16) for i in range(6)]
        mneg = [ph.sb("mneg%d" % i, [128, 192], BF16) for i in range(2)]
        QM = [[ph.sb("QM%d%d" % (i, c), [128, 512], BF16) for c in range(2)] for i in range(3)]
        for i in range(2):
            m.op("pool", lambda e, i=i: e.memset(mneg[i][:], 0.0), [], ["mneg%d" % i])
        for i in range(3):
            for c in range(2):
                m.op("pool", lambda e, i=i, c=c: e.memset(QM[i][c][:], 0.0), [], ["QM%d" % i])
        m.op("pool", lambda e: e.memset(kwin[64:128, :], 0.0), [], ["kwin_hi"])
        for c4 in range(4):
            m.ld("pool", kslc[64:128, c4 * 2048:(c4 + 1) * 2048], asel_c[:, c4 * 2048:(c4 + 1) * 2048], [], ["kslc_hi"])
        sc = ph.sb("sc", [128, 128], F32); sc2 = ph.sb("sc2", [128, 128], F32)
        mx = ph.sb("mx", [128, 16], F32)
        acc = [ph.sb("acc%d" % i, [128, 3, 4, 65], F32) for i in range(2)]
        rinv = [ph.sb("rinv%d" % i, [128, 12], F32) for i in range(2)]
        ob = [ph.sb("ob%d" % i, [128, 256], BF16) for i in range(2)]
        of = ph.sb("of", [128, 4, 64], F32)
        m.op("dve", lambda e: e.memset(vslc[:, :, 64:65], 1.0), [], ["vslc"])
        m.op("dve", lambda e: e.memset(vwin[:, :, 64:65], 1.0), [], ["vwin"])
        SCB = [PS[0][:], PS[1][:], PS[2][:], PB[1][:].bitcast(F32)]
        SCK = ["ps0", "ps1", "ps2", "pb1"]
        WACC = PB[0][:].bitcast(F32)
        sidx = {"i": 0}
        pidx = {"i": 0}
        DEPTH = 3
        pend = []
        delayed = []

        def run_delayed():
            while delayed:
                delayed.pop(0)[1]()

        def flush_one():
            u = pend.pop(0)
            u[0]()
            for p in u[1]:
                p()

        def add_unit(kT_ap, kkeys, rows, qap, extra, bias_ap, bias_keys, accps, acck, v_ap, vkeys, ncol, first, last):
            si = sidx["i"] % 4; sidx["i"] += 1
            ps, pk = SCB[si], SCK[si]
            n_ex = len(extra)
            m.mm(ps[0:rows, :], kT_ap, qap, True, n_ex == 0, kkeys + ["qT"], [pk])
            for xi, (l_ap, r_ap, ks) in enumerate(extra):
                m.mm(ps[0:rows, :], l_ap, r_ap, False, xi == n_ex - 1, ks, [pk])

            def pv():
                pi = pidx["i"] % 6; pidx["i"] += 1
                p_, ppk = pT[pi], "pT%d" % pi
                m.act(p_[0:rows, :], ps[0:rows, :], AF.Exp, [pk] + bias_keys, [ppk], bias=bias_ap)
                for g in range(4):
                    m.mm(accps[:, g * ncol:(g + 1) * ncol] if ncol == 65 else accps[g // 2][:, (g % 2) * ncol:(g % 2 + 1) * ncol],
                         p_[0:rows, g * 128:(g + 1) * 128], v_ap, first and (g == 0 or (ncol != 65 and g == 2)), last and g == 3,
                         [ppk] + vkeys, acck, skip=True)

            pend.append((pv, []))
            if len(pend) > DEPTH:
                flush_one()
            for dl in list(delayed):
                dl[0] -= 1
                if dl[0] <= 0:
                    delayed.remove(dl)
                    dl[1]()

        def add_post(fn):
            if pend:
                pend[-1][1].append(fn)
            else:
                fn()

        for h in range(4):
            for g in range(4):
                m.ld("sp", qT[:].rearrange("d l (g q) -> d l g q", g=4)[:, :, g, :],
                     qT_d[h * 4 + g].rearrange("d (l q) -> d l q", q=128), [], ["qT"])
            m.ld("sp", gts[:], gates_d[:, h * 12:(h + 1) * 12].rearrange("(t p) c -> p t c", p=128), [], ["gts"])
            m.ld("sp", kwin[0:64, :], kwinT_d[h][:, OWN - 512:SV], [], ["kwin"])
            for t8 in range(4):
                m.ld("sp", vwin[:, t8 * 9:(t8 + 1) * 9, 0:64],
                     vwin_d[OWN - 512 + t8 * 1152:OWN - 512 + (t8 + 1) * 1152, h * 64:(h + 1) * 64].rearrange("(t p) d -> p t d", p=128), [], ["vwin"])
            m.ld("sp", kslc[0:64, :], kslcT_d[h], [], ["kslc"])
            for t8 in range(8):
                m.ld("sp", vslc[:, t8 * 8:(t8 + 1) * 8, 0:64],
                     vslc_d[t8 * 1024:(t8 + 1) * 1024, h * 64:(h + 1) * 64].rearrange("(t p) d -> p t d", p=128), [], ["vslc"])

            def emit_cmp(lt, h=h):
                t = 32 + lt
                qmk = "QM%d" % (lt % 3)
                for l2 in ([0, 1] if lt == 0 else [lt + 1]):
                    if l2 < NT:
                        for c in range(2):
                            m.cp("pool", QM[l2 % 3][c][0:64, :], qT[:, l2, :], ["qT"], ["QM%d" % (l2 % 3)])
                qap = QM[lt % 3][0][:]
                a_ = acc[lt % 2]; ak = "acc%d" % (lt % 2)
                rv = rinv[lt % 2]; rvk = "rinv%d" % (lt % 2)
                n_c = 8 * t + 7
                ntile = (n_c + 127) // 128
                b0 = 8 * t - 9
                jb, off = b0 // 128, b0 % 128
                for J in range(ntile):
                    rows = min(128, n_c - 128 * J)
                    extra = []
                    if J == jb:
                        extra.append((dwt[:, 128 - off:128 - off + rows], bandt[:, h * 512:(h + 1) * 512], ["dwt", "bandt"]))
                    elif J == jb + 1:
                        extra.append((dwt[:, 256 - off:256 - off + rows], bandt[:, h * 512:(h + 1) * 512], ["dwt", "bandt"]))
                    add_unit(kcT[:, h, J * 128:J * 128 + rows], ["kcT", qmk], rows, qap, extra, cbias[0:rows, J:J + 1], ["cbias"],
                             (PS[4], PS[5]), ["ps4", "ps5"], vcx[0:rows, J, h, :], ["vcx"], 193, J == 0, J == ntile - 1)

                def post():
                    for half in range(2):
                        pc = PS[4 + half]
                        m.cp("act", a_[:, 0, half * 2:half * 2 + 2, :], pc[:, 0:386].rearrange("p (g c) -> p g c", g=2)[:, :, 0:65],
                             ["ps%d" % (4 + half)], [ak])
                    m.ts("dve", rv[:, 0:4], a_[:, 0, :, 64], 1e-30, None, ALU.max, None, [ak], [rvk])
                    m.op("dve", lambda e: e.reciprocal(out=rv[:, 0:4], in_=rv[:, 0:4]), [rvk], [rvk])
                    for g in range(4):
                        pc = PS[4 + g // 2]
                        u_ap = pc[:, (g % 2) * 193 + 65:(g % 2) * 193 + 193]
                        if g == 0:
                            m.stt(sc[:], u_ap, rv[:, 0:1], f0[:], ALU.mult, ALU.add, ["ps4", rvk, "f0"], ["sc"])
                        else:
                            m.stt(sc[:], u_ap, rv[:, g:g + 1], sc[:], ALU.mult, ALU.add, ["ps%d" % (4 + g // 2), rvk, "sc"], ["sc"])
                    ncol = 2 * t + 2
                    m.tt("dve", sc[:, 2 * t - 1:2 * t + 2], sc[:, 2 * t - 1:2 * t + 2], b3[:], ALU.add, ["sc", "b3"], ["sc"])
                    m.op("dve", lambda e: e.max(out=mx[:, 0:8], in_=sc[:, 0:ncol]), ["sc"], ["mx"])
                    m.op("dve", lambda e: e.match_replace(out=sc2[:, 0:ncol], in_to_replace=mx[:, 0:8], in_values=sc[:, 0:ncol], imm_value=-1e9),
                         ["sc", "mx"], ["sc2"])
                    m.op("dve", lambda e: e.max(out=mx[:, 8:16], in_=sc2[:, 0:ncol]), ["sc2"], ["mx"])
                    mn = mneg[lt % 2]; mk_ = "mneg%d" % (lt % 2)
                    m.ts("dve", sc2[:, 0:ncol], sc[:, 0:ncol], mx[:, 15:16], -NEG, ALU.is_ge, ALU.mult, ["sc", "mx"], ["sc2"])
                    m.ts("dve", mn[:, 64:64 + ncol], sc2[:, 0:ncol], NEG, None, ALU.add, None, ["sc2"], [mk_])

                    def post_b():
                        for c in range(2):
                            m.mm(PS[4 + c][:, :], mn[:, 64 * c:64 * c + 128], i4[:], True, True, [mk_, "i4"], ["ps%d" % (4 + c)])
                            m.cp("dve", QM[lt % 3][c][64:128, :], PS[4 + c][64:128, :], ["ps%d" % (4 + c)], [qmk])
                    delayed.append([10, post_b])
                add_post(post)

            def emit_win(lt, h=h):
                t = 32 + lt
                qmk = "QM%d" % (lt % 3)
                qap = QM[lt % 3][0][:]
                a_ = acc[lt % 2]; ak = "acc%d" % (lt % 2)
                for j in range(t - 4, t + 1):
                    extra = []
                    if j == t:
                        extra.append((ident_b[:], tabs[:, 0, h * 512:(h + 1) * 512], ["ident_b", "tabs"]))
                    elif j == t - 1:
                        extra.append((ident_b[:], tabs[:, 1, h * 512:(h + 1) * 512], ["ident_b", "tabs"]))
                    elif j == t - 4:
                        extra.append((ident_b[:], anti[:], ["ident_b", "anti"]))
                    jl = j - 28
                    add_unit(kwin[:, jl * 128:(jl + 1) * 128], ["kwin", "kwin_hi", qmk], 128, qap, extra, kbias[:, j:j + 1], ["kbias"],
                             WACC, ["pb0"], vwin[:, jl, :], ["vwin"], 65, j == t - 4, j == t)
                add_post(lambda: m.cp("act", a_[:, 2, :, :], WACC[:, 0:260].rearrange("p (g c) -> p g c", g=4), ["pb0"], [ak]))

            def emit_sel(lt, h=h):
                t = 32 + lt
                qmk = "QM%d" % (lt % 3)
                a_ = acc[lt % 2]; ak = "acc%d" % (lt % 2)
                rv = rinv[lt % 2]; rvk = "rinv%d" % (lt % 2)
                if lt == 0:
                    while pend:
                        flush_one()
                    run_delayed()
                for j in range(t + 1):
                    qap = QM[lt % 3][j // 32][:]
                    extra = []
                    if j == t:
                        extra.append((ident_b[:], tabs[:, 0, h * 512:(h + 1) * 512], ["ident_b", "tabs"]))
                    elif j == t - 1:
                        extra.append((ident_b[:], tabs[:, 1, h * 512:(h + 1) * 512], ["ident_b", "tabs"]))
                    add_unit(kslc[:, j * 128:(j + 1) * 128], ["kslc", "kslc_hi", qmk], 128, qap, extra, kbias[:, j:j + 1], ["kbias"],
                             PS[3], ["ps3"], vslc[:, j, :], ["vslc"], 65, j == 0, j == t)

                def post():
                    m.cp("act", a_[:, 1, :, :], PS[3][:, 0:260].rearrange("p (g c) -> p g c", g=4), ["ps3"], [ak])
                    m.ts("dve", rv[:, 4:12].rearrange("p (b g) -> p b g", b=2), a_[:, 1:3, :, 64], 1e-30, None, ALU.max, None, [ak], [rvk])
                    m.op("dve", lambda e: e.reciprocal(out=rv[:, 4:12], in_=rv[:, 4:12]), [rvk], [rvk])
                    m.tt("dve", rv[:].rearrange("p (b g) -> p b g", b=3), rv[:].rearrange("p (b g) -> p b g", b=3),
                         gts[:, lt, :].rearrange("p (g b) -> p b g", b=3), ALU.mult, [rvk, "gts"], [rvk])
                    o_ = ob[lt % 2]; ok = "ob%d" % (lt % 2)
                    for g in range(4):
                        m.ts("dve", of[:, g, :], a_[:, 0, g, 0:64], rv[:, g:g + 1], None, ALU.mult, None, [ak, rvk], ["of"])
                        m.stt(of[:, g, :], a_[:, 1, g, 0:64], rv[:, 4 + g:5 + g], of[:, g, :], ALU.mult, ALU.add, [ak, rvk, "of"], ["of"])
                        m.stt(o_[:, g * 64:(g + 1) * 64], a_[:, 2, g, 0:64], rv[:, 8 + g:9 + g], of[:, g, :], ALU.mult, ALU.add, [ak, rvk, "of"], [ok])
                    m.ld("sp", o_d[lt * 128:(lt + 1) * 128, h * 256:(h + 1) * 256], o_[:], [ok], [])
                add_post(post)

            emit_cmp(0)
            for lt in range(NT):
                emit_win(lt)
                if lt + 1 < NT:
                    emit_cmp(lt + 1)
                emit_sel(lt)
            while pend:
                flush_one()
            run_delayed()
        ph.close()

    if "B" in phases:
        phase_BC()

    def phase_D():
        ph = Phase()
        dg = ph.sb("dg", [128, 8, 31, 128], BF16)
        wc = ph.sb("wc", [128, 8, 31], F32)
        cv = ph.sb("cv", [128, 24], F32)
        hc = ph.sb("hc", [128, 8, 128 + OWN], BF16)
        yb = ph.sb("yb", [128, 8, 512], F32)
        ysq = ph.sb("ysq", [128, 512], F32)
        mean = ph.sb("mean", [128, 512], F32); rstd = ph.sb("rstd", [128, 512], F32)
        z = [ph.sb("z%d" % i, [128, 512], F32) for i in range(4)]
        zo = [ph.sb("zo%d" % i, [128, 512], BF16) for i in range(4)]
        m.ld("sp", wc[:].rearrange("p c k -> p (c k)"), dww, [], ["wc"])
        m.ld("sp", cv[:], cvec, [], ["cv"])
        for cc in range(8):
            m.ld("sp", hc[:, cc, :], hconvT_d[cc], [], ["hc"])
            for k in range(31):
                if k % 2:
                    m.ts("dve", dg[:, cc, k, :], ident_f[:], wc[:, cc, k:k + 1], None, ALU.mult, None, ["ident_f", "wc"], ["dg"])
                else:
                    m.act(dg[:, cc, k, :], ident_f[:], AF.Copy, ["ident_f", "wc"], ["dg"], scale=wc[:, cc, k:k + 1])
        for blk in range(8):
            for cc in range(8):
                ps, pk = PS[cc % 4], PSK[cc % 4]
                for k in range(31):
                    c0 = 128 + blk * 512 - 30 + k
                    m.mm(ps[:, :], dg[:, cc, k, :], hc[:, cc, c0:c0 + 512], k == 0, k == 30, ["dg", "hc"], [pk])
                m.act(yb[:, cc, :], ps[:, :], AF.Identity, [pk, "cv"], ["yb%d" % cc], bias=cv[:, cc:cc + 1])
                m.act(ysq[:], yb[:, cc, :], AF.Square, ["yb%d" % cc], ["ysq"])
                m.mm(PS[4][:, :], ones_f[:], yb[:, cc, :], cc == 0, cc == 7, ["ones_f", "yb%d" % cc], ["ps4"])
                m.mm(PS[5][:, :], ones_f[:], ysq[:], cc == 0, cc == 7, ["ones_f", "ysq"], ["ps5"])
            m.act(mean[:], PS[4][:, :], AF.Copy, ["ps4"], ["mean"], scale=1.0 / 1024)
            m.tt("dve", rstd[:], mean[:], mean[:], ALU.mult, ["mean"], ["rstd"])
            m.stt(rstd[:], PS[5][:, :], 1.0 / 1024, rstd[:], ALU.mult, ALU.subtract, ["ps5", "rstd"], ["rstd"])
            m.act(rstd[:], rstd[:], AF.Sqrt, ["rstd"], ["rstd"], bias=EPS)
            m.op("dve", lambda e: e.reciprocal(out=rstd[:], in_=rstd[:]), ["rstd"], ["rstd"])
            for cc in range(8):
                z_ = z[cc % 4]; zk = "z%d" % (cc % 4)
                zo_ = zo[cc % 4]; zok = "zo%d" % (cc % 4)
                m.tt("dve", z_[:], yb[:, cc, :], mean[:], ALU.subtract, ["yb%d" % cc, "mean"], [zk])
                m.tt("pool", z_[:], z_[:], rstd[:], ALU.mult, [zk, "rstd"], [zk])
                m.act(zo_[:], z_[:], AF.Silu, [zk, "cv"], [zok], bias=cv[:, 16 + cc:17 + cc], scale=cv[:, 8 + cc:9 + cc])
                m.ld("sp", oconvT_d[cc, :, blk * 512:(blk + 1) * 512], zo_[:], [zok], [])
        ph.close()

    if "D" in phases:
        phase_D()

    def load_w_bf(ph, name, src, ncols, q="pool"):
        wt = ph.sb(name, [128, KC, ncols], BF16)
        for k4 in range(0, KC, 4):
            m.ld(q, wt[:, k4:k4 + 4, :], src[k4 * 128:(k4 + 4) * 128, :].rearrange("(k p) n -> p k n", p=128), [], [name])
        return wt

    def phase_E1():
        ph = Phase()
        tm = ln_tmps(ph)
        wo_ = load_w_bf(ph, "w_o", w_out, D)
        lnrep2 = ph.sb("lnrep2", [128, 2, D], F32)
        m.ld("sp", lnrep2[:, 0, :], lnv[2:3, :].broadcast_to([128, D]), [], ["lnrep2"])
        m.ld("sp", lnrep2[:, 1, :], lnv[3:4, :].broadcast_to([128, D]), [], ["lnrep2"])
        load_ln(0)
        ot = [ph.sb("ot%d" % i, [128, 1024], BF16) for i in range(2)]
        mixT = [ph.sb("mixT%d" % i, [128, KC, 128], BF16) for i in range(2)]
        xt = [ph.sb("e1x%d" % i, [128, D], F32) for i in range(2)]
        y = [ph.sb("e1y%d" % i, [128, D], F32) for i in range(2)]
        yb_ = [ph.sb("e1yb%d" % i, [128, D], BF16) for i in range(2)]
        xT = [ph.sb("e1xT%d" % i, [128, KC, 128], BF16) for i in range(2)]
        def e1_part2(lt):
            i2 = lt % 2
            transpose_to(xT[i2], "e1xT%d" % i2, yb_[i2], ["e1yb%d" % i2], 0)
            m.ld("pool", x1T_d.rearrange("c p t -> p c t")[:, :, lt * 128:(lt + 1) * 128], xT[i2][:], ["e1xT%d" % i2], [])

        def e1_stage0(lt):
            i2 = lt % 2
            m.ld("sp", ot[i2][:], o_d[lt * 128:(lt + 1) * 128, :], [], ["ot%d" % i2])
            transpose_to(mixT[i2], "mixT%d" % i2, ot[i2], ["ot%d" % i2], 0, nk=8)
            m.ld("sp", mixT[i2][:, 8:16, :], oconvT_d.rearrange("c p t -> p c t")[:, :, lt * 128:(lt + 1) * 128], [], ["mixT%d" % i2])
            m.ld("sp", xt[i2][:], xv[OWN + lt * 128:OWN + (lt + 1) * 128, :], [], ["e1x%d" % i2])
            layer_norm(tm, xt[i2][:], ["e1x%d" % i2], xt[i2][:], ["e1x%d" % i2])

        e1_stage0(0)
        for lt in range(NT):
            i2 = lt % 2
            if lt + 1 < NT:
                e1_stage0(lt + 1)
            for oc in range(4):
                ps, pk = PS[oc], PSK[oc]
                for k in range(KC):
                    m.mm(ps[:, :], mixT[i2][:, k, :], wo_[:, k, oc * 512:(oc + 1) * 512], k == 0, k == KC - 1, ["mixT%d" % i2, "w_o"], [pk])
                m.stt(y[i2][:, oc * 512:(oc + 1) * 512], xt[i2][:, oc * 512:(oc + 1) * 512], ALPHA, ps[:, :], ALU.mult, ALU.add,
                      ["e1x%d" % i2, pk], ["e1y%d" % i2])
            ln_generic(tm, y[i2][:], ["e1y%d" % i2], lnrep2, "lnrep2")
            m.ld("pool", x1_d[lt * 128:(lt + 1) * 128, :], y[i2][:], ["e1y%d" % i2], [])
            m.cp("pool", yb_[i2][:], y[i2][:], ["e1y%d" % i2], ["e1yb%d" % i2])
            if lt > 0:
                e1_part2(lt - 1)
        e1_part2(NT - 1)
        ph.close()

    def ln_generic(tm, src, src_keys, rep, repk, np_=128, last_eng="pool"):
        st, mv, rs, nb = tm
        for c in range(4):
            m.op("dve", lambda e, c=c: e.bn_stats(out=st[:np_, c, :], in_=src[:, c * 512:(c + 1) * 512]), src_keys, ["ln_st"])
        m.op("dve", lambda e: e.bn_aggr(out=mv[:np_, :], in_=st[:np_, :, :]), ["ln_st"], ["ln_mv"])
        m.act(rs[:np_, :], mv[:np_, 1:2], AF.Sqrt, ["ln_mv"], ["ln_rs"], bias=EPS)
        m.op("dve", lambda e: e.reciprocal(out=rs[:np_, :], in_=rs[:np_, :]), ["ln_rs"], ["ln_rs"])
        m.stt(nb[:np_, :], mv[:np_, 0:1], -1.0, rs[:np_, :], ALU.mult, ALU.mult, ["ln_mv", "ln_rs"], ["ln_nb"])
        m.act(src, src, AF.Identity, src_keys + ["ln_rs", "ln_nb"], src_keys, bias=nb[:np_, :], scale=rs[:np_, :])
        m.tt("dve", src, src, rep[:np_, 0, :], ALU.mult, src_keys + [repk], src_keys)
        m.tt(last_eng, src, src, rep[:np_, 1, :], ALU.add, src_keys + [repk], src_keys)

    if "E1" in phases:
        phase_E1()

    def phase_E2():
        ph = Phase()
        tm = ln_tmps(ph)
        KmT = ph.sb("KmT", [128, KC, 256], BF16)
        Vm = ph.sb("Vm", [128, 2, D], BF16)
        load_ln(4)
        pm = Phase()
        mt_ = pm.sb("mt_", [128, D], F32); mb_ = pm.sb("mb_", [128, D], BF16)
        mT = pm.sb("mT", [128, KC, 256], BF16)
        wkv = [pm.sb("wkv%d" % i, [128, KC, 512], BF16) for i in range(2)]
        wkvf = pm.sb("wkvf", [128, KC, 512], F32)
        for ti in range(2):
            m.ld("sp", mt_[:], memb[ti * 128:(ti + 1) * 128, :], [], ["mt_"])
            layer_norm(tm, mt_[:], ["mt_"], mb_[:], ["mb_"])
            transpose_to(mT, "mT", mb_, ["mb_"], ti)
        for cb in range(8):
            w_ = wkv[cb % 2]; wk_ = "wkv%d" % (cb % 2)
            if cb % 2 == 0:
                for k4 in range(0, KC, 4):
                    m.ld("pool", w_[:, k4:k4 + 4, :],
                         xa_wkv[k4 * 128:(k4 + 4) * 128, cb * 512:(cb + 1) * 512].rearrange("(k p) n -> p k n", p=128), [], [wk_])
            else:
                for k4 in range(0, KC, 4):
                    m.ld("sp", wkvf[:, k4:k4 + 4, :],
                         xa_wkv[k4 * 128:(k4 + 4) * 128, cb * 512:(cb + 1) * 512].rearrange("(k p) n -> p k n", p=128), [], ["wkvf%d" % k4])
                    m.cp("dve" if (k4 // 4) % 2 else "act", w_[:, k4:k4 + 4, :], wkvf[:, k4:k4 + 4, :], ["wkvf%d" % k4], [wk_])
            if cb < 4:
                for sub in range(4):
                    ps, pk = PS[sub], PSK[sub]
                    for k in range(KC):
                        m.mm(ps[:, 0:256], w_[:, k, sub * 128:(sub + 1) * 128], mT[:, k, :], k == 0, k == KC - 1, [wk_, "mT"], [pk])
                    m.cp("dve" if sub % 2 else "act", KmT[:, cb * 4 + sub, :], ps[:, 0:256], [pk], ["KmT"])
            else:
                for ti in range(2):
                    ps, pk = PS[ti], PSK[ti]
                    for k in range(KC):
                        m.mm(ps[:, :], mT[:, k, ti * 128:(ti + 1) * 128], w_[:, k, :], k == 0, k == KC - 1, [wk_, "mT"], [pk])
                    m.cp("dve" if ti else "act", Vm[:, ti, (cb - 4) * 512:(cb - 3) * 512], ps[:, :], [pk], ["Vm"])
        pm.close()
        wq_ = load_w_bf(ph, "xwq", xa_wq, D)
        xT = [ph.sb("e2xT%d" % i, [128, KC, 512], BF16) for i in range(2)]
        qx = ph.sb("qx", [128, KC, 512], BF16)
        pT = [ph.sb("e2pT%d" % i, [128, 2, 512], BF16) for i in range(2)]
        rinv = ph.sb("e2rinv", [128, 512], F32)
        ox = [ph.sb("e2ox0", [128, KC, 512], BF16)] * 2
        sc_ = 512 ** -0.5
        for gi in range(8):
            i2 = gi % 2
            xk = "e2xT%d" % i2
            m.ld("sp", xT[i2][:], x1T_d.rearrange("c p t -> p c t")[:, :, gi * 512:(gi + 1) * 512], [], [xk])
            for oc in range(KC):
                ps, pk = PS[oc % 4], PSK[oc % 4]
                for k in range(KC):
                    m.mm(ps[:, :], wq_[:, k, oc * 128:(oc + 1) * 128], xT[i2][:, k, :], k == 0, k == KC - 1, ["xwq", xk], [pk])
                m.cp("dve" if oc % 2 else "act", qx[:, oc, :], ps[:, :], [pk], ["qx%d" % oc])
            for hd in range(4):
                p_ = pT[hd % 2]; ppk = "e2pT%d" % (hd % 2)
                for mt in range(2):
                    ps, pk = PS[mt], PSK[mt]
                    for kk in range(4):
                        m.mm(ps[:, :], KmT[:, hd * 4 + kk, mt * 128:(mt + 1) * 128], qx[:, hd * 4 + kk, :], kk == 0, kk == 3,
                             ["KmT", "qx%d" % (hd * 4 + kk)], [pk])
                    m.act(p_[:, mt, :], ps[:, :], AF.Exp, [pk], [ppk], scale=sc_)
                for mt in range(2):
                    m.mm(PS[2][:, :], ones_b[:], p_[:, mt, :], mt == 0, mt == 1, ["ones_b", ppk], ["ps2"])
                m.op("dve", lambda e: e.reciprocal(out=rinv[:], in_=PS[2][:, :]), ["ps2"], ["e2rinv"])
                for dc in range(4):
                    ps, pk = PS[3 + dc % 3], PSK[3 + dc % 3]
                    for mt in range(2):
                        m.mm(ps[:, :], Vm[:, mt, (hd * 4 + dc) * 128:(hd * 4 + dc + 1) * 128], p_[:, mt, :], mt == 0, mt == 1, ["Vm", ppk], [pk])
                    m.tt("dve", ox[i2][:, hd * 4 + dc, :], ps[:, :], rinv[:], ALU.mult, [pk, "e2rinv"], ["e2ox0"])
            m.ld("pool", oxT_d.rearrange("c p t -> p c t")[:, :, gi * 512:(gi + 1) * 512], ox[i2][:], ["e2ox0"], [])
        ph.close()

    if "E2" in phases:
        phase_E2()

    def phase_E3():
        ph = Phase()
        tm = ln_tmps(ph)
        wo_ = load_w_bf(ph, "xwo", xa_wo, D)
        load_ln(6)
        rws = ph.sb("rws", [128, KC, 36], F32)
        rbs = ph.sb("rbs", [128, 36], F32)
        ut = ph.sb("ut", [128, 128], BF16)
        ecs = ph.sb("ecs", [128, 32], F32)
        carry = ph.sb("carry", [128, 32], F32)
        m.ld("sp", rws[:], rw.rearrange("(c p) j -> p c j", p=128), [], ["rws"])
        m.ld("sp", rbs[:], rb.broadcast_to([128, 36]), [], ["rbs"])
        m.ld("pool", ut[:], ut_c, [], ["ut"])
        m.ld("sp", ecs[:], ec_c, [], ["ecs"])
        m.op("dve", lambda e: e.memset(carry[:], 0.0), [], ["carry"])
        oT = [ph.sb("e3oT%d" % i, [128, KC, 128], BF16) for i in range(2)]
        x1 = [ph.sb("e3x1%d" % i, [128, D], F32) for i in range(2)]
        y = [ph.sb("e3y%d" % i, [128, D], F32) for i in range(2)]
        yb_ = [ph.sb("e3yb%d" % i, [128, D], BF16) for i in range(2)]
        yT = ph.sb("e3yT", [128, KC, 128], F32)
        lg = ph.sb("lg", [128, 36], F32)
        sm = ph.sb("sm", [128, 16], F32)
        oh = ph.sb("oh", [128, 4, 32], F32)
        ohb = ph.sb("ohb", [128, 32], BF16)
        lem = ph.sb("lem", [128, 32], F32)
        mx8 = ph.sb("mx8", [128, 8], F32)
        rk = ph.sb("rk", [128, 32], F32)
        rto = [ph.sb("rto%d" % i, [128, 4], F32) for i in range(2)]
        dsti = [ph.sb("dsti%d" % i, [128, 2], I32) for i in range(2)]
        def e3_part2a(lt):
            i2 = lt % 2
            yk = "e3y%d" % i2
            for k0 in range(0, KC, 4):
                ps, pk = PS[4 + (k0 // 4) % 2], PSK[4 + (k0 // 4) % 2]
                for kk in range(4):
                    m.tr(ps[:, kk * 128:(kk + 1) * 128], y[i2][:, (k0 + kk) * 128:(k0 + kk + 1) * 128], ident_f[:], [yk, "ident_f"], [pk])
                m.cp("act", yT[:, k0:k0 + 4, :], ps[:, :].rearrange("p (k t) -> p k t", k=4), [pk], ["e3yT"])

        def e3_part2(lt):
            i2 = lt % 2
            yk = "e3y%d" % i2
            for k in range(KC):
                m.mm(PS[4][:, 0:36], yT[:, k, :], rws[:, k, :], k == 0, k == KC - 1, ["e3yT", "rws"], ["ps4"])
            m.tt("dve", lg[:], PS[4][:, 0:36], rbs[:], ALU.add, ["ps4", "rbs"], ["lg"])
            m.op("dve", lambda e: e.tensor_reduce(out=sm[:, 0:1], in_=lg[:, 0:4], axis=AX.X, op=ALU.max), ["lg"], ["sm"])
            m.ts("dve", sm[:, 1:2], sm[:, 0:1], -1.0, None, ALU.mult, None, ["sm"], ["sm"])
            m.act(sm[:, 4:8], lg[:, 0:4], AF.Exp, ["lg", "sm"], ["sm"], bias=sm[:, 1:2], accum=sm[:, 2:3])
            m.op("dve", lambda e: e.reciprocal(out=sm[:, 3:4], in_=sm[:, 2:3]), ["sm"], ["sm"])
            m.ts("dve", sm[:, 8:12], lg[:, 0:4], sm[:, 0:1], None, ALU.is_ge, None, ["lg", "sm"], ["sm"])
            m.ts("dve", oh[:, 0, :].rearrange("p (g e) -> p g e", g=4), sm[:, 8:12].unsqueeze(2).broadcast_to([128, 4, 8]), 1.0, 1e4,
                 ALU.subtract, ALU.mult, ["sm"], ["oh"])
            m.tt("dve", lem[:], lg[:, 4:36], oh[:, 0, :], ALU.add, ["lg", "oh"], ["lem"])
            m.op("dve", lambda e: e.max(out=mx8[:], in_=lem[:]), ["lem"], ["mx8"])
            m.ts("dve", oh[:, 1, :], lem[:], mx8[:, 0:1], None, ALU.is_ge, None, ["lem", "mx8"], ["oh"])
            m.ts("dve", oh[:, 3, :], lem[:], mx8[:, 1:2], None, ALU.is_ge, None, ["lem", "mx8"], ["oh"])
            m.tt("dve", oh[:, 2, :], oh[:, 3, :], oh[:, 1, :], ALU.subtract, ["oh"], ["oh"])
            m.cp("dve", ohb[:], oh[:, 3, :], ["oh"], ["ohb"])
            m.tt("dve", sm[:, 12:13], mx8[:, 1:2], mx8[:, 0:1], ALU.subtract, ["mx8"], ["sm"])
            m.act(sm[:, 13:14], sm[:, 12:13], AF.Exp, ["sm"], ["sm"])
            m.ts("dve", sm[:, 14:15], sm[:, 13:14], 1.0, None, ALU.add, None, ["sm"], ["sm"])
            m.op("dve", lambda e: e.reciprocal(out=sm[:, 14:15], in_=sm[:, 14:15]), ["sm"], ["sm"])
            r_ = rto[i2]; rk_ = "rto%d" % i2
            m.tt("dve", r_[:, 2:3], sm[:, 14:15], sm[:, 3:4], ALU.mult, ["sm"], [rk_])
            m.tt("dve", r_[:, 3:4], sm[:, 13:14], r_[:, 2:3], ALU.mult, ["sm", rk_], [rk_])
            m.mm(PS[5][:, 0:32], ut[:], ohb[:], True, True, ["ut", "ohb"], ["ps5"])
            m.tt("dve", rk[:], PS[5][:, 0:32], carry[:], ALU.add, ["ps5", "carry"], ["rk"])
            m.tt("dve", rk[:], rk[:], ecs[:], ALU.add, ["rk", "ecs"], ["rk"])
            m.mm(PS[5][:, 32:64], ones_b[:], ohb[:], True, True, ["ones_b", "ohb"], ["ps5"])
            m.tt("dve", carry[:], carry[:], PS[5][:, 32:64], ALU.add, ["ps5", "carry"], ["carry"])
            for kq in range(2):
                m.tt("dve", oh[:, 0, :], oh[:, 1 + kq, :], rk[:], ALU.mult, ["oh", "rk"], ["oh"])
                m.op("dve", lambda e, kq=kq, r_=r_: e.tensor_reduce(out=r_[:, kq:kq + 1], in_=oh[:, 0, :], axis=AX.X, op=ALU.add), ["oh"], [rk_])
            d_ = dsti[i2]; dk = "dsti%d" % i2
            m.cp("dve", d_[:], r_[:, 0:2], [rk_], [dk])
            m.ld("pool", rt_d[lt * 128:(lt + 1) * 128, :], r_[:], [rk_], [])
            for kq in range(2):
                m.dma("pool", lambda e, kq=kq, d_=d_, yy=yb_[i2]: e.indirect_dma_start(
                    out=xs_d, out_offset=bass.IndirectOffsetOnAxis(ap=d_[:, kq:kq + 1], axis=0), in_=yy[:], in_offset=None),
                    ["e3yb%d" % i2, dk], ["xs_d"])

        for lt in range(NT):
            i2 = lt % 2
            m.ld("sp", oT[i2][:], oxT_d.rearrange("c p t -> p c t")[:, :, lt * 128:(lt + 1) * 128], [], ["e3oT%d" % i2])
            m.ld("sp", x1[i2][:], x1_d[lt * 128:(lt + 1) * 128, :], [], ["e3x1%d" % i2])
            if lt > 0:
                e3_part2a(lt - 1)
            for oc in range(4):
                ps, pk = PS[oc], PSK[oc]
                for k in range(KC):
                    m.mm(ps[:, :], oT[i2][:, k, :], wo_[:, k, oc * 512:(oc + 1) * 512], k == 0, k == KC - 1, ["e3oT%d" % i2, "xwo"], [pk])
                m.stt(y[i2][:, oc * 512:(oc + 1) * 512], x1[i2][:, oc * 512:(oc + 1) * 512], ALPHA, ps[:, :], ALU.mult, ALU.add,
                      ["e3x1%d" % i2, pk], ["e3y%d" % i2])
            yk = "e3y%d" % i2
            ln_generic(tm, y[i2][:], [yk], lnrep, "lnrep", last_eng="dve")
            m.ld("pool", x2_d[lt * 128:(lt + 1) * 128, :], y[i2][:], [yk], [])
            m.cp("dve", yb_[i2][:], y[i2][:], [yk], ["e3yb%d" % i2])
            if lt > 0:
                e3_part2(lt - 1)
        e3_part2a(NT - 1)
        e3_part2(NT - 1)
        ph.close()

    if "E3" in phases:
        phase_E3()

    def phase_F():
        ph = Phase()
        wg_ = [ph.sb("fwg%d" % i, [128, KC, 512], BF16) for i in range(2)]
        wu_ = [ph.sb("fwu%d" % i, [128, KC, 512], BF16) for i in range(2)]
        wd_ = [ph.sb("fwd%d" % i, [128, 4, D], BF16) for i in range(2)]
        xr = [ph.sb("fxr%d" % i, [128, D], BF16) for i in range(2)]
        xsT = [ph.sb("fxsT%d" % i, [128, KC, CAP], BF16) for i in range(2)]
        hT = ph.sb("fhT", [128, 4, CAP], BF16)
        sg = [ph.sb("fsg%d" % i, [128, CAP], F32) for i in range(2)]
        yo = [ph.sb("fyo%d" % i, [128, D], F32) for i in range(2)]
        wdf = ph.sb("fwdf", [128, 2, D], F32)
        cnt = {"i": 0}

        def f_loadw(e_):
            i2 = e_ % 2
            for k8 in range(0, KC, 8):
                m.ld("pool", wg_[i2][:, k8:k8 + 8, :], mwg[e_, k8 * 128:(k8 + 8) * 128, :].rearrange("(k p) n -> p k n", p=128), [], ["fwg%d" % i2])
            for k8 in range(0, KC, 8):
                m.ld("pool", wu_[i2][:, k8:k8 + 8, :], mwu[e_, k8 * 128:(k8 + 8) * 128, :].rearrange("(k p) n -> p k n", p=128), [], ["fwu%d" % i2])
            for k2 in range(0, 4, 2):
                for kk in range(2):
                    m.ld("sp", wdf[:, kk, :], mwd[e_, (k2 + kk) * 128:(k2 + kk + 1) * 128, :], [], ["fwdf%d" % kk])
                    m.cp("pool", wd_[i2][:, k2 + kk, :], wdf[:, kk, :], ["fwdf%d" % kk], ["fwd%d" % i2])

        def f_xsT(e_):
            i2 = e_ % 2
            for rt in range(4):
                x_ = xr[rt % 2]; xk = "fxr%d" % (rt % 2)
                m.ld("sp", x_[:], xs_d[e_ * CAP + rt * 128:e_ * CAP + (rt + 1) * 128, :], ["xs_d"], [xk])
                transpose_to(xsT[i2], "fxsT%d" % i2, x_, [xk], rt, only="dve")

        f_loadw(0)
        f_xsT(0)
        for e_ in range(NE):
            i2 = e_ % 2
            if e_ + 1 < NE:
                f_loadw(e_ + 1)
            for hc in range(4):
                pg, pgk = PS[(hc % 2) * 2], PSK[(hc % 2) * 2]
                pu, puk = PS[(hc % 2) * 2 + 1], PSK[(hc % 2) * 2 + 1]
                for k in range(KC):
                    m.mm(pg[:, :], wg_[i2][:, k, hc * 128:(hc + 1) * 128], xsT[i2][:, k, :], k == 0, k == KC - 1, ["fwg%d" % i2, "fxsT%d" % i2], [pgk])
                for k in range(KC):
                    m.mm(pu[:, :], wu_[i2][:, k, hc * 128:(hc + 1) * 128], xsT[i2][:, k, :], k == 0, k == KC - 1, ["fwu%d" % i2, "fxsT%d" % i2], [puk])
                s_ = sg[hc % 2]; sk = "fsg%d" % (hc % 2)
                m.act(s_[:], pg[:, :], AF.Silu, [pgk], [sk])
                m.tt("dve", hT[:, hc, :], pu[:, :], s_[:], ALU.mult, [puk, sk], ["fhT%d" % hc])
            if e_ + 1 < NE:
                f_xsT(e_ + 1)
            for rt in range(4):
                y_ = yo[rt % 2]; yk = "fyo%d" % (rt % 2)
                for oc in range(4):
                    pi = 4 + cnt["i"] % 2; cnt["i"] += 1
                    ps, pk = PS[pi], PSK[pi]
                    for hc in range(4):
                        m.mm(ps[:, :], hT[:, hc, rt * 128:(rt + 1) * 128], wd_[i2][:, hc, oc * 512:(oc + 1) * 512], hc == 0, hc == 3,
                             ["fhT%d" % hc, "fwd%d" % i2], [pk])
                    m.cp("dve", y_[:, oc * 512:(oc + 1) * 512], ps[:, :], [pk], [yk])
                r0 = e_ * CAP + rt * 128
                m.ld("act", ys_d[r0:r0 + 128, :], y_[:], [yk], ["ys_d"])
        ph.close()

    if "F" in phases:
        phase_F()

    def phase_G():
        ph = Phase()
        tm = ln_tmps(ph)
        load_ln(8)
        NB = 4
        x2 = [ph.sb("gx%d" % i, [128, D], F32) for i in range(NB)]
        y1 = [ph.sb("gy1%d" % i, [128, D], F32) for i in range(NB)]
        y2 = [ph.sb("gy2%d" % i, [128, D], F32) for i in range(NB)]
        rto = [ph.sb("grt%d" % i, [128, 4], F32) for i in range(NB)]
        dsti = [ph.sb("gds%d" % i, [128, 2], I32) for i in range(NB)]

        def issue(lt):
            i2 = lt % NB
            m.ld("sp", x2[i2][:], x2_d[lt * 128:(lt + 1) * 128, :], [], ["gx%d" % i2])
            m.ld("sp", rto[i2][:], rt_d[lt * 128:(lt + 1) * 128, :], [], ["grt%d" % i2])
            m.cp("dve", dsti[i2][:], rto[i2][:, 0:2], ["grt%d" % i2], ["gds%d" % i2])
            for kq, yy in enumerate((y1[i2], y2[i2])):
                yk = "gy%d%d" % (kq + 1, i2)
                m.dma("pool", lambda e, kq=kq, yy=yy, d_=dsti[i2]: e.indirect_dma_start(
                    out=yy[:], out_offset=None, in_=ys_d, in_offset=bass.IndirectOffsetOnAxis(ap=d_[:, kq:kq + 1], axis=0)),
                    ["ys_d", "gds%d" % i2], [yk])

        issue(0)
        issue(1)
        for lt in range(NT):
            i2 = lt % NB
            if lt + 2 < NT:
                issue(lt + 2)
            xk = "gx%d" % i2
            m.act(x2[i2][:], x2[i2][:], AF.Copy, [xk], [xk], scale=ALPHA)
            m.stt(x2[i2][:], y1[i2][:], rto[i2][:, 2:3], x2[i2][:], ALU.mult, ALU.add, ["gy1%d" % i2, "grt%d" % i2, xk], [xk])
            m.stt(x2[i2][:], y2[i2][:], rto[i2][:, 3:4], x2[i2][:], ALU.mult, ALU.add, ["gy2%d" % i2, "grt%d" % i2, xk], [xk])
            ln_generic(tm, x2[i2][:], [xk], lnrep, "lnrep", last_eng="dve")
            m.ld("sp", out[lt * 128:(lt + 1) * 128, :], x2[i2][:], [xk], [])
        ph.close()

    if "G" in phases:
        phase_G()

    m.finish()
    return nc, m


def _t5_bucket(dist):
    n = np.maximum(dist, 0)
    nf = np.maximum(n, 1).astype(np.float32)
    large = 16 + (np.log(nf / np.float32(16)) / np.float32(math.log(8.0)) * np.float32(16)).astype(np.int32)
    large = np.minimum(large, 31)
    return np.where(n < 16, n, large).astype(np.int64)


def _prep(inputs):
    f = lambda a: np.ascontiguousarray(np.asarray(a, dtype=np.float32))
    x = f(inputs["x"]); mem = f(inputs["mem"])
    w_in = f(inputs["w_in"])[0]
    sizes = [1024] + [256] * 6 + [48, 2048]
    cuts = np.cumsum([0] + sizes)
    sec = [w_in[:, cuts[i]:cuts[i + 1]] for i in range(9)]
    q, k_c, v_c, k_s, v_s, k_w, v_w, g, u = sec
    w_inp = np.ascontiguousarray(np.concatenate([k_c, v_c, k_s, k_w, v_s, v_w, q, g, u], axis=1))
    lnv = np.stack([f(inputs["ln_in_g"]), f(inputs["ln_in_b"]), f(inputs["ln1_g"])[0], f(inputs["ln1_b"])[0],
                    f(inputs["mem_ln_g"])[0], f(inputs["mem_ln_b"])[0], f(inputs["ln2_g"])[0], f(inputs["ln2_b"])[0],
                    f(inputs["ln3_g"])[0], f(inputs["ln3_b"])[0]])

    def w1l(w):
        return np.ascontiguousarray(f(w)[0].reshape(32, 64, 256).transpose(1, 0, 2).reshape(64, 32 * 256))

    rel = f(inputs["rel_table"])
    kk = np.arange(128)[:, None]; qq = np.arange(128)[None, :]
    d_diag = qq - kk
    diag_raw = rel[_t5_bucket(d_diag)]
    diag_raw = np.ascontiguousarray(diag_raw.transpose(0, 2, 1).reshape(128, 16 * 128))
    diag_mask = np.where(d_diag >= 0, 0.0, NEG).astype(np.float32)
    sub1_raw = np.ascontiguousarray(rel[_t5_bucket(128 + qq - kk)].transpose(0, 2, 1).reshape(128, 16 * 128))
    ii = np.arange(16)[:, None]
    d_band = qq - 16 * ii + 113
    band_raw = np.ascontiguousarray(rel[_t5_bucket(d_band)].transpose(0, 2, 1).reshape(16, 16 * 128))
    band_mask = np.where(d_band >= 0, 0.0, NEG).astype(np.float32)
    anti_mask = np.where(kk > qq, 0.0, NEG).astype(np.float32)
    r31 = np.ascontiguousarray(np.broadcast_to(rel[31][None, :], (128, 16)))
    dw_c = np.zeros((16, 272), np.float32)
    dw_c[np.arange(16), 128 + np.arange(16)] = 1.0
    ident = np.eye(128, dtype=np.float32)
    ut = np.triu(np.ones((128, 128), np.float32), 1)
    cs = np.arange(512) * 16; ce = cs + 32
    ss = np.arange(128) * 64; se = ss + 64
    ov = (np.clip(np.minimum(ce[:, None], se[None, :]) - np.maximum(cs[:, None], ss[None, :]), 0, None) / 32.0).astype(np.float32)
    ov[511] = 0.0
    ec = np.ascontiguousarray(np.broadcast_to((np.arange(32) * CAP).astype(np.float32)[None, :], (128, 32)))
    asel = np.zeros((64, SV), np.float32)
    kcol = np.arange(SV)
    asel[(2 * (kcol // 128) + (kcol % 128) // 64) % 64, kcol] = 1.0
    band3 = np.zeros((128, 3), np.float32)
    band3[:64, 0] = 100.0; band3[:, 1] = 100.0; band3[64:, 2] = 100.0; band3[:64, 2] = -100.0
    dww = np.ascontiguousarray(f(inputs["conv_dw_w"])[0][:, 0, :].reshape(31, 8, 128).transpose(2, 1, 0).reshape(128, 8 * 31))
    cvec = np.concatenate([f(inputs["conv_dw_b"])[0].reshape(8, 128).T, f(inputs["conv_ln_g"])[0].reshape(8, 128).T,
                           f(inputs["conv_ln_b"])[0].reshape(8, 128).T], axis=1)
    cvec = np.ascontiguousarray(cvec)
    rwm = np.ascontiguousarray(np.concatenate([f(inputs["router_group_w"])[0], f(inputs["router_expert_w"])[0]], axis=1))
    rbm = np.concatenate([f(inputs["router_group_b"])[0], f(inputs["router_expert_b"])[0]])[None, :]
    common = dict(
        lnv=np.ascontiguousarray(lnv), w_inp=w_inp,
        w1k=w1l(inputs["cmp_w1_k"]), w1v=w1l(inputs["cmp_w1_v"]),
        pek=np.ascontiguousarray(f(inputs["cmp_pe_k"])[0].T), pev=np.ascontiguousarray(f(inputs["cmp_pe_v"])[0].T),
        w2k=f(inputs["cmp_w2_k"])[0], w2v=f(inputs["cmp_w2_v"])[0],
        diag_raw=diag_raw, sub1_raw=sub1_raw, band_raw=band_raw, r31=r31,
        diag_mask=diag_mask, anti_mask=anti_mask, band_mask=band_mask,
        dw_c=dw_c, asel_c=asel, ident_c=ident, ut_c=ut, ov_c=ov, ec_c=ec, band3_c=band3,
        dww=dww, cvec=cvec,
        w_out=f(inputs["w_out"])[0], xa_wq=f(inputs["xa_wq"])[0], xa_wkv=f(inputs["xa_wkv"])[0], xa_wo=f(inputs["xa_wo"])[0],
        rw=rwm, rb=np.ascontiguousarray(rbm),
        mwg=f(inputs["moe_w_gate"])[0], mwu=f(inputs["moe_w_up"])[0], mwd=f(inputs["moe_w_down"])[0],
    )
    in_maps = []
    for c in range(8):
        b, s = c // 2, c % 2
        if s == 1:
            xvv = x[b]
        else:
            xvv = np.ascontiguousarray(np.concatenate([x[b, OWN:], x[b, :OWN]], axis=0))
        keyb = np.zeros((128, 64), np.float32)
        cmpb = np.zeros((128, 4), np.float32)
        f0 = np.zeros((128, 128), np.float32)
        if s == 0:
            keyb[:, :32] = NEG
            cmpb[:, :2] = NEG
            f0[:, 64] = 100.0
        else:
            f0[:, 0] = 100.0
        cmpb[127, 3] = NEG
        halo = np.full((128, 1), float(s), np.float32)
        d = dict(common)
        d.update(xv=xvv, memb=mem[b], keybias_c=keyb, cmpbias_c=cmpb, f0_c=f0, halo_c=halo)
        in_maps.append(d)
    return in_maps


_CACHE = {}


def kernel(**inputs):
    in_maps = _prep(inputs)
    if "nc" not in _CACHE:
        _CACHE["nc"] = build()[0]
    res = run_bass_kernel_spmd(_CACHE["nc"], in_maps, core_ids=list(range(8)))
    outp = np.zeros((4, SV, D), np.float32)
    for c in range(8):
        b, s = c // 2, c % 2
        outp[b, s * OWN:(s + 1) * OWN] = res.results[c]["out"]
    return outp
```

```python
import os
import math
from contextlib import ExitStack
import numpy as np
import concourse.bass as bass
import concourse.mybir as mybir
from concourse.bass_utils import run_bass_kernel_spmd

F32 = mybir.dt.float32
BF16 = mybir.dt.bfloat16
I32 = mybir.dt.int32
AF = mybir.ActivationFunctionType
ALU = mybir.AluOpType
AX = mybir.AxisListType

SEM_LIMIT = 30000
N_DMA_SLOTS = 12

D = 2048
KC = 16
SV = 8192
OWN = 4096
NT = 32
ALPHA = 2 ** 0.25
EPS = 1e-5
NEG = -30000.0
CAP = 512
NE = 32
O_KF, O_VT, O_Q, O_G, O_U = 0, 1024, 1536, 2560, 2608
PH_ALL = "A1,A2,B,C,D,E1,M,E2,E3,F,G"


class MK:
    ENG = ("pe", "act", "dve", "pool", "sp")

    def __init__(self, nc):
        self.nc = nc
        self.streams = {e: [] for e in self.ENG}
        self.cur_sem = {}
        self.cur_cnt = {}
        for e in ("pe", "act", "dve", "pool"):
            self.cur_sem[e] = nc.alloc_semaphore("s_" + e + "0")
            self.cur_cnt[e] = 0
        self.nsem = 4
        self.slots = {}
        self.slot_rr = {}
        for q in ("sp", "pool", "act"):
            self.slots[q] = [[nc.alloc_semaphore("d_%s%d" % (q, i)), 0] for i in range(N_DMA_SLOTS)]
            self.slot_rr[q] = 0
        self.seen = {e: {} for e in self.ENG}
        self.state = {}
        self.all_events = {}
        self.n_inst = {e: 0 for e in self.ENG}

    def _need(self, eng, reads, writes):
        need = {}

        def add(ev):
            if ev is None:
                return
            sem, val = ev
            k = id(sem)
            if k not in need or need[k][1] < val:
                need[k] = (sem, val)

        for k in reads:
            st = self.state.get(k)
            if st:
                add(st[0])
        for k in writes:
            st = self.state.get(k)
            if st:
                add(st[0])
                for ev in st[1].values():
                    add(ev)
        out = []
        for k, (sem, val) in need.items():
            if eng == "pe" and sem is self.cur_sem["pe"]:
                continue
            if self.seen[eng].get(k, 0) < val:
                self.seen[eng][k] = val
                out.append((sem, val))
        return out

    def _emit_waits(self, eng, waits):
        for sem, val in waits:
            self.streams[eng].append(lambda e, sem=sem, val=val: e.wait_ge(sem, val))

    def _commit(self, ev, reads, writes):
        sem, val = ev
        self.all_events[id(sem)] = ev
        for k in reads:
            st = self.state.setdefault(k, [None, {}])
            st[1][id(sem)] = ev
        for k in writes:
            self.state[k] = [ev, {}]

    def op(self, eng, fn, reads=(), writes=()):
        waits = self._need(eng, reads, writes)
        self._emit_waits(eng, waits)
        if self.cur_cnt[eng] >= SEM_LIMIT:
            self.cur_sem[eng] = self.nc.alloc_semaphore("s_%s%d" % (eng, self.nsem))
            self.nsem += 1
            self.cur_cnt[eng] = 0
        self.cur_cnt[eng] += 1
        sem, val = self.cur_sem[eng], self.cur_cnt[eng]
        self.streams[eng].append(lambda e, sem=sem: fn(e).then_inc(sem, 1))
        self.n_inst[eng] += 1
        self._commit((sem, val), reads, writes)

    def dma(self, q, fn, reads=(), writes=()):
        slot = self.slots[q][self.slot_rr[q]]
        self.slot_rr[q] = (self.slot_rr[q] + 1) % N_DMA_SLOTS
        sem, prev = slot
        waits = self._need(q, reads, writes)
        k = id(sem)
        if self.seen[q].get(k, 0) < prev:
            self.seen[q][k] = prev
            waits.append((sem, prev))
        self._emit_waits(q, waits)
        val = prev + 16
        slot[1] = val
        self.streams[q].append(lambda e, sem=sem: fn(e).then_inc(sem, 16))
        self.n_inst[q] += 1
        self._commit((sem, val), reads, writes)

    def barrier(self):
        evs = list(self.all_events.values())
        for eng in self.ENG:
            for sem, val in evs:
                k = id(sem)
                if eng == "pe" and sem is self.cur_sem["pe"]:
                    continue
                if self.seen[eng].get(k, 0) < val:
                    self.seen[eng][k] = val
                    self.streams[eng].append(lambda e, sem=sem, val=val: e.wait_ge(sem, val))
        self.state = {}

    def finish(self):
        self.barrier()
        nc = self.nc
        with nc.Block() as block:
            @block.tensor
            def _(e):
                for f in self.streams["pe"]:
                    f(e)

            @block.scalar
            def _(e):
                for f in self.streams["act"]:
                    f(e)

            @block.vector
            def _(e):
                for f in self.streams["dve"]:
                    f(e)

            @block.gpsimd
            def _(e):
                for f in self.streams["pool"]:
                    f(e)

            @block.sync
            def _(e):
                for f in self.streams["sp"]:
                    f(e)

    def mm(self, out, lhsT, rhs, start, stop, r, w, skip=False):
        self.op("pe", lambda e: e.matmul(out, lhsT=lhsT, rhs=rhs, start=start, stop=stop,
                                         skip_group_check=skip), r, w)

    def tr(self, out, in_, ident, r, w):
        self.op("pe", lambda e: e.transpose(out, in_, ident), r, w)

    def act(self, out, in_, func, r, w, bias=None, scale=None, accum=None):
        kw = {}
        if bias is not None:
            kw["bias"] = bias
        if scale is not None:
            kw["scale"] = scale
        if accum is not None:
            kw["accum_out"] = accum
        self.op("act", lambda e: e.activation(out=out, in_=in_, func=func, **kw), r, w)

    def tt(self, eng, out, in0, in1, op, r, w):
        self.op(eng, lambda e: e.tensor_tensor(out=out, in0=in0, in1=in1, op=op), r, w)

    def ts(self, eng, out, in0, s1, s2, op0, op1, r, w):
        if op1 is None:
            self.op(eng, lambda e: e.tensor_scalar(out=out, in0=in0, scalar1=s1, scalar2=None, op0=op0), r, w)
        else:
            self.op(eng, lambda e: e.tensor_scalar(out=out, in0=in0, scalar1=s1, scalar2=s2, op0=op0, op1=op1), r, w)

    def stt(self, out, in0, scalar, in1, op0, op1, r, w):
        self.op("dve", lambda e: e.scalar_tensor_tensor(out=out, in0=in0, scalar=scalar, in1=in1, op0=op0, op1=op1), r, w)

    def cp(self, eng, out, in_, r, w):
        if eng == "act":
            self.op("act", lambda e: e.activation(out=out, in_=in_, func=AF.Copy), r, w)
        else:
            self.op(eng, lambda e: e.tensor_copy(out=out, in_=in_), r, w)

    def ld(self, q, out, in_, r, w):
        self.dma(q, lambda e: e.dma_start(out=out, in_=in_), r, w)


def build(phases=PH_ALL, debug=()):
    phases = phases.split(",")
    nc = bass.Bass("TRN2", target_bir_lowering=False)
    m = MK(nc)

    def din(name, shape, dt=F32):
        return nc.dram_tensor(name, list(shape), dt, kind="ExternalInput").ap()

    def dscr(name, shape, dt):
        return nc.dram_tensor(name, list(shape), dt, kind="ExternalOutput" if name in debug else "Internal").ap()

    xv = din("xv", [SV, D])
    memb = din("memb", [256, D])
    lnv = din("lnv", [10, D])
    w_inp = din("w_inp", [D, 4656])
    w1k = din("w1k", [64, 32 * 256]); w1v = din("w1v", [64, 32 * 256])
    pek = din("pek", [64, 32]); pev = din("pev", [64, 32])
    w2k = din("w2k", [256, 64]); w2v = din("w2v", [256, 64])
    diag_raw = din("diag_raw", [128, 16 * 128]); sub1_raw = din("sub1_raw", [128, 16 * 128])
    band_raw = din("band_raw", [16, 16 * 128]); r31 = din("r31", [128, 16])
    diag_mask = din("diag_mask", [128, 128]); anti_mask = din("anti_mask", [128, 128]); band_mask = din("band_mask", [16, 128])
    dw_c = din("dw_c", [16, 272]); ident_c = din("ident_c", [128, 128]); ut_c = din("ut_c", [128, 128])
    ov_c = din("ov_c", [512, 128]); ec_c = din("ec_c", [128, 32]); f0_c = din("f0_c", [128, 128]); band3_c = din("band3_c", [128, 3])
    asel_c = din("asel_c", [64, SV]); keybias_c = din("keybias_c", [128, 64]); cmpbias_c = din("cmpbias_c", [128, 4]); halo_c = din("halo_c", [128, 1])
    dww = din("dww", [128, 8 * 31]); cvec = din("cvec", [128, 24])
    w_out = din("w_out", [D, D]); xa_wq = din("xa_wq", [D, D]); xa_wkv = din("xa_wkv", [D, 2 * D]); xa_wo = din("xa_wo", [D, D])
    rw = din("rw", [D, 36]); rb = din("rb", [1, 36])
    mwg = din("mwg", [NE, D, 512]); mwu = din("mwu", [NE, D, 512]); mwd = din("mwd", [NE, 512, D])
    out = nc.dram_tensor("out", [OWN, D], F32, kind="ExternalOutput").ap()

    kcmpT_d = dscr("kcmpT_d", [4, 64, SV], BF16); vcmpT_d = dscr("vcmpT_d", [4, 64, SV], BF16)
    kslcT_d = dscr("kslcT_d", [4, 64, SV], BF16); kwinT_d = dscr("kwinT_d", [4, 64, SV], BF16)
    vslc_d = dscr("vslc_d", [SV, 256], BF16); vwin_d = dscr("vwin_d", [SV, 256], BF16)
    qT_d = dscr("qT_d", [16, 64, OWN], BF16)
    gates_d = dscr("gates_d", [OWN, 48], F32)
    hconvT_d = dscr("hconvT_d", [8, 128, 128 + OWN], BF16)
    o_d = dscr("o_d", [OWN, 1024], BF16)
    oconvT_d = dscr("oconvT_d", [8, 128, OWN], BF16)
    x1_d = dscr("x1_d", [OWN, D], F32); x1T_d = dscr("x1T_d", [KC, 128, OWN], BF16)
    oxT_d = dscr("oxT_d", [KC, 128, OWN], BF16)
    x2_d = dscr("x2_d", [OWN, D], F32)
    xs_d = dscr("xs_d", [NE * CAP, D], BF16)
    ys_d = dscr("ys_d", [NE * CAP, D], F32)
    rt_d = dscr("rt_d", [OWN, 4], F32)

    ident_f = nc.alloc_sbuf_tensor("ident_f", [128, 128], F32)
    ident_b = nc.alloc_sbuf_tensor("ident_b", [128, 128], BF16)
    ones_b = nc.alloc_sbuf_tensor("ones_b", [128, 128], BF16)
    ones_f = nc.alloc_sbuf_tensor("ones_f", [128, 128], F32)
    lnrep = nc.alloc_sbuf_tensor("lnrep", [128, 2, D], F32)
    PS = [nc.alloc_psum_tensor("ps%d" % i, [128, 512], F32) for i in range(6)]
    PB = [nc.alloc_psum_tensor("pb%d" % i, [128, 1024], BF16) for i in range(2)]
    PSK = ["ps%d" % i for i in range(6)]
    PBK = ["pb%d" % i for i in range(2)]

    m.ld("sp", ident_f[:], ident_c, [], ["ident_f"])
    m.ld("pool", ident_b[:], ident_c, [], ["ident_b"])
    m.op("dve", lambda e: e.memset(ones_b[:], 1.0), [], ["ones_b"])
    m.op("dve", lambda e: e.memset(ones_f[:], 1.0), [], ["ones_f"])

    def load_ln(row):
        m.ld("sp", lnrep[:, 0, :], lnv[row:row + 1, :].broadcast_to([128, D]), [], ["lnrep"])
        m.ld("sp", lnrep[:, 1, :], lnv[row + 1:row + 2, :].broadcast_to([128, D]), [], ["lnrep"])

    uid = {"n": 0}

    class Phase:
        def __init__(self):
            self.es = ExitStack()
            self.n = 0

        def sb(self, name, shape, dt):
            uid["n"] += 1
            return self.es.enter_context(nc.sbuf_tensor("%s_%d" % (name, uid["n"]), list(shape), dt))

        def close(self):
            m.barrier()
            self.es.close()

    rr = {"i": 0}

    def layer_norm(ph_tmp, src, src_keys, dst, dst_keys, np_=128, last_eng="pool"):
        st, mv, rs, nb = ph_tmp
        for c in range(4):
            m.op("dve", lambda e, c=c: e.bn_stats(out=st[:np_, c, :], in_=src[:, c * 512:(c + 1) * 512]), src_keys, ["ln_st"])
        m.op("dve", lambda e: e.bn_aggr(out=mv[:np_, :], in_=st[:np_, :, :]), ["ln_st"], ["ln_mv"])
        m.act(rs[:np_, :], mv[:np_, 1:2], AF.Sqrt, ["ln_mv"], ["ln_rs"], bias=EPS)
        m.op("dve", lambda e: e.reciprocal(out=rs[:np_, :], in_=rs[:np_, :]), ["ln_rs"], ["ln_rs"])
        m.stt(nb[:np_, :], mv[:np_, 0:1], -1.0, rs[:np_, :], ALU.mult, ALU.mult, ["ln_mv", "ln_rs"], ["ln_nb"])
        m.act(src, src, AF.Identity, src_keys + ["ln_rs", "ln_nb"], src_keys, bias=nb[:np_, :], scale=rs[:np_, :])
        m.tt("dve", src, src, lnrep[:np_, 0, :], ALU.mult, src_keys + ["lnrep"], src_keys)
        m.tt(last_eng, dst, src, lnrep[:np_, 1, :], ALU.add, src_keys + ["lnrep"], dst_keys)

    def ln_tmps(ph):
        return (ph.sb("ln_st", [128, 4, 6], F32), ph.sb("ln_mv", [128, 2], F32),
                ph.sb("ln_rs", [128, 1], F32), ph.sb("ln_nb", [128, 1], F32))

    def transpose_to(hT, hT_key, src_bf, src_keys, ntile_idx, nk=KC, col0=0, only=None):
        for k0 in range(0, nk, 8):
            pi = rr["i"] % 2
            rr["i"] += 1
            pb = PB[pi]
            n8 = min(8, nk - k0)
            for kk in range(n8):
                m.tr(pb[:, kk * 128:(kk + 1) * 128], src_bf[:, (k0 + kk) * 128:(k0 + kk + 1) * 128], ident_b[:],
                     src_keys + ["ident_b"], [PBK[pi]])
            eng = only or ("dve" if (rr["i"] % 2) else "act")
            m.cp(eng, hT[:, k0:k0 + n8, col0 + ntile_idx * 128: col0 + (ntile_idx + 1) * 128],
                 pb[:, 0:n8 * 128].rearrange("p (k t) -> p k t", k=n8), [PBK[pi]], [hT_key])

    def phase_A(own_pass):
        ph = Phase()
        tm = ln_tmps(ph)
        load_ln(0)
        if not own_pass:
            wkf = ph.sb("wkf", [128, KC, 1024], BF16)
            wvt = ph.sb("wvt", [128, KC, 512], BF16)
            for k4 in range(0, KC, 4):
                m.ld("pool", wkf[:, k4:k4 + 4, :], w_inp[k4 * 128:(k4 + 4) * 128, O_KF:O_KF + 1024].rearrange("(k p) n -> p k n", p=128), [], ["wkf"])
                m.ld("pool", wvt[:, k4:k4 + 4, :], w_inp[k4 * 128:(k4 + 4) * 128, O_VT:O_VT + 512].rearrange("(k p) n -> p k n", p=128), [], ["wvt"])
        else:
            wq = ph.sb("wq", [128, KC, 1024], BF16)
            wg = ph.sb("wg", [128, KC, 48], BF16)
            wu = ph.sb("wu", [128, KC, 2048], BF16)
            hal = ph.sb("hal", [128, 1], F32)
            m.ld("sp", hal[:], halo_c, [], ["hal"])
            for k4 in range(0, KC, 4):
                m.ld("pool", wq[:, k4:k4 + 4, :], w_inp[k4 * 128:(k4 + 4) * 128, O_Q:O_Q + 1024].rearrange("(k p) n -> p k n", p=128), [], ["wq"])
                m.ld("pool", wg[:, k4:k4 + 4, :], w_inp[k4 * 128:(k4 + 4) * 128, O_G:O_G + 48].rearrange("(k p) n -> p k n", p=128), [], ["wg"])
                m.ld("pool", wu[:, k4:k4 + 4, :], w_inp[k4 * 128:(k4 + 4) * 128, O_U:O_U + 2048].rearrange("(k p) n -> p k n", p=128), [], ["wu"])
        xt = [ph.sb("xt%d" % i, [128, D], F32) for i in range(2)]
        hb = [ph.sb("hb%d" % i, [128, D], BF16) for i in range(4)]
        hT = [ph.sb("hT%d" % i, [128, KC, 512], BF16) for i in range(2)]
        stg = [ph.sb("stg%d" % i, [128, 4, 512], BF16) for i in range(2)]
        vst = [ph.sb("vst%d" % i, [128, 512], BF16) for i in range(2)]
        sig = [ph.sb("sig%d" % i, [128, 512], F32) for i in range(2)]
        gst = ph.sb("gst", [128, 48], F32)
        psi = {"i": 0}

        def nextps():
            i = psi["i"] % 6
            psi["i"] += 1
            return PS[i], PSK[i]

        if not own_pass:
            blocks = [(b * 512, 4) for b in range(16)]
        else:
            blocks = [(OWN - 128, 1)] + [(OWN + b * 512, 4) for b in range(8)]

        def ln_tile(bi, ti):
            tok0 = blocks[bi][0]
            x_ = xt[ti % 2]; xk = "xt%d" % (ti % 2)
            m.ld("sp", x_[:], xv[tok0 + ti * 128: tok0 + (ti + 1) * 128, :], [], [xk])
            layer_norm(tm, x_[:], [xk], hb[ti][:], ["hb%d" % ti])

        def tr_block(bi):
            for ti in range(blocks[bi][1]):
                transpose_to(hT[bi % 2], "hT%d" % (bi % 2), hb[ti], ["hb%d" % ti], ti)

        def groups(bi):
            tok0, ntile = blocks[bi]
            hTb = hT[bi % 2]; hk = "hT%d" % (bi % 2)
            ntok = ntile * 128
            gl = []
            if not own_pass:
                for kind, dst in enumerate((kcmpT_d, vcmpT_d, kslcT_d, kwinT_d)):
                    for pp in range(2):
                        def g_(kind=kind, dst=dst, pp=pp):
                            sg = stg[kind % 2]; sk = "stg%d" % (kind % 2)
                            ps, pk = nextps()
                            c0 = kind * 256 + pp * 128
                            for k in range(KC):
                                m.mm(ps[:, 0:ntok], wkf[:, k, c0:c0 + 128], hTb[:, k, 0:ntok], k == 0, k == KC - 1,
                                     ["wkf", hk], [pk])
                            m.cp("act" if pp % 2 else "dve", sg[:, pp, 0:ntok], ps[:, 0:ntok], [pk], [sk])
                            if pp == 1:
                                m.ld("pool", dst.rearrange("h d t -> (h d) t").rearrange("(a p) t -> p a t", p=128)[:, :, tok0:tok0 + ntok],
                                     sg[:, 0:2, 0:ntok], [sk], [])
                        gl.append(g_)
                for ti in range(ntile):
                    def g_(ti=ti):
                        ps, pk = nextps()
                        for k in range(KC):
                            m.mm(ps[:, :], hTb[:, k, ti * 128:(ti + 1) * 128], wvt[:, k, :], k == 0, k == KC - 1, ["wvt", hk], [pk])
                        v_ = vst[ti % 2]; vk = "vst%d" % (ti % 2)
                        m.cp("act" if ti % 2 else "dve", v_[:], ps[:, :], [pk], [vk])
                        r0 = tok0 + ti * 128
                        m.ld("pool", vslc_d[r0:r0 + 128, :], v_[:, 0:256], [vk], [])
                        m.ld("pool", vwin_d[r0:r0 + 128, :], v_[:, 256:512], [vk], [])
                    gl.append(g_)
            else:
                halo = (bi == 0)
                if not halo:
                    o0 = tok0 - OWN
                    for hg in range(4):
                        for pp in range(2):
                            def g_(hg=hg, pp=pp):
                                sg = stg[hg % 2]; sk = "stg%d" % (hg % 2)
                                c0 = (hg * 4 + 2 * pp) * 64
                                ps, pk = nextps()
                                for k in range(KC):
                                    m.mm(ps[:, 0:ntok], wq[:, k, c0:c0 + 128], hTb[:, k, 0:ntok], k == 0, k == KC - 1,
                                         ["wq", hk], [pk])
                                m.act(sg[:, pp, 0:ntok], ps[:, 0:ntok], AF.Copy, [pk], [sk], scale=0.125)
                                if pp == 1:
                                    m.ld("pool", qT_d[hg * 4:(hg + 1) * 4].rearrange("h d t -> (h d) t").rearrange("(a p) t -> p a t", p=128)[:, :, o0:o0 + ntok],
                                         sg[:, 0:2, 0:ntok], [sk], [])
                            gl.append(g_)
                    for ti in range(ntile):
                        def g_(ti=ti):
                            ps, pk = nextps()
                            for k in range(KC):
                                m.mm(ps[:, 0:48], hTb[:, k, ti * 128:(ti + 1) * 128], wg[:, k, :], k == 0, k == KC - 1, ["wg", hk], [pk])
                            m.act(gst[:], ps[:, 0:48], AF.Sigmoid, [pk], ["gst"])
                            m.ld("pool", gates_d[o0 + ti * 128:o0 + (ti + 1) * 128, :], gst[:], ["gst"], [])
                        gl.append(g_)
                for cc in range(8):
                    def g_(cc=cc):
                        pa, pak = nextps()
                        pg, pgk = nextps()
                        for k in range(KC):
                            m.mm(pa[:, 0:ntok], wu[:, k, cc * 128:(cc + 1) * 128], hTb[:, k, 0:ntok], k == 0, k == KC - 1, ["wu", hk], [pak])
                        for k in range(KC):
                            m.mm(pg[:, 0:ntok], wu[:, k, 1024 + cc * 128:1024 + (cc + 1) * 128], hTb[:, k, 0:ntok], k == 0, k == KC - 1, ["wu", hk], [pgk])
                        s_ = sig[cc % 2]; sk2 = "sig%d" % (cc % 2)
                        v_ = vst[cc % 2]; vk = "vst%d" % (cc % 2)
                        m.act(s_[:, 0:ntok], pg[:, 0:ntok], AF.Sigmoid, [pgk], [sk2])
                        if halo:
                            m.ts("dve", s_[:, 0:ntok], s_[:, 0:ntok], hal[:, 0:1], None, ALU.mult, None, [sk2, "hal"], [sk2])
                        m.tt("dve", v_[:, 0:ntok], pa[:, 0:ntok], s_[:, 0:ntok], ALU.mult, [pak, sk2], [vk])
                        c0 = 0 if halo else 128 + (tok0 - OWN)
                        m.ld("pool", hconvT_d[cc, :, c0:c0 + ntok], v_[:, 0:ntok], [vk], [])
                    gl.append(g_)
            return gl

        for ti in range(blocks[0][1]):
            ln_tile(0, ti)
        tr_block(0)
        for bi in range(len(blocks)):
            gs = groups(bi)
            nxt = bi + 1 < len(blocks)
            nt_n = blocks[bi + 1][1] if nxt else 0
            npart = nt_n + 1
            bounds = [len(gs) * p // npart for p in range(npart + 1)]
            for p in range(npart):
                for g_ in gs[bounds[p]:bounds[p + 1]]:
                    g_()
                if p < nt_n:
                    ln_tile(bi + 1, p)
            if nxt:
                tr_block(bi + 1)
        ph.close()

    if "A1" in phases:
        phase_A(False)
    if "A2" in phases:
        phase_A(True)

    def phase_BC():
        ph = Phase()
        kcT = ph.sb("kcT", [128, 4, 512], BF16)
        vcx = ph.sb("vcx", [128, 4, 4, 193], BF16)
        m.op("dve", lambda e: e.memset(kcT[:], 0.0), [], ["kcT"])
        m.op("dve", lambda e: e.memset(vcx[:], 0.0), [], ["vcx"])
        for h in range(4):
            m.op("dve", lambda e, h=h: e.memset(vcx[:, :, h, 64:65], 1.0), [], ["vcx"])
            m.ld("pool", vcx[:, :, h, 65:193], ov_c.rearrange("(t p) j -> p t j", p=128), [], ["vcx"])
        pb_ = Phase()
        w1s = pb_.sb("w1s", [64, 32, 256], BF16)
        w2s = pb_.sb("w2s", [128, 2, 64], BF16)
        pes = pb_.sb("pes", [64, 32], BF16)
        cj = pb_.sb("cj", [128, 2], F32)
        Tin = [pb_.sb("Tin%d" % i, [64, SV], BF16) for i in range(2)]
        T2 = [pb_.sb("T2_%d" % i, [64, 16, 512], BF16) for i in range(2)]
        gu = pb_.sb("gu", [128, 512], F32); g2 = pb_.sb("g2", [128, 512], F32)
        gT = pb_.sb("gT", [128, 2, 512], BF16)
        m.op("dve", lambda e: e.memset(gT[:], 0.0), [], ["gT"])
        for kv in range(2):
            m.ld("pool", w1s[:], (w1k, w1v)[kv].rearrange("d (r j) -> d r j", r=32), [], ["w1s"])
            m.ld("pool", w2s[:], (w2k, w2v)[kv].rearrange("(c p) d -> p c d", p=128), [], ["w2s"])
            m.ld("pool", pes[:], (pek, pev)[kv], [], ["pes"])
            for jc in range(2):
                for r in range(32):
                    m.mm(PS[0][:, jc * 2:jc * 2 + 2], w1s[:, r, jc * 128:(jc + 1) * 128], pes[:, r:r + 1].broadcast_to([64, 2]), r == 0, r == 31,
                         ["w1s", "pes"], ["ps0"], skip=True)
            m.cp("dve", cj[:], PS[0][:, 0:4:2], ["ps0"], ["cj"])
            for h in range(4):
                T_ = Tin[h % 2]; tk = "Tin%d" % (h % 2)
                m.ld("sp", T_[:], (kcmpT_d, vcmpT_d)[kv][h], [], [tk])
                t2 = T2[h % 2]; t2k = "T2_%d" % (h % 2)
                for r8 in range(0, 16, 8):
                    m.cp("dve" if r8 else "pool", t2[:, r8:r8 + 8, :], T_[:].rearrange("d (n r) -> d r n", r=16)[:, r8:r8 + 8, :], [tk], [t2k])
                for jc in range(2):
                    ps, pk = PS[1 + jc], PSK[1 + jc]
                    for r in range(32):
                        m.mm(ps[:, 0:511], w1s[:, r, jc * 128:(jc + 1) * 128], t2[:, r % 16, r // 16:r // 16 + 511], r == 0, r == 31,
                             ["w1s", t2k], [pk])
                    m.act(gu[:, 0:511], ps[:, 0:511], AF.Identity, [pk, "cj"], ["gu"], bias=cj[:, jc:jc + 1])
                    m.tt("dve", g2[:, 0:511], gu[:, 0:511], gu[:, 0:511], ALU.mult, ["gu"], ["g2"])
                    m.ts("dve", g2[:, 0:511], g2[:, 0:511], 0.044715, 1.0, ALU.mult, ALU.add, ["g2"], ["g2"])
                    m.tt("dve", g2[:, 0:511], g2[:, 0:511], gu[:, 0:511], ALU.mult, ["g2", "gu"], ["g2"])
                    m.act(g2[:, 0:511], g2[:, 0:511], AF.Tanh, ["g2"], ["g2"], scale=0.7978845608028654)
                    m.ts("dve", g2[:, 0:511], g2[:, 0:511], 1.0, 0.5, ALU.add, ALU.mult, ["g2"], ["g2"])
                    m.tt("dve", gT[:, jc, 0:511], g2[:, 0:511], gu[:, 0:511], ALU.mult, ["g2", "gu"], ["gT"])
                if kv == 0:
                    for jc in range(2):
                        m.mm(PS[3][0:64, 0:512], w2s[:, jc, :], gT[:, jc, :], jc == 0, jc == 1, ["w2s", "gT"], ["ps3"])
                    m.cp("dve", kcT[0:64, h, 0:511], PS[3][0:64, 0:511], ["ps3"], ["kcT"])
                else:
                    for bt in range(4):
                        for jc in range(2):
                            m.mm(PS[3][:, bt * 64:(bt + 1) * 64], gT[:, jc, bt * 128:(bt + 1) * 128], w2s[:, jc, :], jc == 0, jc == 1,
                                 ["w2s", "gT"], ["ps3"], skip=True)
                    m.cp("dve", vcx[:, :, h, 0:64], PS[3][:, 0:256].rearrange("p (t d) -> p t d", t=4), ["ps3"], ["vcx"])
        pb_.close()
        if "C" not in phases:
            ph.close()
            return
        tabs = ph.sb("tabs", [128, 2, 2048], BF16)
        anti = ph.sb("anti", [128, 512], BF16)
        bandt = ph.sb("bandt", [16, 2048], BF16)
        dwt = ph.sb("dwt", [16, 272], BF16)
        i4 = ph.sb("i4", [128, 512], BF16)
        f0 = ph.sb("f0", [128, 128], F32); b3 = ph.sb("b3", [128, 3], F32)
        kbias = ph.sb("kbias", [128, 64], F32); cbias = ph.sb("cbias", [128, 4], F32)
        with ExitStack() as es:
            raw = es.enter_context(nc.sbuf_tensor("raw_t", [128, 16, 128], F32))
            r31s = es.enter_context(nc.sbuf_tensor("r31s", [128, 16], F32))
            msk = es.enter_context(nc.sbuf_tensor("msk", [128, 128], F32))
            m.ld("sp", r31s[:], r31, [], ["r31s"])
            m.ld("sp", raw[:].rearrange("p h q -> p (h q)"), diag_raw, [], ["raw"])
            m.ld("sp", msk[:], diag_mask, [], ["msk"])
            for hd in range(16):
                m.stt(tabs[:, 0, hd * 128:(hd + 1) * 128], raw[:, hd, :], r31s[:, hd:hd + 1], msk[:], ALU.subtract, ALU.add, ["raw", "r31s", "msk"], ["tabs"])
            m.ld("sp", raw[:].rearrange("p h q -> p (h q)"), sub1_raw, [], ["raw"])
            for hd in range(16):
                m.ts("dve", tabs[:, 1, hd * 128:(hd + 1) * 128], raw[:, hd, :], r31s[:, hd:hd + 1], None, ALU.subtract, None, ["raw", "r31s"], ["tabs"])
            m.ld("sp", raw[0:16].rearrange("p h q -> p (h q)"), band_raw, [], ["raw"])
            m.ld("sp", msk[0:16, :], band_mask, [], ["msk"])
            for hd in range(16):
                m.stt(bandt[:, hd * 128:(hd + 1) * 128], raw[0:16, hd, :], r31s[0:16, hd:hd + 1], msk[0:16, :], ALU.subtract, ALU.add, ["raw", "r31s", "msk"], ["bandt"])
            m.ld("sp", msk[:], anti_mask, [], ["msk"])
            for g in range(4):
                m.cp("dve", anti[:, g * 128:(g + 1) * 128], msk[:], ["msk"], ["anti"])
                m.cp("dve", i4[:, g * 128:(g + 1) * 128], ident_f[:], ["ident_f"], ["i4"])
            m.barrier()
        m.ld("pool", dwt[:], dw_c, [], ["dwt"])
        m.ld("sp", f0[:], f0_c, [], ["f0"]); m.ld("sp", b3[:], band3_c, [], ["b3"])
        m.ld("sp", kbias[:], keybias_c, [], ["kbias"]); m.ld("sp", cbias[:], cmpbias_c, [], ["cbias"])

        kslc = ph.sb("kslc", [128, SV], BF16)
        kwin = ph.sb("kwin", [128, OWN + 512], BF16)
        vslc = ph.sb("vslc", [128, 64, 65], BF16)
        vwin = ph.sb("vwin", [128, 36, 65], BF16)
        qT = ph.sb("qT", [64, NT, 512], BF16)
        gts = ph.sb("gts", [128, NT, 12], F32)
        pT = [ph.sb("pT%d" % i, [128, 512], BF16) for i in range(6)]
        mneg = [ph.sb("mneg%d" % i, [128, 192], BF16) for i in range(2)]
        QM = [[ph.sb("QM%d%d" % (i, c), [128, 512], BF16) for c in range(2)] for i in range(3)]
        for i in range(2):
            m.op("pool", lambda e, i=i: e.memset(mneg[i][:], 0.0), [], ["mneg%d" % i])
        for i in range(3):
            for c in range(2):
                m.op("pool", lambda e, i=i, c=c: e.memset(QM[i][c][:], 0.0), [], ["QM%d" % i])
        m.op("pool", lambda e: e.memset(kwin[64:128, :], 0.0), [], ["kwin_hi"])
        for c4 in range(4):
            m.ld("pool", kslc[64:128, c4 * 2048:(c4 + 1) * 2048], asel_c[:, c4 * 2048:(c4 + 1) * 2048], [], ["kslc_hi"])
        sc = ph.sb("sc", [128, 128], F32); sc2 = ph.sb("sc2", [128, 128], F32)
        mx = ph.sb("mx", [128, 16], F32)
        acc = [ph.sb("acc%d" % i, [128, 3, 4, 65], F32) for i in range(2)]
        rinv = [ph.sb("rinv%d" % i, [128, 12], F32) for i in range(2)]
        ob = [ph.sb("ob%d" % i, [128, 256], BF16) for i in range(2)]
        of = ph.sb("of", [128, 4, 64], F32)
        m.op("dve", lambda e: e.memset(vslc[:, :, 64:65], 1.0), [], ["vslc"])
        m.op("dve", lambda e: e.memset(vwin[:, :, 64:65], 1.0), [], ["vwin"])
        SCB = [PS[0][:], PS[1][:], PS[2][:], PB[1][:].bitcast(F32)]
        SCK = ["ps0", "ps1", "ps2", "pb1"]
        WACC = PB[0][:].bitcast(F32)
        sidx = {"i": 0}
        pidx = {"i": 0}
        DEPTH = 3
        pend = []
        delayed = []

        def run_delayed():
            while delayed:
                delayed.pop(0)[1]()

        def flush_one():
            u = pend.pop(0)
            u[0]()
            for p in u[1]:
                p()

        def add_unit(kT_ap, kkeys, rows, qap, extra, bias_ap, bias_keys, accps, acck, v_ap, vkeys, ncol, first, last):
            si = sidx["i"] % 4; sidx["i"] += 1
            ps, pk = SCB[si], SCK[si]
            n_ex = len(extra)
            m.mm(ps[0:rows, :], kT_ap, qap, True, n_ex == 0, kkeys + ["qT"], [pk])
            for xi, (l_ap, r_ap, ks) in enumerate(extra):
                m.mm(ps[0:rows, :], l_ap, r_ap, False, xi == n_ex - 1, ks, [pk])

            def pv():
                pi = pidx["i"] % 6; pidx["i"] += 1
                p_, ppk = pT[pi], "pT%d" % pi
                m.act(p_[0:rows, :], ps[0:rows, :], AF.Exp, [pk] + bias_keys, [ppk], bias=bias_ap)
                for g in range(4):
                    m.mm(accps[:, g * ncol:(g + 1) * ncol] if ncol == 65 else accps[g // 2][:, (g % 2) * ncol:(g % 2 + 1) * ncol],
                         p_[0:rows, g * 128:(g + 1) * 128], v_ap, first and (g == 0 or (ncol != 65 and g == 2)), last and g == 3,
                         [ppk] + vkeys, acck, skip=True)

            pend.append((pv, []))
            if len(pend) > DEPTH:
                flush_one()
            for dl in list(delayed):
                dl[0] -= 1
                if dl[0] <= 0:
                    delayed.remove(dl)
                    dl[1]()

        def add_post(fn):
            if pend:
                pend[-1][1].append(fn)
            else:
                fn()

        for h in range(4):
            for g in range(4):
                m.ld("sp", qT[:].rearrange("d l (g q) -> d l g q", g=4)[:, :, g, :],
                     qT_d[h * 4 + g].rearrange("d (l q) -> d l q", q=128), [], ["qT"])
            m.ld("sp", gts[:], gates_d[:, h * 12:(h + 1) * 12].rearrange("(t p) c -> p t c", p=128), [], ["gts"])
            m.ld("sp", kwin[0:64, :], kwinT_d[h][:, OWN - 512:SV], [], ["kwin"])
            for t8 in range(4):
                m.ld("sp", vwin[:, t8 * 9:(t8 + 1) * 9, 0:64],
                     vwin_d[OWN - 512 + t8 * 1152:OWN - 512 + (t8 + 1) * 1152, h * 64:(h + 1) * 64].rearrange("(t p) d -> p t d", p=128), [], ["vwin"])
            m.ld("sp", kslc[0:64, :], kslcT_d[h], [], ["kslc"])
            for t8 in range(8):
                m.ld("sp", vslc[:, t8 * 8:(t8 + 1) * 8, 0:64],
                     vslc_d[t8 * 1024:(t8 + 1) * 1024, h * 64:(h + 1) * 64].rearrange("(t p) d -> p t d", p=128), [], ["vslc"])

            def emit_cmp(lt, h=h):
                t = 32 + lt
                qmk = "QM%d" % (lt % 3)
                for l2 in ([0, 1] if lt == 0 else [lt + 1]):
                    if l2 < NT:
                        for c in range(2):
                            m.cp("pool", QM[l2 % 3][c][0:64, :], qT[:, l2, :], ["qT"], ["QM%d" % (l2 % 3)])
                qap = QM[lt % 3][0][:]
                a_ = acc[lt % 2]; ak = "acc%d" % (lt % 2)
                rv = rinv[lt % 2]; rvk = "rinv%d" % (lt % 2)
                n_c = 8 * t + 7
                ntile = (n_c + 127) // 128
                b0 = 8 * t - 9
                jb, off = b0 // 128, b0 % 128
                for J in range(ntile):
                    rows = min(128, n_c - 128 * J)
                    extra = []
                    if J == jb:
                        extra.append((dwt[:, 128 - off:128 - off + rows], bandt[:, h * 512:(h + 1) * 512], ["dwt", "bandt"]))
                    elif J == jb + 1:
                        extra.append((dwt[:, 256 - off:256 - off + rows], bandt[:, h * 512:(h + 1) * 512], ["dwt", "bandt"]))
                    add_unit(kcT[:, h, J * 128:J * 128 + rows], ["kcT", qmk], rows, qap, extra, (cbias[0:rows, J:J + 1] if J < 2 else None), ["cbias"],
                             (PS[4], PS[5]), ["ps4", "ps5"], vcx[0:rows, J, h, :], ["vcx"], 193, J == 0, J == ntile - 1)

                def post():
                    for half in range(2):
                        pc = PS[4 + half]
                        m.cp("act", a_[:, 0, half * 2:half * 2 + 2, :], pc[:, 0:386].rearrange("p (g c) -> p g c", g=2)[:, :, 0:65],
                             ["ps%d" % (4 + half)], [ak])
                    m.ts("dve", rv[:, 0:4], a_[:, 0, :, 64], 1e-30, None, ALU.max, None, [ak], [rvk])
                    m.op("dve", lambda e: e.reciprocal(out=rv[:, 0:4], in_=rv[:, 0:4]), [rvk], [rvk])
                    for g in range(4):
                        pc = PS[4 + g // 2]
                        u_ap = pc[:, (g % 2) * 193 + 65:(g % 2) * 193 + 193]
                        if g == 0:
                            m.stt(sc[:], u_ap, rv[:, 0:1], f0[:], ALU.mult, ALU.add, ["ps4", rvk, "f0"], ["sc"])
                        else:
                            m.stt(sc[:], u_ap, rv[:, g:g + 1], sc[:], ALU.mult, ALU.add, ["ps%d" % (4 + g // 2), rvk, "sc"], ["sc"])
                    ncol = 2 * t + 2
                    m.tt("dve", sc[:, 2 * t - 1:2 * t + 2], sc[:, 2 * t - 1:2 * t + 2], b3[:], ALU.add, ["sc", "b3"], ["sc"])
                    m.op("dve", lambda e: e.max(out=mx[:, 0:8], in_=sc[:, 0:ncol]), ["sc"], ["mx"])
                    m.op("dve", lambda e: e.match_replace(out=sc2[:, 0:ncol], in_to_replace=mx[:, 0:8], in_values=sc[:, 0:ncol], imm_value=-1e9),
                         ["sc", "mx"], ["sc2"])
                    m.op("dve", lambda e: e.max(out=mx[:, 8:16], in_=sc2[:, 0:ncol]), ["sc2"], ["mx"])
                    mn = mneg[lt % 2]; mk_ = "mneg%d" % (lt % 2)
                    m.ts("dve", sc2[:, 0:ncol], sc[:, 0:ncol], mx[:, 15:16], -NEG, ALU.is_ge, ALU.mult, ["sc", "mx"], ["sc2"])
                    m.ts("dve", mn[:, 64:64 + ncol], sc2[:, 0:ncol], NEG, None, ALU.add, None, ["sc2"], [mk_])

                    def post_b():
                        for c in range(2):
                            m.mm(PS[4 + c][:, :], mn[:, 64 * c:64 * c + 128], i4[:], True, True, [mk_, "i4"], ["ps%d" % (4 + c)])
                            m.cp("dve", QM[lt % 3][c][64:128, :], PS[4 + c][64:128, :], ["ps%d" % (4 + c)], [qmk])
                    delayed.append([10, post_b])
                add_post(post)

            def emit_win(lt, h=h):
                t = 32 + lt
                qmk = "QM%d" % (lt % 3)
                qap = QM[lt % 3][0][:]
                a_ = acc[lt % 2]; ak = "acc%d" % (lt % 2)
                for j in range(t - 4, t + 1):
                    extra = []
                    if j == t:
                        extra.append((ident_b[:], tabs[:, 0, h * 512:(h + 1) * 512], ["ident_b", "tabs"]))
                    elif j == t - 1:
                        extra.append((ident_b[:], tabs[:, 1, h * 512:(h + 1) * 512], ["ident_b", "tabs"]))
                    elif j == t - 4:
                        extra.append((ident_b[:], anti[:], ["ident_b", "anti"]))
                    jl = j - 28
                    add_unit(kwin[:, jl * 128:(jl + 1) * 128], ["kwin", "kwin_hi", qmk], 128, qap, extra, (kbias[:, j:j + 1] if j < 32 else None), ["kbias"],
                             WACC, ["pb0"], vwin[:, jl, :], ["vwin"], 65, j == t - 4, j == t)
                add_post(lambda: m.cp("act", a_[:, 2, :, :], WACC[:, 0:260].rearrange("p (g c) -> p g c", g=4), ["pb0"], [ak]))

            def emit_sel(lt, h=h):
                t = 32 + lt
                qmk = "QM%d" % (lt % 3)
                a_ = acc[lt % 2]; ak = "acc%d" % (lt % 2)
                rv = rinv[lt % 2]; rvk = "rinv%d" % (lt % 2)
                if lt == 0:
                    while pend:
                        flush_one()
                    run_delayed()
                for j in range(t + 1):
                    qap = QM[lt % 3][j // 32][:]
                    extra = []
                    if j == t:
                        extra.append((ident_b[:], tabs[:, 0, h * 512:(h + 1) * 512], ["ident_b", "tabs"]))
                    elif j == t - 1:
                        extra.append((ident_b[:], tabs[:, 1, h * 512:(h + 1) * 512], ["ident_b", "tabs"]))
                    add_unit(kslc[:, j * 128:(j + 1) * 128], ["kslc", "kslc_hi", qmk], 128, qap, extra, (kbias[:, j:j + 1] if j < 32 else None), ["kbias"],
                             PS[3], ["ps3"], vslc[:, j, :], ["vslc"], 65, j == 0, j == t)

                def post():
                    m.cp("act", a_[:, 1, :, :], PS[3][:, 0:260].rearrange("p (g c) -> p g c", g=4), ["ps3"], [ak])
                    m.ts("dve", rv[:, 4:12].rearrange("p (b g) -> p b g", b=2), a_[:, 1:3, :, 64], 1e-30, None, ALU.max, None, [ak], [rvk])
                    m.op("dve", lambda e: e.reciprocal(out=rv[:, 4:12], in_=rv[:, 4:12]), [rvk], [rvk])
                    m.tt("dve", rv[:].rearrange("p (b g) -> p b g", b=3), rv[:].rearrange("p (b g) -> p b g", b=3),
                         gts[:, lt, :].rearrange("p (g b) -> p b g", b=3), ALU.mult, [rvk, "gts"], [rvk])
                    o_ = ob[lt % 2]; ok = "ob%d" % (lt % 2)
                    for g in range(4):
                        m.ts("dve", of[:, g, :], a_[:, 0, g, 0:64], rv[:, g:g + 1], None, ALU.mult, None, [ak, rvk], ["of"])
                        m.stt(of[:, g, :], a_[:, 1, g, 0:64], rv[:, 4 + g:5 + g], of[:, g, :], ALU.mult, ALU.add, [ak, rvk, "of"], ["of"])
                        m.stt(o_[:, g * 64:(g + 1) * 64], a_[:, 2, g, 0:64], rv[:, 8 + g:9 + g], of[:, g, :], ALU.mult, ALU.add, [ak, rvk, "of"], [ok])
                    m.ld("sp", o_d[lt * 128:(lt + 1) * 128, h * 256:(h + 1) * 256], o_[:], [ok], [])
                add_post(post)

            emit_cmp(0)
            for lt in range(NT):
                emit_win(lt)
                if lt + 1 < NT:
                    emit_cmp(lt + 1)
                emit_sel(lt)
            while pend:
                flush_one()
            run_delayed()
        ph.close()

    if "B" in phases:
        phase_BC()

    def phase_D():
        ph = Phase()
        dg = ph.sb("dg", [128, 8, 31, 128], BF16)
        wc = ph.sb("wc", [128, 8, 31], F32)
        cv = ph.sb("cv", [128, 24], F32)
        hc = ph.sb("hc", [128, 8, 128 + OWN], BF16)
        yb = ph.sb("yb", [128, 8, 512], F32)
        ysq = ph.sb("ysq", [128, 512], F32)
        mean = ph.sb("mean", [128, 512], F32); rstd = ph.sb("rstd", [128, 512], F32)
        z = [ph.sb("z%d" % i, [128, 512], F32) for i in range(4)]
        zo = [ph.sb("zo%d" % i, [128, 512], BF16) for i in range(4)]
        m.ld("sp", wc[:].rearrange("p c k -> p (c k)"), dww, [], ["wc"])
        m.ld("sp", cv[:], cvec, [], ["cv"])
        for cc in range(8):
            m.ld("sp", hc[:, cc, :], hconvT_d[cc], [], ["hc"])
            for k in range(31):
                if k % 2:
                    m.ts("dve", dg[:, cc, k, :], ident_f[:], wc[:, cc, k:k + 1], None, ALU.mult, None, ["ident_f", "wc"], ["dg"])
                else:
                    m.act(dg[:, cc, k, :], ident_f[:], AF.Copy, ["ident_f", "wc"], ["dg"], scale=wc[:, cc, k:k + 1])
        for blk in range(8):
            for cc in range(8):
                ps, pk = PS[cc % 4], PSK[cc % 4]
                for k in range(31):
                    c0 = 128 + blk * 512 - 30 + k
                    m.mm(ps[:, :], dg[:, cc, k, :], hc[:, cc, c0:c0 + 512], k == 0, k == 30, ["dg", "hc"], [pk])
                m.act(yb[:, cc, :], ps[:, :], AF.Identity, [pk, "cv"], ["yb%d" % cc], bias=cv[:, cc:cc + 1])
                m.act(ysq[:], yb[:, cc, :], AF.Square, ["yb%d" % cc], ["ysq"])
                m.mm(PS[4][:, :], ones_f[:], yb[:, cc, :], cc == 0, cc == 7, ["ones_f", "yb%d" % cc], ["ps4"])
                m.mm(PS[5][:, :], ones_f[:], ysq[:], cc == 0, cc == 7, ["ones_f", "ysq"], ["ps5"])
            m.act(mean[:], PS[4][:, :], AF.Copy, ["ps4"], ["mean"], scale=1.0 / 1024)
            m.tt("dve", rstd[:], mean[:], mean[:], ALU.mult, ["mean"], ["rstd"])
            m.stt(rstd[:], PS[5][:, :], 1.0 / 1024, rstd[:], ALU.mult, ALU.subtract, ["ps5", "rstd"], ["rstd"])
            m.act(rstd[:], rstd[:], AF.Sqrt, ["rstd"], ["rstd"], bias=EPS)
            m.op("dve", lambda e: e.reciprocal(out=rstd[:], in_=rstd[:]), ["rstd"], ["rstd"])
            for cc in range(8):
                z_ = z[cc % 4]; zk = "z%d" % (cc % 4)
                zo_ = zo[cc % 4]; zok = "zo%d" % (cc % 4)
                m.tt("dve", z_[:], yb[:, cc, :], mean[:], ALU.subtract, ["yb%d" % cc, "mean"], [zk])
                m.tt("pool", z_[:], z_[:], rstd[:], ALU.mult, [zk, "rstd"], [zk])
                m.act(zo_[:], z_[:], AF.Silu, [zk, "cv"], [zok], bias=cv[:, 16 + cc:17 + cc], scale=cv[:, 8 + cc:9 + cc])
                m.ld("sp", oconvT_d[cc, :, blk * 512:(blk + 1) * 512], zo_[:], [zok], [])
        ph.close()

    if "D" in phases:
        phase_D()

    def load_w_bf(ph, name, src, ncols, q="pool"):
        wt = ph.sb(name, [128, KC, ncols], BF16)
        for k4 in range(0, KC, 4):
            m.ld(q, wt[:, k4:k4 + 4, :], src[k4 * 128:(k4 + 4) * 128, :].rearrange("(k p) n -> p k n", p=128), [], [name])
        return wt

    def phase_E1():
        ph = Phase()
        tm = ln_tmps(ph)
        wo_ = load_w_bf(ph, "w_o", w_out, D)
        lnrep2 = ph.sb("lnrep2", [128, 2, D], F32)
        m.ld("sp", lnrep2[:, 0, :], lnv[2:3, :].broadcast_to([128, D]), [], ["lnrep2"])
        m.ld("sp", lnrep2[:, 1, :], lnv[3:4, :].broadcast_to([128, D]), [], ["lnrep2"])
        load_ln(0)
        ot = [ph.sb("ot%d" % i, [128, 1024], BF16) for i in range(2)]
        mixT = [ph.sb("mixT%d" % i, [128, KC, 128], BF16) for i in range(2)]
        xt = [ph.sb("e1x%d" % i, [128, D], F32) for i in range(2)]
        y = [ph.sb("e1y%d" % i, [128, D], F32) for i in range(2)]
        yb_ = [ph.sb("e1yb%d" % i, [128, D], BF16) for i in range(2)]
        xT = [ph.sb("e1xT%d" % i, [128, KC, 128], BF16) for i in range(2)]
        def e1_part2(lt):
            i2 = lt % 2
            transpose_to(xT[i2], "e1xT%d" % i2, yb_[i2], ["e1yb%d" % i2], 0)
            m.ld("pool", x1T_d.rearrange("c p t -> p c t")[:, :, lt * 128:(lt + 1) * 128], xT[i2][:], ["e1xT%d" % i2], [])

        def e1_stage0(lt):
            i2 = lt % 2
            m.ld("sp", ot[i2][:], o_d[lt * 128:(lt + 1) * 128, :], [], ["ot%d" % i2])
            transpose_to(mixT[i2], "mixT%d" % i2, ot[i2], ["ot%d" % i2], 0, nk=8)
            m.ld("sp", mixT[i2][:, 8:16, :], oconvT_d.rearrange("c p t -> p c t")[:, :, lt * 128:(lt + 1) * 128], [], ["mixT%d" % i2])
            m.ld("sp", xt[i2][:], xv[OWN + lt * 128:OWN + (lt + 1) * 128, :], [], ["e1x%d" % i2])
            layer_norm(tm, xt[i2][:], ["e1x%d" % i2], xt[i2][:], ["e1x%d" % i2])

        e1_stage0(0)
        for lt in range(NT):
            i2 = lt % 2
            if lt + 1 < NT:
                e1_stage0(lt + 1)
            for oc in range(4):
                ps, pk = PS[oc], PSK[oc]
                for k in range(KC):
                    m.mm(ps[:, :], mixT[i2][:, k, :], wo_[:, k, oc * 512:(oc + 1) * 512], k == 0, k == KC - 1, ["mixT%d" % i2, "w_o"], [pk])
                m.stt(y[i2][:, oc * 512:(oc + 1) * 512], xt[i2][:, oc * 512:(oc + 1) * 512], ALPHA, ps[:, :], ALU.mult, ALU.add,
                      ["e1x%d" % i2, pk], ["e1y%d" % i2])
            ln_generic(tm, y[i2][:], ["e1y%d" % i2], lnrep2, "lnrep2")
            m.ld("pool", x1_d[lt * 128:(lt + 1) * 128, :], y[i2][:], ["e1y%d" % i2], [])
            m.cp("pool", yb_[i2][:], y[i2][:], ["e1y%d" % i2], ["e1yb%d" % i2])
            if lt > 0:
                e1_part2(lt - 1)
        e1_part2(NT - 1)
        ph.close()

    def ln_generic(tm, src, src_keys, rep, repk, np_=128, last_eng="pool"):
        st, mv, rs, nb = tm
        for c in range(4):
            m.op("dve", lambda e, c=c: e.bn_stats(out=st[:np_, c, :], in_=src[:, c * 512:(c + 1) * 512]), src_keys, ["ln_st"])
        m.op("dve", lambda e: e.bn_aggr(out=mv[:np_, :], in_=st[:np_, :, :]), ["ln_st"], ["ln_mv"])
        m.act(rs[:np_, :], mv[:np_, 1:2], AF.Sqrt, ["ln_mv"], ["ln_rs"], bias=EPS)
        m.op("dve", lambda e: e.reciprocal(out=rs[:np_, :], in_=rs[:np_, :]), ["ln_rs"], ["ln_rs"])
        m.stt(nb[:np_, :], mv[:np_, 0:1], -1.0, rs[:np_, :], ALU.mult, ALU.mult, ["ln_mv", "ln_rs"], ["ln_nb"])
        m.act(src, src, AF.Identity, src_keys + ["ln_rs", "ln_nb"], src_keys, bias=nb[:np_, :], scale=rs[:np_, :])
        m.tt("dve", src, src, rep[:np_, 0, :], ALU.mult, src_keys + [repk], src_keys)
        m.tt(last_eng, src, src, rep[:np_, 1, :], ALU.add, src_keys + [repk], src_keys)

    if "E1" in phases:
        phase_E1()

    def phase_E2():
        ph = Phase()
        tm = ln_tmps(ph)
        KmT = ph.sb("KmT", [128, KC, 256], BF16)
        Vm = ph.sb("Vm", [128, 2, D], BF16)
        load_ln(4)
        pm = Phase()
        mt_ = pm.sb("mt_", [128, D], F32); mb_ = pm.sb("mb_", [128, D], BF16)
        mT = pm.sb("mT", [128, KC, 256], BF16)
        wkv = [pm.sb("wkv%d" % i, [128, KC, 512], BF16) for i in range(2)]
        wkvf = pm.sb("wkvf", [128, KC, 512], F32)
        for ti in range(2):
            m.ld("sp", mt_[:], memb[ti * 128:(ti + 1) * 128, :], [], ["mt_"])
            layer_norm(tm, mt_[:], ["mt_"], mb_[:], ["mb_"])
            transpose_to(mT, "mT", mb_, ["mb_"], ti)
        for cb in range(8):
            w_ = wkv[cb % 2]; wk_ = "wkv%d" % (cb % 2)
            if cb % 2 == 0:
                for k4 in range(0, KC, 4):
                    m.ld("pool", w_[:, k4:k4 + 4, :],
                         xa_wkv[k4 * 128:(k4 + 4) * 128, cb * 512:(cb + 1) * 512].rearrange("(k p) n -> p k n", p=128), [], [wk_])
            else:
                for k4 in range(0, KC, 4):
                    m.ld("sp", wkvf[:, k4:k4 + 4, :],
                         xa_wkv[k4 * 128:(k4 + 4) * 128, cb * 512:(cb + 1) * 512].rearrange("(k p) n -> p k n", p=128), [], ["wkvf%d" % k4])
                    m.cp("dve" if (k4 // 4) % 2 else "act", w_[:, k4:k4 + 4, :], wkvf[:, k4:k4 + 4, :], ["wkvf%d" % k4], [wk_])
            if cb < 4:
                for sub in range(4):
                    ps, pk = PS[sub], PSK[sub]
                    for k in range(KC):
                        m.mm(ps[:, 0:256], w_[:, k, sub * 128:(sub + 1) * 128], mT[:, k, :], k == 0, k == KC - 1, [wk_, "mT"], [pk])
                    m.cp("dve" if sub % 2 else "act", KmT[:, cb * 4 + sub, :], ps[:, 0:256], [pk], ["KmT"])
            else:
                for ti in range(2):
                    ps, pk = PS[ti], PSK[ti]
                    for k in range(KC):
                        m.mm(ps[:, :], mT[:, k, ti * 128:(ti + 1) * 128], w_[:, k, :], k == 0, k == KC - 1, [wk_, "mT"], [pk])
                    m.cp("dve" if ti else "act", Vm[:, ti, (cb - 4) * 512:(cb - 3) * 512], ps[:, :], [pk], ["Vm"])
        pm.close()
        wq_ = load_w_bf(ph, "xwq", xa_wq, D)
        xT = [ph.sb("e2xT%d" % i, [128, KC, 512], BF16) for i in range(2)]
        qx = ph.sb("qx", [128, KC, 512], BF16)
        pT = [ph.sb("e2pT%d" % i, [128, 2, 512], BF16) for i in range(2)]
        rinv = ph.sb("e2rinv", [128, 512], F32)
        ox = [ph.sb("e2ox0", [128, KC, 512], BF16)] * 2
        sc_ = 512 ** -0.5
        for gi in range(8):
            i2 = gi % 2
            xk = "e2xT%d" % i2
            m.ld("sp", xT[i2][:], x1T_d.rearrange("c p t -> p c t")[:, :, gi * 512:(gi + 1) * 512], [], [xk])
            for oc in range(KC):
                ps, pk = PS[oc % 4], PSK[oc % 4]
                for k in range(KC):
                    m.mm(ps[:, :], wq_[:, k, oc * 128:(oc + 1) * 128], xT[i2][:, k, :], k == 0, k == KC - 1, ["xwq", xk], [pk])
                m.cp("dve" if oc % 2 else "act", qx[:, oc, :], ps[:, :], [pk], ["qx%d" % oc])
            for hd in range(4):
                p_ = pT[hd % 2]; ppk = "e2pT%d" % (hd % 2)
                for mt in range(2):
                    ps, pk = PS[mt], PSK[mt]
                    for kk in range(4):
                        m.mm(ps[:, :], KmT[:, hd * 4 + kk, mt * 128:(mt + 1) * 128], qx[:, hd * 4 + kk, :], kk == 0, kk == 3,
                             ["KmT", "qx%d" % (hd * 4 + kk)], [pk])
                    m.act(p_[:, mt, :], ps[:, :], AF.Exp, [pk], [ppk], scale=sc_)
                for mt in range(2):
                    m.mm(PS[2][:, :], ones_b[:], p_[:, mt, :], mt == 0, mt == 1, ["ones_b", ppk], ["ps2"])
                m.op("dve", lambda e: e.reciprocal(out=rinv[:], in_=PS[2][:, :]), ["ps2"], ["e2rinv"])
                for dc in range(4):
                    ps, pk = PS[3 + dc % 3], PSK[3 + dc % 3]
                    for mt in range(2):
                        m.mm(ps[:, :], Vm[:, mt, (hd * 4 + dc) * 128:(hd * 4 + dc + 1) * 128], p_[:, mt, :], mt == 0, mt == 1, ["Vm", ppk], [pk])
                    m.tt("dve", ox[i2][:, hd * 4 + dc, :], ps[:, :], rinv[:], ALU.mult, [pk, "e2rinv"], ["e2ox0"])
            m.ld("pool", oxT_d.rearrange("c p t -> p c t")[:, :, gi * 512:(gi + 1) * 512], ox[i2][:], ["e2ox0"], [])
        ph.close()

    if "E2" in phases:
        phase_E2()

    def phase_E3():
        ph = Phase()
        tm = ln_tmps(ph)
        wo_ = load_w_bf(ph, "xwo", xa_wo, D)
        load_ln(6)
        rws = ph.sb("rws", [128, KC, 36], F32)
        rbs = ph.sb("rbs", [128, 36], F32)
        ut = ph.sb("ut", [128, 128], BF16)
        ecs = ph.sb("ecs", [128, 32], F32)
        carry = ph.sb("carry", [128, 32], F32)
        m.ld("sp", rws[:], rw.rearrange("(c p) j -> p c j", p=128), [], ["rws"])
        m.ld("sp", rbs[:], rb.broadcast_to([128, 36]), [], ["rbs"])
        m.ld("pool", ut[:], ut_c, [], ["ut"])
        m.ld("sp", ecs[:], ec_c, [], ["ecs"])
        m.op("dve", lambda e: e.memset(carry[:], 0.0), [], ["carry"])
        oT = [ph.sb("e3oT%d" % i, [128, KC, 128], BF16) for i in range(2)]
        x1 = [ph.sb("e3x1%d" % i, [128, D], F32) for i in range(2)]
        y = [ph.sb("e3y%d" % i, [128, D], F32) for i in range(2)]
        yb_ = [ph.sb("e3yb%d" % i, [128, D], BF16) for i in range(2)]
        yT = ph.sb("e3yT", [128, KC, 128], F32)
        lg = ph.sb("lg", [128, 36], F32)
        sm = ph.sb("sm", [128, 16], F32)
        oh = ph.sb("oh", [128, 4, 32], F32)
        ohb = ph.sb("ohb", [128, 32], BF16)
        lem = ph.sb("lem", [128, 32], F32)
        mx8 = ph.sb("mx8", [128, 8], F32)
        rk = ph.sb("rk", [128, 32], F32)
        rto = [ph.sb("rto%d" % i, [128, 4], F32) for i in range(2)]
        dsti = [ph.sb("dsti%d" % i, [128, 2], I32) for i in range(2)]
        def e3_part2a(lt):
            i2 = lt % 2
            yk = "e3y%d" % i2
            for k0 in range(0, KC, 4):
                ps, pk = PS[4 + (k0 // 4) % 2], PSK[4 + (k0 // 4) % 2]
                for kk in range(4):
                    m.tr(ps[:, kk * 128:(kk + 1) * 128], y[i2][:, (k0 + kk) * 128:(k0 + kk + 1) * 128], ident_f[:], [yk, "ident_f"], [pk])
                m.cp("act", yT[:, k0:k0 + 4, :], ps[:, :].rearrange("p (k t) -> p k t", k=4), [pk], ["e3yT"])

        def e3_part2(lt):
            i2 = lt % 2
            yk = "e3y%d" % i2
            for k in range(KC):
                m.mm(PS[4][:, 0:36], yT[:, k, :], rws[:, k, :], k == 0, k == KC - 1, ["e3yT", "rws"], ["ps4"])
            m.tt("dve", lg[:], PS[4][:, 0:36], rbs[:], ALU.add, ["ps4", "rbs"], ["lg"])
            m.op("dve", lambda e: e.tensor_reduce(out=sm[:, 0:1], in_=lg[:, 0:4], axis=AX.X, op=ALU.max), ["lg"], ["sm"])
            m.ts("dve", sm[:, 1:2], sm[:, 0:1], -1.0, None, ALU.mult, None, ["sm"], ["sm"])
            m.act(sm[:, 4:8], lg[:, 0:4], AF.Exp, ["lg", "sm"], ["sm"], bias=sm[:, 1:2], accum=sm[:, 2:3])
            m.op("dve", lambda e: e.reciprocal(out=sm[:, 3:4], in_=sm[:, 2:3]), ["sm"], ["sm"])
            m.ts("dve", sm[:, 8:12], lg[:, 0:4], sm[:, 0:1], None, ALU.is_ge, None, ["lg", "sm"], ["sm"])
            m.ts("dve", oh[:, 0, :].rearrange("p (g e) -> p g e", g=4), sm[:, 8:12].unsqueeze(2).broadcast_to([128, 4, 8]), 1.0, 1e4,
                 ALU.subtract, ALU.mult, ["sm"], ["oh"])
            m.tt("dve", lem[:], lg[:, 4:36], oh[:, 0, :], ALU.add, ["lg", "oh"], ["lem"])
            m.op("dve", lambda e: e.max(out=mx8[:], in_=lem[:]), ["lem"], ["mx8"])
            m.ts("dve", oh[:, 1, :], lem[:], mx8[:, 0:1], None, ALU.is_ge, None, ["lem", "mx8"], ["oh"])
            m.ts("dve", oh[:, 3, :], lem[:], mx8[:, 1:2], None, ALU.is_ge, None, ["lem", "mx8"], ["oh"])
            m.tt("dve", oh[:, 2, :], oh[:, 3, :], oh[:, 1, :], ALU.subtract, ["oh"], ["oh"])
            m.cp("dve", ohb[:], oh[:, 3, :], ["oh"], ["ohb"])
            m.tt("dve", sm[:, 12:13], mx8[:, 1:2], mx8[:, 0:1], ALU.subtract, ["mx8"], ["sm"])
            m.act(sm[:, 13:14], sm[:, 12:13], AF.Exp, ["sm"], ["sm"])
            m.ts("dve", sm[:, 14:15], sm[:, 13:14], 1.0, None, ALU.add, None, ["sm"], ["sm"])
            m.op("dve", lambda e: e.reciprocal(out=sm[:, 14:15], in_=sm[:, 14:15]), ["sm"], ["sm"])
            r_ = rto[i2]; rk_ = "rto%d" % i2
            m.tt("dve", r_[:, 2:3], sm[:, 14:15], sm[:, 3:4], ALU.mult, ["sm"], [rk_])
            m.tt("dve", r_[:, 3:4], sm[:, 13:14], r_[:, 2:3], ALU.mult, ["sm", rk_], [rk_])
            m.mm(PS[5][:, 0:32], ut[:], ohb[:], True, True, ["ut", "ohb"], ["ps5"])
            m.tt("dve", rk[:], PS[5][:, 0:32], carry[:], ALU.add, ["ps5", "carry"], ["rk"])
            m.tt("dve", rk[:], rk[:], ecs[:], ALU.add, ["rk", "ecs"], ["rk"])
            m.mm(PS[5][:, 32:64], ones_b[:], ohb[:], True, True, ["ones_b", "ohb"], ["ps5"])
            m.tt("dve", carry[:], carry[:], PS[5][:, 32:64], ALU.add, ["ps5", "carry"], ["carry"])
            for kq in range(2):
                m.tt("dve", oh[:, 0, :], oh[:, 1 + kq, :], rk[:], ALU.mult, ["oh", "rk"], ["oh"])
                m.op("dve", lambda e, kq=kq, r_=r_: e.tensor_reduce(out=r_[:, kq:kq + 1], in_=oh[:, 0, :], axis=AX.X, op=ALU.add), ["oh"], [rk_])
            d_ = dsti[i2]; dk = "dsti%d" % i2
            m.cp("dve", d_[:], r_[:, 0:2], [rk_], [dk])
            m.ld("pool", rt_d[lt * 128:(lt + 1) * 128, :], r_[:], [rk_], [])
            for kq in range(2):
                m.dma("pool", lambda e, kq=kq, d_=d_, yy=yb_[i2]: e.indirect_dma_start(
                    out=xs_d, out_offset=bass.IndirectOffsetOnAxis(ap=d_[:, kq:kq + 1], axis=0), in_=yy[:], in_offset=None),
                    ["e3yb%d" % i2, dk], ["xs_d"])

        for lt in range(NT):
            i2 = lt % 2
            m.ld("sp", oT[i2][:], oxT_d.rearrange("c p t -> p c t")[:, :, lt * 128:(lt + 1) * 128], [], ["e3oT%d" % i2])
            m.ld("sp", x1[i2][:], x1_d[lt * 128:(lt + 1) * 128, :], [], ["e3x1%d" % i2])
            if lt > 0:
                e3_part2a(lt - 1)
            for oc in range(4):
                ps, pk = PS[oc], PSK[oc]
                for k in range(KC):
                    m.mm(ps[:, :], oT[i2][:, k, :], wo_[:, k, oc * 512:(oc + 1) * 512], k == 0, k == KC - 1, ["e3oT%d" % i2, "xwo"], [pk])
                m.stt(y[i2][:, oc * 512:(oc + 1) * 512], x1[i2][:, oc * 512:(oc + 1) * 512], ALPHA, ps[:, :], ALU.mult, ALU.add,
                      ["e3x1%d" % i2, pk], ["e3y%d" % i2])
            yk = "e3y%d" % i2
            ln_generic(tm, y[i2][:], [yk], lnrep, "lnrep", last_eng="dve")
            m.ld("pool", x2_d[lt * 128:(lt + 1) * 128, :], y[i2][:], [yk], [])
            m.cp("dve", yb_[i2][:], y[i2][:], [yk], ["e3yb%d" % i2])
            if lt > 0:
                e3_part2(lt - 1)
        e3_part2a(NT - 1)
        e3_part2(NT - 1)
        ph.close()

    if "E3" in phases:
        phase_E3()

    def phase_F():
        ph = Phase()
        wg_ = [ph.sb("fwg%d" % i, [128, KC, 512], BF16) for i in range(2)]
        wu_ = [ph.sb("fwu%d" % i, [128, KC, 512], BF16) for i in range(2)]
        wd_ = [ph.sb("fwd%d" % i, [128, 4, D], BF16) for i in range(2)]
        xr = [ph.sb("fxr%d" % i, [128, D], BF16) for i in range(2)]
        xsT = [ph.sb("fxsT%d" % i, [128, KC, CAP], BF16) for i in range(2)]
        hT = ph.sb("fhT", [128, 4, CAP], BF16)
        sg = [ph.sb("fsg%d" % i, [128, CAP], F32) for i in range(2)]
        yo = [ph.sb("fyo%d" % i, [128, D], F32) for i in range(2)]
        wdf = ph.sb("fwdf", [128, 2, D], F32)
        cnt = {"i": 0}

        def f_loadw(e_):
            i2 = e_ % 2
            for k8 in range(0, KC, 8):
                m.ld("pool", wg_[i2][:, k8:k8 + 8, :], mwg[e_, k8 * 128:(k8 + 8) * 128, :].rearrange("(k p) n -> p k n", p=128), [], ["fwg%d" % i2])
            for k8 in range(0, KC, 8):
                m.ld("pool", wu_[i2][:, k8:k8 + 8, :], mwu[e_, k8 * 128:(k8 + 8) * 128, :].rearrange("(k p) n -> p k n", p=128), [], ["fwu%d" % i2])
            for k2 in range(0, 4, 2):
                for kk in range(2):
                    m.ld("sp", wdf[:, kk, :], mwd[e_, (k2 + kk) * 128:(k2 + kk + 1) * 128, :], [], ["fwdf%d" % kk])
                    m.cp("pool", wd_[i2][:, k2 + kk, :], wdf[:, kk, :], ["fwdf%d" % kk], ["fwd%d" % i2])

        def f_xsT(e_):
            i2 = e_ % 2
            for rt in range(4):
                x_ = xr[rt % 2]; xk = "fxr%d" % (rt % 2)
                m.ld("sp", x_[:], xs_d[e_ * CAP + rt * 128:e_ * CAP + (rt + 1) * 128, :], ["xs_d"], [xk])
                transpose_to(xsT[i2], "fxsT%d" % i2, x_, [xk], rt, only="dve")

        f_loadw(0)
        f_xsT(0)
        for e_ in range(NE):
            i2 = e_ % 2
            if e_ + 1 < NE:
                f_loadw(e_ + 1)
            for hc in range(4):
                pg, pgk = PS[(hc % 2) * 2], PSK[(hc % 2) * 2]
                pu, puk = PS[(hc % 2) * 2 + 1], PSK[(hc % 2) * 2 + 1]
                for k in range(KC):
                    m.mm(pg[:, :], wg_[i2][:, k, hc * 128:(hc + 1) * 128], xsT[i2][:, k, :], k == 0, k == KC - 1, ["fwg%d" % i2, "fxsT%d" % i2], [pgk])
                for k in range(KC):
                    m.mm(pu[:, :], wu_[i2][:, k, hc * 128:(hc + 1) * 128], xsT[i2][:, k, :], k == 0, k == KC - 1, ["fwu%d" % i2, "fxsT%d" % i2], [puk])
                s_ = sg[hc % 2]; sk = "fsg%d" % (hc % 2)
                m.act(s_[:], pg[:, :], AF.Silu, [pgk], [sk])
                m.tt("dve", hT[:, hc, :], pu[:, :], s_[:], ALU.mult, [puk, sk], ["fhT%d" % hc])
            if e_ + 1 < NE:
                f_xsT(e_ + 1)
            for rt in range(4):
                y_ = yo[rt % 2]; yk = "fyo%d" % (rt % 2)
                for oc in range(4):
                    pi = 4 + cnt["i"] % 2; cnt["i"] += 1
                    ps, pk = PS[pi], PSK[pi]
                    for hc in range(4):
                        m.mm(ps[:, :], hT[:, hc, rt * 128:(rt + 1) * 128], wd_[i2][:, hc, oc * 512:(oc + 1) * 512], hc == 0, hc == 3,
                             ["fhT%d" % hc, "fwd%d" % i2], [pk])
                    m.cp("dve", y_[:, oc * 512:(oc + 1) * 512], ps[:, :], [pk], [yk])
                r0 = e_ * CAP + rt * 128
                m.ld("act", ys_d[r0:r0 + 128, :], y_[:], [yk], ["ys_d"])
        ph.close()

    if "F" in phases:
        phase_F()

    def phase_G():
        ph = Phase()
        tm = ln_tmps(ph)
        load_ln(8)
        NB = 4
        x2 = [ph.sb("gx%d" % i, [128, D], F32) for i in range(NB)]
        y1 = [ph.sb("gy1%d" % i, [128, D], F32) for i in range(NB)]
        y2 = [ph.sb("gy2%d" % i, [128, D], F32) for i in range(NB)]
        rto = [ph.sb("grt%d" % i, [128, 4], F32) for i in range(NB)]
        dsti = [ph.sb("gds%d" % i, [128, 2], I32) for i in range(NB)]

        def issue(lt):
            i2 = lt % NB
            m.ld("sp", x2[i2][:], x2_d[lt * 128:(lt + 1) * 128, :], [], ["gx%d" % i2])
            m.ld("sp", rto[i2][:], rt_d[lt * 128:(lt + 1) * 128, :], [], ["grt%d" % i2])
            m.cp("dve", dsti[i2][:], rto[i2][:, 0:2], ["grt%d" % i2], ["gds%d" % i2])
            for kq, yy in enumerate((y1[i2], y2[i2])):
                yk = "gy%d%d" % (kq + 1, i2)
                m.dma("pool", lambda e, kq=kq, yy=yy, d_=dsti[i2]: e.indirect_dma_start(
                    out=yy[:], out_offset=None, in_=ys_d, in_offset=bass.IndirectOffsetOnAxis(ap=d_[:, kq:kq + 1], axis=0)),
                    ["ys_d", "gds%d" % i2], [yk])

        issue(0)
        issue(1)
        for lt in range(NT):
            i2 = lt % NB
            if lt + 2 < NT:
                issue(lt + 2)
            xk = "gx%d" % i2
            m.act(x2[i2][:], x2[i2][:], AF.Copy, [xk], [xk], scale=ALPHA)
            m.stt(x2[i2][:], y1[i2][:], rto[i2][:, 2:3], x2[i2][:], ALU.mult, ALU.add, ["gy1%d" % i2, "grt%d" % i2, xk], [xk])
            m.stt(x2[i2][:], y2[i2][:], rto[i2][:, 3:4], x2[i2][:], ALU.mult, ALU.add, ["gy2%d" % i2, "grt%d" % i2, xk], [xk])
            ln_generic(tm, x2[i2][:], [xk], lnrep, "lnrep", last_eng="dve")
            m.ld("sp", out[lt * 128:(lt + 1) * 128, :], x2[i2][:], [xk], [])
        ph.close()

    if "G" in phases:
        phase_G()

    m.finish()
    return nc, m


def _t5_bucket(dist):
    n = np.maximum(dist, 0)
    nf = np.maximum(n, 1).astype(np.float32)
    large = 16 + (np.log(nf / np.float32(16)) / np.float32(math.log(8.0)) * np.float32(16)).astype(np.int32)
    large = np.minimum(large, 31)
    return np.where(n < 16, n, large).astype(np.int64)


def _prep(inputs):
    f = lambda a: np.ascontiguousarray(np.asarray(a, dtype=np.float32))
    x = f(inputs["x"]); mem = f(inputs["mem"])
    w_in = f(inputs["w_in"])[0]
    sizes = [1024] + [256] * 6 + [48, 2048]
    cuts = np.cumsum([0] + sizes)
    sec = [w_in[:, cuts[i]:cuts[i + 1]] for i in range(9)]
    q, k_c, v_c, k_s, v_s, k_w, v_w, g, u = sec
    w_inp = np.ascontiguousarray(np.concatenate([k_c, v_c, k_s, k_w, v_s, v_w, q, g, u], axis=1))
    lnv = np.stack([f(inputs["ln_in_g"]), f(inputs["ln_in_b"]), f(inputs["ln1_g"])[0], f(inputs["ln1_b"])[0],
                    f(inputs["mem_ln_g"])[0], f(inputs["mem_ln_b"])[0], f(inputs["ln2_g"])[0], f(inputs["ln2_b"])[0],
                    f(inputs["ln3_g"])[0], f(inputs["ln3_b"])[0]])

    def w1l(w):
        return np.ascontiguousarray(f(w)[0].reshape(32, 64, 256).transpose(1, 0, 2).reshape(64, 32 * 256))

    rel = f(inputs["rel_table"])
    kk = np.arange(128)[:, None]; qq = np.arange(128)[None, :]
    d_diag = qq - kk
    diag_raw = rel[_t5_bucket(d_diag)]
    diag_raw = np.ascontiguousarray(diag_raw.transpose(0, 2, 1).reshape(128, 16 * 128))
    diag_mask = np.where(d_diag >= 0, 0.0, NEG).astype(np.float32)
    sub1_raw = np.ascontiguousarray(rel[_t5_bucket(128 + qq - kk)].transpose(0, 2, 1).reshape(128, 16 * 128))
    ii = np.arange(16)[:, None]
    d_band = qq - 16 * ii + 113
    band_raw = np.ascontiguousarray(rel[_t5_bucket(d_band)].transpose(0, 2, 1).reshape(16, 16 * 128))
    band_mask = np.where(d_band >= 0, 0.0, NEG).astype(np.float32)
    anti_mask = np.where(kk > qq, 0.0, NEG).astype(np.float32)
    r31 = np.ascontiguousarray(np.broadcast_to(rel[31][None, :], (128, 16)))
    dw_c = np.zeros((16, 272), np.float32)
    dw_c[np.arange(16), 128 + np.arange(16)] = 1.0
    ident = np.eye(128, dtype=np.float32)
    ut = np.triu(np.ones((128, 128), np.float32), 1)
    cs = np.arange(512) * 16; ce = cs + 32
    ss = np.arange(128) * 64; se = ss + 64
    ov = (np.clip(np.minimum(ce[:, None], se[None, :]) - np.maximum(cs[:, None], ss[None, :]), 0, None) / 32.0).astype(np.float32)
    ov[511] = 0.0
    ec = np.ascontiguousarray(np.broadcast_to((np.arange(32) * CAP).astype(np.float32)[None, :], (128, 32)))
    asel = np.zeros((64, SV), np.float32)
    kcol = np.arange(SV)
    asel[(2 * (kcol // 128) + (kcol % 128) // 64) % 64, kcol] = 1.0
    band3 = np.zeros((128, 3), np.float32)
    band3[:64, 0] = 100.0; band3[:, 1] = 100.0; band3[64:, 2] = 100.0; band3[:64, 2] = -100.0
    dww = np.ascontiguousarray(f(inputs["conv_dw_w"])[0][:, 0, :].reshape(31, 8, 128).transpose(2, 1, 0).reshape(128, 8 * 31))
    cvec = np.concatenate([f(inputs["conv_dw_b"])[0].reshape(8, 128).T, f(inputs["conv_ln_g"])[0].reshape(8, 128).T,
                           f(inputs["conv_ln_b"])[0].reshape(8, 128).T], axis=1)
    cvec = np.ascontiguousarray(cvec)
    rwm = np.ascontiguousarray(np.concatenate([f(inputs["router_group_w"])[0], f(inputs["router_expert_w"])[0]], axis=1))
    rbm = np.concatenate([f(inputs["router_group_b"])[0], f(inputs["router_expert_b"])[0]])[None, :]
    common = dict(
        lnv=np.ascontiguousarray(lnv), w_inp=w_inp,
        w1k=w1l(inputs["cmp_w1_k"]), w1v=w1l(inputs["cmp_w1_v"]),
        pek=np.ascontiguousarray(f(inputs["cmp_pe_k"])[0].T), pev=np.ascontiguousarray(f(inputs["cmp_pe_v"])[0].T),
        w2k=f(inputs["cmp_w2_k"])[0], w2v=f(inputs["cmp_w2_v"])[0],
        diag_raw=diag_raw, sub1_raw=sub1_raw, band_raw=band_raw, r31=r31,
        diag_mask=diag_mask, anti_mask=anti_mask, band_mask=band_mask,
        dw_c=dw_c, asel_c=asel, ident_c=ident, ut_c=ut, ov_c=ov, ec_c=ec, band3_c=band3,
        dww=dww, cvec=cvec,
        w_out=f(inputs["w_out"])[0], xa_wq=f(inputs["xa_wq"])[0], xa_wkv=f(inputs["xa_wkv"])[0], xa_wo=f(inputs["xa_wo"])[0],
        rw=rwm, rb=np.ascontiguousarray(rbm),
        mwg=f(inputs["moe_w_gate"])[0], mwu=f(inputs["moe_w_up"])[0], mwd=f(inputs["moe_w_down"])[0],
    )
    in_maps = []
    for c in range(8):
        b, s = c // 2, c % 2
        if s == 1:
            xvv = x[b]
        else:
            xvv = np.ascontiguousarray(np.concatenate([x[b, OWN:], x[b, :OWN]], axis=0))
        keyb = np.zeros((128, 64), np.float32)
        cmpb = np.zeros((128, 4), np.float32)
        f0 = np.zeros((128, 128), np.float32)
        if s == 0:
            keyb[:, :32] = NEG
            cmpb[:, :2] = NEG
            f0[:, 64] = 100.0
        else:
            f0[:, 0] = 100.0
        cmpb[127, 3] = NEG
        halo = np.full((128, 1), float(s), np.float32)
        d = dict(common)
        d.update(xv=xvv, memb=mem[b], keybias_c=keyb, cmpbias_c=cmpb, f0_c=f0, halo_c=halo)
        in_maps.append(d)
    return in_maps


_CACHE = {}


def kernel(**inputs):
    in_maps = _prep(inputs)
    if "nc" not in _CACHE:
        _CACHE["nc"] = build()[0]
    res = run_bass_kernel_spmd(_CACHE["nc"], in_maps, core_ids=list(range(8)))
    outp = np.zeros((4, SV, D), np.float32)
    for c in range(8):
        b, s = c // 2, c % 2
        outp[b, s * OWN:(s + 1) * OWN] = res.results[c]["out"]
    return outp
```

```python
import os
import math
from contextlib import ExitStack
import numpy as np
import concourse.bass as bass
import concourse.mybir as mybir
from concourse.bass_utils import run_bass_kernel_spmd

F32 = mybir.dt.float32
BF16 = mybir.dt.bfloat16
I32 = mybir.dt.int32
AF = mybir.ActivationFunctionType
ALU = mybir.AluOpType
AX = mybir.AxisListType

SEM_LIMIT = 30000
N_DMA_SLOTS = 12

D = 2048
KC = 16
SV = 8192
OWN = 4096
NT = 32
ALPHA = 2 ** 0.25
EPS = 1e-5
NEG = -30000.0
CAP = 512
NE = 32
O_KF, O_VT, O_Q, O_G, O_U = 0, 1024, 1536, 2560, 2608
PH_ALL = "A1,A2,B,C,D,E1,M,E2,E3,F,G"


class MK:
    ENG = ("pe", "act", "dve", "pool", "sp")

    def __init__(self, nc):
        self.nc = nc
        self.streams = {e: [] for e in self.ENG}
        self.cur_sem = {}
        self.cur_cnt = {}
        for e in ("pe", "act", "dve", "pool"):
            self.cur_sem[e] = nc.alloc_semaphore("s_" + e + "0")
            self.cur_cnt[e] = 0
        self.nsem = 4
        self.slots = {}
        self.slot_rr = {}
        for q in ("sp", "pool", "act"):
            self.slots[q] = [[nc.alloc_semaphore("d_%s%d" % (q, i)), 0] for i in range(N_DMA_SLOTS)]
            self.slot_rr[q] = 0
        self.seen = {e: {} for e in self.ENG}
        self.state = {}
        self.all_events = {}
        self.n_inst = {e: 0 for e in self.ENG}

    def _need(self, eng, reads, writes):
        need = {}

        def add(ev):
            if ev is None:
                return
            sem, val = ev
            k = id(sem)
            if k not in need or need[k][1] < val:
                need[k] = (sem, val)

        for k in reads:
            st = self.state.get(k)
            if st:
                add(st[0])
        for k in writes:
            st = self.state.get(k)
            if st:
                add(st[0])
                for ev in st[1].values():
                    add(ev)
        out = []
        for k, (sem, val) in need.items():
            if eng == "pe" and sem is self.cur_sem["pe"]:
                continue
            if self.seen[eng].get(k, 0) < val:
                self.seen[eng][k] = val
                out.append((sem, val))
        return out

    def _emit_waits(self, eng, waits):
        for sem, val in waits:
            self.streams[eng].append(lambda e, sem=sem, val=val: e.wait_ge(sem, val))

    def _commit(self, ev, reads, writes):
        sem, val = ev
        self.all_events[id(sem)] = ev
        for k in reads:
            st = self.state.setdefault(k, [None, {}])
            st[1][id(sem)] = ev
        for k in writes:
            self.state[k] = [ev, {}]

    def op(self, eng, fn, reads=(), writes=()):
        waits = self._need(eng, reads, writes)
        self._emit_waits(eng, waits)
        if self.cur_cnt[eng] >= SEM_LIMIT:
            self.cur_sem[eng] = self.nc.alloc_semaphore("s_%s%d" % (eng, self.nsem))
            self.nsem += 1
            self.cur_cnt[eng] = 0
        self.cur_cnt[eng] += 1
        sem, val = self.cur_sem[eng], self.cur_cnt[eng]
        self.streams[eng].append(lambda e, sem=sem: fn(e).then_inc(sem, 1))
        self.n_inst[eng] += 1
        self._commit((sem, val), reads, writes)

    def dma(self, q, fn, reads=(), writes=()):
        slot = self.slots[q][self.slot_rr[q]]
        self.slot_rr[q] = (self.slot_rr[q] + 1) % N_DMA_SLOTS
        sem, prev = slot
        waits = self._need(q, reads, writes)
        k = id(sem)
        if self.seen[q].get(k, 0) < prev:
            self.seen[q][k] = prev
            waits.append((sem, prev))
        self._emit_waits(q, waits)
        val = prev + 16
        slot[1] = val
        self.streams[q].append(lambda e, sem=sem: fn(e).then_inc(sem, 16))
        self.n_inst[q] += 1
        self._commit((sem, val), reads, writes)

    def barrier(self):
        evs = list(self.all_events.values())
        for eng in self.ENG:
            for sem, val in evs:
                k = id(sem)
                if eng == "pe" and sem is self.cur_sem["pe"]:
                    continue
                if self.seen[eng].get(k, 0) < val:
                    self.seen[eng][k] = val
                    self.streams[eng].append(lambda e, sem=sem, val=val: e.wait_ge(sem, val))
        self.state = {}

    def finish(self):
        self.barrier()
        nc = self.nc
        with nc.Block() as block:
            @block.tensor
            def _(e):
                for f in self.streams["pe"]:
                    f(e)

            @block.scalar
            def _(e):
                for f in self.streams["act"]:
                    f(e)

            @block.vector
            def _(e):
                for f in self.streams["dve"]:
                    f(e)

            @block.gpsimd
            def _(e):
                for f in self.streams["pool"]:
                    f(e)

            @block.sync
            def _(e):
                for f in self.streams["sp"]:
                    f(e)

    def mm(self, out, lhsT, rhs, start, stop, r, w, skip=False):
        self.op("pe", lambda e: e.matmul(out, lhsT=lhsT, rhs=rhs, start=start, stop=stop,
                                         skip_group_check=skip), r, w)

    def tr(self, out, in_, ident, r, w):
        self.op("pe", lambda e: e.transpose(out, in_, ident), r, w)

    def act(self, out, in_, func, r, w, bias=None, scale=None, accum=None):
        kw = {}
        if bias is not None:
            kw["bias"] = bias
        if scale is not None:
            kw["scale"] = scale
        if accum is not None:
            kw["accum_out"] = accum
        self.op("act", lambda e: e.activation(out=out, in_=in_, func=func, **kw), r, w)

    def tt(self, eng, out, in0, in1, op, r, w):
        self.op(eng, lambda e: e.tensor_tensor(out=out, in0=in0, in1=in1, op=op), r, w)

    def ts(self, eng, out, in0, s1, s2, op0, op1, r, w):
        if op1 is None:
            self.op(eng, lambda e: e.tensor_scalar(out=out, in0=in0, scalar1=s1, scalar2=None, op0=op0), r, w)
        else:
            self.op(eng, lambda e: e.tensor_scalar(out=out, in0=in0, scalar1=s1, scalar2=s2, op0=op0, op1=op1), r, w)

    def stt(self, out, in0, scalar, in1, op0, op1, r, w):
        self.op("dve", lambda e: e.scalar_tensor_tensor(out=out, in0=in0, scalar=scalar, in1=in1, op0=op0, op1=op1), r, w)

    def cp(self, eng, out, in_, r, w):
        if eng == "act":
            self.op("act", lambda e: e.activation(out=out, in_=in_, func=AF.Copy), r, w)
        else:
            self.op(eng, lambda e: e.tensor_copy(out=out, in_=in_), r, w)

    def ld(self, q, out, in_, r, w):
        self.dma(q, lambda e: e.dma_start(out=out, in_=in_), r, w)


def build(phases=PH_ALL, debug=()):
    phases = phases.split(",")
    nc = bass.Bass("TRN2", target_bir_lowering=False)
    m = MK(nc)

    def din(name, shape, dt=F32):
        return nc.dram_tensor(name, list(shape), dt, kind="ExternalInput").ap()

    def dscr(name, shape, dt):
        return nc.dram_tensor(name, list(shape), dt, kind="ExternalOutput" if name in debug else "Internal").ap()

    xv = din("xv", [SV, D])
    memb = din("memb", [256, D])
    lnv = din("lnv", [10, D])
    w_inp = din("w_inp", [D, 4656])
    w1k = din("w1k", [64, 32 * 256]); w1v = din("w1v", [64, 32 * 256])
    pek = din("pek", [64, 32]); pev = din("pev", [64, 32])
    w2k = din("w2k", [256, 64]); w2v = din("w2v", [256, 64])
    diag_raw = din("diag_raw", [128, 16 * 128]); sub1_raw = din("sub1_raw", [128, 16 * 128])
    band_raw = din("band_raw", [16, 16 * 128]); r31 = din("r31", [128, 16])
    diag_mask = din("diag_mask", [128, 128]); anti_mask = din("anti_mask", [128, 128]); band_mask = din("band_mask", [16, 128])
    dw_c = din("dw_c", [16, 272]); ident_c = din("ident_c", [128, 128]); ut_c = din("ut_c", [128, 128])
    ov_c = din("ov_c", [512, 128]); ec_c = din("ec_c", [128, 32]); f0_c = din("f0_c", [128, 128]); band3_c = din("band3_c", [128, 3])
    asel_c = din("asel_c", [64, SV]); keybias_c = din("keybias_c", [128, 64]); cmpbias_c = din("cmpbias_c", [128, 4]); halo_c = din("halo_c", [128, 1])
    dww = din("dww", [128, 8 * 31]); cvec = din("cvec", [128, 24])
    w_out = din("w_out", [D, D]); xa_wq = din("xa_wq", [D, D]); xa_wkv = din("xa_wkv", [D, 2 * D]); xa_wo = din("xa_wo", [D, D])
    rw = din("rw", [D, 36]); rb = din("rb", [1, 36])
    mwg = din("mwg", [NE, D, 512]); mwu = din("mwu", [NE, D, 512]); mwd = din("mwd", [NE, 512, D])
    out = nc.dram_tensor("out", [OWN, D], F32, kind="ExternalOutput").ap()

    kcmpT_d = dscr("kcmpT_d", [4, 64, SV], BF16); vcmpT_d = dscr("vcmpT_d", [4, 64, SV], BF16)
    kslcT_d = dscr("kslcT_d", [4, 64, SV], BF16); kwinT_d = dscr("kwinT_d", [4, 64, SV], BF16)
    vslc_d = dscr("vslc_d", [SV, 256], BF16); vwin_d = dscr("vwin_d", [SV, 256], BF16)
    qT_d = dscr("qT_d", [16, 64, OWN], BF16)
    gates_d = dscr("gates_d", [OWN, 48], F32)
    hconvT_d = dscr("hconvT_d", [8, 128, 128 + OWN], BF16)
    o_d = dscr("o_d", [OWN, 1024], BF16)
    oconvT_d = dscr("oconvT_d", [8, 128, OWN], BF16)
    x1_d = dscr("x1_d", [OWN, D], F32); x1T_d = dscr("x1T_d", [KC, 128, OWN], BF16)
    oxT_d = dscr("oxT_d", [KC, 128, OWN], BF16)
    x2_d = dscr("x2_d", [OWN, D], F32)
    xs_d = dscr("xs_d", [NE * CAP, D], BF16)
    ys_d = dscr("ys_d", [NE * CAP, D], F32)
    rt_d = dscr("rt_d", [OWN, 4], F32)

    ident_f = nc.alloc_sbuf_tensor("ident_f", [128, 128], F32)
    ident_b = nc.alloc_sbuf_tensor("ident_b", [128, 128], BF16)
    ones_b = nc.alloc_sbuf_tensor("ones_b", [128, 128], BF16)
    ones_f = nc.alloc_sbuf_tensor("ones_f", [128, 128], F32)
    lnrep = nc.alloc_sbuf_tensor("lnrep", [128, 2, D], F32)
    PS = [nc.alloc_psum_tensor("ps%d" % i, [128, 512], F32) for i in range(6)]
    PB = [nc.alloc_psum_tensor("pb%d" % i, [128, 1024], BF16) for i in range(2)]
    PSK = ["ps%d" % i for i in range(6)]
    PBK = ["pb%d" % i for i in range(2)]

    m.ld("sp", ident_f[:], ident_c, [], ["ident_f"])
    m.ld("pool", ident_b[:], ident_c, [], ["ident_b"])
    m.op("dve", lambda e: e.memset(ones_b[:], 1.0), [], ["ones_b"])
    m.op("dve", lambda e: e.memset(ones_f[:], 1.0), [], ["ones_f"])

    def load_ln(row):
        m.ld("sp", lnrep[:, 0, :], lnv[row:row + 1, :].broadcast_to([128, D]), [], ["lnrep"])
        m.ld("sp", lnrep[:, 1, :], lnv[row + 1:row + 2, :].broadcast_to([128, D]), [], ["lnrep"])

    uid = {"n": 0}

    class Phase:
        def __init__(self):
            self.es = ExitStack()
            self.n = 0

        def sb(self, name, shape, dt):
            uid["n"] += 1
            return self.es.enter_context(nc.sbuf_tensor("%s_%d" % (name, uid["n"]), list(shape), dt))

        def close(self):
            m.barrier()
            self.es.close()

    rr = {"i": 0}

    def layer_norm(ph_tmp, src, src_keys, dst, dst_keys, np_=128, last_eng="pool"):
        st, mv, rs, nb = ph_tmp
        for c in range(4):
            m.op("dve", lambda e, c=c: e.bn_stats(out=st[:np_, c, :], in_=src[:, c * 512:(c + 1) * 512]), src_keys, ["ln_st"])
        m.op("dve", lambda e: e.bn_aggr(out=mv[:np_, :], in_=st[:np_, :, :]), ["ln_st"], ["ln_mv"])
        m.act(rs[:np_, :], mv[:np_, 1:2], AF.Sqrt, ["ln_mv"], ["ln_rs"], bias=EPS)
        m.op("dve", lambda e: e.reciprocal(out=rs[:np_, :], in_=rs[:np_, :]), ["ln_rs"], ["ln_rs"])
        m.stt(nb[:np_, :], mv[:np_, 0:1], -1.0, rs[:np_, :], ALU.mult, ALU.mult, ["ln_mv", "ln_rs"], ["ln_nb"])
        m.act(src, src, AF.Identity, src_keys + ["ln_rs", "ln_nb"], src_keys, bias=nb[:np_, :], scale=rs[:np_, :])
        m.tt("dve", src, src, lnrep[:np_, 0, :], ALU.mult, src_keys + ["lnrep"], src_keys)
        m.tt(last_eng, dst, src, lnrep[:np_, 1, :], ALU.add, src_keys + ["lnrep"], dst_keys)

    def ln_tmps(ph):
        return (ph.sb("ln_st", [128, 4, 6], F32), ph.sb("ln_mv", [128, 2], F32),
                ph.sb("ln_rs", [128, 1], F32), ph.sb("ln_nb", [128, 1], F32))

    def transpose_to(hT, hT_key, src_bf, src_keys, ntile_idx, nk=KC, col0=0, only=None):
        for k0 in range(0, nk, 8):
            pi = rr["i"] % 2
            rr["i"] += 1
            pb = PB[pi]
            n8 = min(8, nk - k0)
            for kk in range(n8):
                m.tr(pb[:, kk * 128:(kk + 1) * 128], src_bf[:, (k0 + kk) * 128:(k0 + kk + 1) * 128], ident_b[:],
                     src_keys + ["ident_b"], [PBK[pi]])
            eng = only or ("dve" if (rr["i"] % 2) else "act")
            m.cp(eng, hT[:, k0:k0 + n8, col0 + ntile_idx * 128: col0 + (ntile_idx + 1) * 128],
                 pb[:, 0:n8 * 128].rearrange("p (k t) -> p k t", k=n8), [PBK[pi]], [hT_key])

    def phase_A(own_pass):
        ph = Phase()
        tm = ln_tmps(ph)
        load_ln(0)
        if not own_pass:
            wkf = ph.sb("wkf", [128, KC, 1024], BF16)
            wvt = ph.sb("wvt", [128, KC, 512], BF16)
            for k4 in range(0, KC, 4):
                m.ld("pool", wkf[:, k4:k4 + 4, :], w_inp[k4 * 128:(k4 + 4) * 128, O_KF:O_KF + 1024].rearrange("(k p) n -> p k n", p=128), [], ["wkf"])
                m.ld("pool", wvt[:, k4:k4 + 4, :], w_inp[k4 * 128:(k4 + 4) * 128, O_VT:O_VT + 512].rearrange("(k p) n -> p k n", p=128), [], ["wvt"])
        else:
            wq = ph.sb("wq", [128, KC, 1024], BF16)
            wg = ph.sb("wg", [128, KC, 48], BF16)
            wu = ph.sb("wu", [128, KC, 2048], BF16)
            hal = ph.sb("hal", [128, 1], F32)
            m.ld("sp", hal[:], halo_c, [], ["hal"])
            for k4 in range(0, KC, 4):
                m.ld("pool", wq[:, k4:k4 + 4, :], w_inp[k4 * 128:(k4 + 4) * 128, O_Q:O_Q + 1024].rearrange("(k p) n -> p k n", p=128), [], ["wq"])
                m.ld("pool", wg[:, k4:k4 + 4, :], w_inp[k4 * 128:(k4 + 4) * 128, O_G:O_G + 48].rearrange("(k p) n -> p k n", p=128), [], ["wg"])
                m.ld("pool", wu[:, k4:k4 + 4, :], w_inp[k4 * 128:(k4 + 4) * 128, O_U:O_U + 2048].rearrange("(k p) n -> p k n", p=128), [], ["wu"])
        xt = [ph.sb("xt%d" % i, [128, D], F32) for i in range(2)]
        hb = [ph.sb("hb%d" % i, [128, D], BF16) for i in range(4)]
        hT = [ph.sb("hT%d" % i, [128, KC, 512], BF16) for i in range(2)]
        stg = [ph.sb("stg%d" % i, [128, 4, 512], BF16) for i in range(2)]
        vst = [ph.sb("vst%d" % i, [128, 512], BF16) for i in range(2)]
        sig = [ph.sb("sig%d" % i, [128, 512], F32) for i in range(2)]
        gst = ph.sb("gst", [128, 48], F32)
        psi = {"i": 0}

        def nextps():
            i = psi["i"] % 6
            psi["i"] += 1
            return PS[i], PSK[i]

        if not own_pass:
            blocks = [(b * 512, 4) for b in range(16)]
        else:
            blocks = [(OWN - 128, 1)] + [(OWN + b * 512, 4) for b in range(8)]

        def ln_tile(bi, ti):
            tok0 = blocks[bi][0]
            x_ = xt[ti % 2]; xk = "xt%d" % (ti % 2)
            m.ld("sp", x_[:], xv[tok0 + ti * 128: tok0 + (ti + 1) * 128, :], [], [xk])
            layer_norm(tm, x_[:], [xk], hb[ti][:], ["hb%d" % ti])

        def tr_block(bi):
            for ti in range(blocks[bi][1]):
                transpose_to(hT[bi % 2], "hT%d" % (bi % 2), hb[ti], ["hb%d" % ti], ti)

        def groups(bi):
            tok0, ntile = blocks[bi]
            hTb = hT[bi % 2]; hk = "hT%d" % (bi % 2)
            ntok = ntile * 128
            gl = []
            if not own_pass:
                for kind, dst in enumerate((kcmpT_d, vcmpT_d, kslcT_d, kwinT_d)):
                    for pp in range(2):
                        def g_(kind=kind, dst=dst, pp=pp):
                            sg = stg[kind % 2]; sk = "stg%d" % (kind % 2)
                            ps, pk = nextps()
                            c0 = kind * 256 + pp * 128
                            for k in range(KC):
                                m.mm(ps[:, 0:ntok], wkf[:, k, c0:c0 + 128], hTb[:, k, 0:ntok], k == 0, k == KC - 1,
                                     ["wkf", hk], [pk])
                            m.cp("act" if pp % 2 else "dve", sg[:, pp, 0:ntok], ps[:, 0:ntok], [pk], [sk])
                            if pp == 1:
                                m.ld("pool", dst.rearrange("h d t -> (h d) t").rearrange("(a p) t -> p a t", p=128)[:, :, tok0:tok0 + ntok],
                                     sg[:, 0:2, 0:ntok], [sk], [])
                        gl.append(g_)
                for ti in range(ntile):
                    def g_(ti=ti):
                        ps, pk = nextps()
                        for k in range(KC):
                            m.mm(ps[:, :], hTb[:, k, ti * 128:(ti + 1) * 128], wvt[:, k, :], k == 0, k == KC - 1, ["wvt", hk], [pk])
                        v_ = vst[ti % 2]; vk = "vst%d" % (ti % 2)
                        m.cp("act" if ti % 2 else "dve", v_[:], ps[:, :], [pk], [vk])
                        r0 = tok0 + ti * 128
                        m.ld("pool", vslc_d[r0:r0 + 128, :], v_[:, 0:256], [vk], [])
                        m.ld("pool", vwin_d[r0:r0 + 128, :], v_[:, 256:512], [vk], [])
                    gl.append(g_)
            else:
                halo = (bi == 0)
                if not halo:
                    o0 = tok0 - OWN
                    for hg in range(4):
                        for pp in range(2):
                            def g_(hg=hg, pp=pp):
                                sg = stg[hg % 2]; sk = "stg%d" % (hg % 2)
                                c0 = (hg * 4 + 2 * pp) * 64
                                ps, pk = nextps()
                                for k in range(KC):
                                    m.mm(ps[:, 0:ntok], wq[:, k, c0:c0 + 128], hTb[:, k, 0:ntok], k == 0, k == KC - 1,
                                         ["wq", hk], [pk])
                                m.act(sg[:, pp, 0:ntok], ps[:, 0:ntok], AF.Copy, [pk], [sk], scale=0.125)
                                if pp == 1:
                                    m.ld("pool", qT_d[hg * 4:(hg + 1) * 4].rearrange("h d t -> (h d) t").rearrange("(a p) t -> p a t", p=128)[:, :, o0:o0 + ntok],
                                         sg[:, 0:2, 0:ntok], [sk], [])
                            gl.append(g_)
                    for ti in range(ntile):
                        def g_(ti=ti):
                            ps, pk = nextps()
                            for k in range(KC):
                                m.mm(ps[:, 0:48], hTb[:, k, ti * 128:(ti + 1) * 128], wg[:, k, :], k == 0, k == KC - 1, ["wg", hk], [pk])
                            m.act(gst[:], ps[:, 0:48], AF.Sigmoid, [pk], ["gst"])
                            m.ld("pool", gates_d[o0 + ti * 128:o0 + (ti + 1) * 128, :], gst[:], ["gst"], [])
                        gl.append(g_)
                for cc in range(8):
                    def g_(cc=cc):
                        pa, pak = nextps()
                        pg, pgk = nextps()
                        for k in range(KC):
                            m.mm(pa[:, 0:ntok], wu[:, k, cc * 128:(cc + 1) * 128], hTb[:, k, 0:ntok], k == 0, k == KC - 1, ["wu", hk], [pak])
                        for k in range(KC):
                            m.mm(pg[:, 0:ntok], wu[:, k, 1024 + cc * 128:1024 + (cc + 1) * 128], hTb[:, k, 0:ntok], k == 0, k == KC - 1, ["wu", hk], [pgk])
                        s_ = sig[cc % 2]; sk2 = "sig%d" % (cc % 2)
                        v_ = vst[cc % 2]; vk = "vst%d" % (cc % 2)
                        m.act(s_[:, 0:ntok], pg[:, 0:ntok], AF.Sigmoid, [pgk], [sk2])
                        if halo:
                            m.ts("dve", s_[:, 0:ntok], s_[:, 0:ntok], hal[:, 0:1], None, ALU.mult, None, [sk2, "hal"], [sk2])
                        m.tt("dve", v_[:, 0:ntok], pa[:, 0:ntok], s_[:, 0:ntok], ALU.mult, [pak, sk2], [vk])
                        c0 = 0 if halo else 128 + (tok0 - OWN)
                        m.ld("pool", hconvT_d[cc, :, c0:c0 + ntok], v_[:, 0:ntok], [vk], [])
                    gl.append(g_)
            return gl

        for ti in range(blocks[0][1]):
            ln_tile(0, ti)
        tr_block(0)
        for bi in range(len(blocks)):
            gs = groups(bi)
            nxt = bi + 1 < len(blocks)
            nt_n = blocks[bi + 1][1] if nxt else 0
            npart = nt_n + 1
            bounds = [len(gs) * p // npart for p in range(npart + 1)]
            for p in range(npart):
                for g_ in gs[bounds[p]:bounds[p + 1]]:
                    g_()
                if p < nt_n:
                    ln_tile(bi + 1, p)
            if nxt:
                tr_block(bi + 1)
        ph.close()

    if "A1" in phases:
        phase_A(False)
    if "A2" in phases:
        phase_A(True)

    def phase_BC():
        ph = Phase()
        kcT = ph.sb("kcT", [128, 4, 512], BF16)
        vcx = ph.sb("vcx", [128, 4, 4, 193], BF16)
        m.op("dve", lambda e: e.memset(kcT[:], 0.0), [], ["kcT"])
        m.op("dve", lambda e: e.memset(vcx[:], 0.0), [], ["vcx"])
        for h in range(4):
            m.op("dve", lambda e, h=h: e.memset(vcx[:, :, h, 64:65], 1.0), [], ["vcx"])
            m.ld("pool", vcx[:, :, h, 65:193], ov_c.rearrange("(t p) j -> p t j", p=128), [], ["vcx"])
        pb_ = Phase()
        w1s = pb_.sb("w1s", [64, 32, 256], BF16)
        w2s = pb_.sb("w2s", [128, 2, 64], BF16)
        pes = pb_.sb("pes", [64, 32], BF16)
        cj = pb_.sb("cj", [128, 2], F32)
        Tin = [pb_.sb("Tin%d" % i, [64, SV], BF16) for i in range(2)]
        T2 = [pb_.sb("T2_%d" % i, [64, 16, 512], BF16) for i in range(2)]
        gu = pb_.sb("gu", [128, 512], F32); g2 = pb_.sb("g2", [128, 512], F32)
        gT = pb_.sb("gT", [128, 2, 512], BF16)
        m.op("dve", lambda e: e.memset(gT[:], 0.0), [], ["gT"])
        for kv in range(2):
            m.ld("pool", w1s[:], (w1k, w1v)[kv].rearrange("d (r j) -> d r j", r=32), [], ["w1s"])
            m.ld("pool", w2s[:], (w2k, w2v)[kv].rearrange("(c p) d -> p c d", p=128), [], ["w2s"])
            m.ld("pool", pes[:], (pek, pev)[kv], [], ["pes"])
            for jc in range(2):
                for r in range(32):
                    m.mm(PS[0][:, jc * 2:jc * 2 + 2], w1s[:, r, jc * 128:(jc + 1) * 128], pes[:, r:r + 1].broadcast_to([64, 2]), r == 0, r == 31,
                         ["w1s", "pes"], ["ps0"], skip=True)
            m.cp("dve", cj[:], PS[0][:, 0:4:2], ["ps0"], ["cj"])
            for h in range(4):
                T_ = Tin[h % 2]; tk = "Tin%d" % (h % 2)
                m.ld("sp", T_[:], (kcmpT_d, vcmpT_d)[kv][h], [], [tk])
                t2 = T2[h % 2]; t2k = "T2_%d" % (h % 2)
                for r8 in range(0, 16, 8):
                    m.cp("dve" if r8 else "pool", t2[:, r8:r8 + 8, :], T_[:].rearrange("d (n r) -> d r n", r=16)[:, r8:r8 + 8, :], [tk], [t2k])
                for jc in range(2):
                    ps, pk = PS[1 + jc], PSK[1 + jc]
                    for r in range(32):
                        m.mm(ps[:, 0:511], w1s[:, r, jc * 128:(jc + 1) * 128], t2[:, r % 16, r // 16:r // 16 + 511], r == 0, r == 31,
                             ["w1s", t2k], [pk])
                    m.act(gu[:, 0:511], ps[:, 0:511], AF.Identity, [pk, "cj"], ["gu"], bias=cj[:, jc:jc + 1])
                    m.tt("dve", g2[:, 0:511], gu[:, 0:511], gu[:, 0:511], ALU.mult, ["gu"], ["g2"])
                    m.ts("dve", g2[:, 0:511], g2[:, 0:511], 0.044715, 1.0, ALU.mult, ALU.add, ["g2"], ["g2"])
                    m.tt("dve", g2[:, 0:511], g2[:, 0:511], gu[:, 0:511], ALU.mult, ["g2", "gu"], ["g2"])
                    m.act(g2[:, 0:511], g2[:, 0:511], AF.Tanh, ["g2"], ["g2"], scale=0.7978845608028654)
                    m.ts("dve", g2[:, 0:511], g2[:, 0:511], 1.0, 0.5, ALU.add, ALU.mult, ["g2"], ["g2"])
                    m.tt("dve", gT[:, jc, 0:511], g2[:, 0:511], gu[:, 0:511], ALU.mult, ["g2", "gu"], ["gT"])
                if kv == 0:
                    for jc in range(2):
                        m.mm(PS[3][0:64, 0:512], w2s[:, jc, :], gT[:, jc, :], jc == 0, jc == 1, ["w2s", "gT"], ["ps3"])
                    m.cp("dve", kcT[0:64, h, 0:511], PS[3][0:64, 0:511], ["ps3"], ["kcT"])
                else:
                    for bt in range(4):
                        for jc in range(2):
                            m.mm(PS[3][:, bt * 64:(bt + 1) * 64], gT[:, jc, bt * 128:(bt + 1) * 128], w2s[:, jc, :], jc == 0, jc == 1,
                                 ["w2s", "gT"], ["ps3"], skip=True)
                    m.cp("dve", vcx[:, :, h, 0:64], PS[3][:, 0:256].rearrange("p (t d) -> p t d", t=4), ["ps3"], ["vcx"])
        pb_.close()
        if "C" not in phases:
            ph.close()
            return
        tabs = ph.sb("tabs", [128, 2, 2048], BF16)
        anti = ph.sb("anti", [128, 512], BF16)
        bandt = ph.sb("bandt", [16, 2048], BF16)
        dwt = ph.sb("dwt", [16, 272], BF16)
        i4 = ph.sb("i4", [128, 512], BF16)
        f0 = ph.sb("f0", [128, 128], F32); b3 = ph.sb("b3", [128, 3], F32)
        kbias = ph.sb("kbias", [128, 64], F32); cbias = ph.sb("cbias", [128, 4], F32)
        with ExitStack() as es:
            raw = es.enter_context(nc.sbuf_tensor("raw_t", [128, 16, 128], F32))
            r31s = es.enter_context(nc.sbuf_tensor("r31s", [128, 16], F32))
            msk = es.enter_context(nc.sbuf_tensor("msk", [128, 128], F32))
            m.ld("sp", r31s[:], r31, [], ["r31s"])
            m.ld("sp", raw[:].rearrange("p h q -> p (h q)"), diag_raw, [], ["raw"])
            m.ld("sp", msk[:], diag_mask, [], ["msk"])
            for hd in range(16):
                m.stt(tabs[:, 0, hd * 128:(hd + 1) * 128], raw[:, hd, :], r31s[:, hd:hd + 1], msk[:], ALU.subtract, ALU.add, ["raw", "r31s", "msk"], ["tabs"])
            m.ld("sp", raw[:].rearrange("p h q -> p (h q)"), sub1_raw, [], ["raw"])
            for hd in range(16):
                m.ts("dve", tabs[:, 1, hd * 128:(hd + 1) * 128], raw[:, hd, :], r31s[:, hd:hd + 1], None, ALU.subtract, None, ["raw", "r31s"], ["tabs"])
            m.ld("sp", raw[0:16].rearrange("p h q -> p (h q)"), band_raw, [], ["raw"])
            m.ld("sp", msk[0:16, :], band_mask, [], ["msk"])
            for hd in range(16):
                m.stt(bandt[:, hd * 128:(hd + 1) * 128], raw[0:16, hd, :], r31s[0:16, hd:hd + 1], msk[0:16, :], ALU.subtract, ALU.add, ["raw", "r31s", "msk"], ["bandt"])
            m.ld("sp", msk[:], anti_mask, [], ["msk"])
            for g in range(4):
                m.cp("dve", anti[:, g * 128:(g + 1) * 128], msk[:], ["msk"], ["anti"])
                m.cp("dve", i4[:, g * 128:(g + 1) * 128], ident_f[:], ["ident_f"], ["i4"])
            m.barrier()
        m.ld("pool", dwt[:], dw_c, [], ["dwt"])
        m.ld("sp", f0[:], f0_c, [], ["f0"]); m.ld("sp", b3[:], band3_c, [], ["b3"])
        m.ld("sp", kbias[:], keybias_c, [], ["kbias"]); m.ld("sp", cbias[:], cmpbias_c, [], ["cbias"])

        kslc = ph.sb("kslc", [128, SV], BF16)
        kwin = ph.sb("kwin", [128, OWN + 512], BF16)
        vslc = ph.sb("vslc", [128, 64, 65], BF16)
        vwin = ph.sb("vwin", [128, 36, 65], BF16)
        qT = ph.sb("qT", [64, NT, 512], BF16)
        gts = ph.sb("gts", [128, NT, 12], F32)
        pT = [ph.sb("pT%d" % i, [128, 512], BF16) for i in range(6)]
        mneg = [ph.sb("mneg%d" % i, [128, 192], BF16) for i in range(2)]
        QM = [[ph.sb("QM%d%d" % (i, c), [128, 512], BF16) for c in range(2)] for i in range(3)]
        for i in range(2):
            m.op("pool", lambda e, i=i: e.memset(mneg[i][:], 0.0), [], ["mneg%d" % i])
        for i in range(3):
            for c in range(2):
                m.op("pool", lambda e, i=i, c=c: e.memset(QM[i][c][:], 0.0), [], ["QM%d" % i])
        m.op("pool", lambda e: e.memset(kwin[64:128, :], 0.0), [], ["kwin_hi"])
        for c4 in range(4):
            m.ld("pool", kslc[64:128, c4 * 2048:(c4 + 1) * 2048], asel_c[:, c4 * 2048:(c4 + 1) * 2048], [], ["kslc_hi"])
        sc = ph.sb("sc", [128, 128], F32); sc2 = ph.sb("sc2", [128, 128], F32)
        mx = ph.sb("mx", [128, 16], F32)
        acc = [ph.sb("acc%d" % i, [128, 3, 4, 65], F32) for i in range(2)]
        rinv = [ph.sb("rinv%d" % i, [128, 12], F32) for i in range(2)]
        ob = [ph.sb("ob%d" % i, [128, 256], BF16) for i in range(2)]
        of = ph.sb("of", [128, 4, 64], F32)
        m.op("dve", lambda e: e.memset(vslc[:, :, 64:65], 1.0), [], ["vslc"])
        m.op("dve", lambda e: e.memset(vwin[:, :, 64:65], 1.0), [], ["vwin"])
        SCB = [PS[0][:], PS[1][:], PS[2][:], PB[1][:].bitcast(F32)]
        SCK = ["ps0", "ps1", "ps2", "pb1"]
        WACC = PB[0][:].bitcast(F32)
        sidx = {"i": 0}
        pidx = {"i": 0}
        DEPTH = 3
        pend = []
        delayed = []

        def run_delayed():
            while delayed:
                delayed.pop(0)[1]()

        def flush_one():
            u = pend.pop(0)
            u[0]()
            for p in u[1]:
                p()

        def add_unit(kT_ap, kkeys, rows, qap, extra, bias_ap, bias_keys, accps, acck, v_ap, vkeys, ncol, first, last):
            si = sidx["i"] % 4; sidx["i"] += 1
            ps, pk = SCB[si], SCK[si]
            n_ex = len(extra)
            m.mm(ps[0:rows, :], kT_ap, qap, True, n_ex == 0, kkeys + ["qT"], [pk])
            for xi, (l_ap, r_ap, ks) in enumerate(extra):
                m.mm(ps[0:rows, :], l_ap, r_ap, False, xi == n_ex - 1, ks, [pk])

            def pv():
                pi = pidx["i"] % 6; pidx["i"] += 1
                p_, ppk = pT[pi], "pT%d" % pi
                m.act(p_[0:rows, :], ps[0:rows, :], AF.Exp, [pk] + bias_keys, [ppk], bias=bias_ap)
                for g in range(4):
                    m.mm(accps[:, g * ncol:(g + 1) * ncol] if ncol == 65 else accps[g // 2][:, (g % 2) * ncol:(g % 2 + 1) * ncol],
                         p_[0:rows, g * 128:(g + 1) * 128], v_ap, first and (g == 0 or (ncol != 65 and g == 2)), last and g == 3,
                         [ppk] + vkeys, acck, skip=True)

            pend.append((pv, []))
            if len(pend) > DEPTH:
                flush_one()
            for dl in list(delayed):
                dl[0] -= 1
                if dl[0] <= 0:
                    delayed.remove(dl)
                    dl[1]()

        def add_post(fn):
            if pend:
                pend[-1][1].append(fn)
            else:
                fn()

        for h in range(4):
            for g in range(4):
                m.ld("sp", qT[:].rearrange("d l (g q) -> d l g q", g=4)[:, :, g, :],
                     qT_d[h * 4 + g].rearrange("d (l q) -> d l q", q=128), [], ["qT"])
            m.ld("sp", gts[:], gates_d[:, h * 12:(h + 1) * 12].rearrange("(t p) c -> p t c", p=128), [], ["gts"])
            m.ld("sp", kwin[0:64, :], kwinT_d[h][:, OWN - 512:SV], [], ["kwin"])
            for t8 in range(4):
                m.ld("sp", vwin[:, t8 * 9:(t8 + 1) * 9, 0:64],
                     vwin_d[OWN - 512 + t8 * 1152:OWN - 512 + (t8 + 1) * 1152, h * 64:(h + 1) * 64].rearrange("(t p) d -> p t d", p=128), [], ["vwin"])
            m.ld("sp", kslc[0:64, :], kslcT_d[h], [], ["kslc"])
            for t8 in range(8):
                m.ld("sp", vslc[:, t8 * 8:(t8 + 1) * 8, 0:64],
                     vslc_d[t8 * 1024:(t8 + 1) * 1024, h * 64:(h + 1) * 64].rearrange("(t p) d -> p t d", p=128), [], ["vslc"])

            def emit_cmp(lt, h=h):
                t = 32 + lt
                qmk = "QM%d" % (lt % 3)
                for l2 in ([0, 1] if lt == 0 else [lt + 1]):
                    if l2 < NT:
                        for c in range(2):
                            m.cp("pool", QM[l2 % 3][c][0:64, :], qT[:, l2, :], ["qT"], ["QM%d" % (l2 % 3)])
                qap = QM[lt % 3][0][:]
                a_ = acc[lt % 2]; ak = "acc%d" % (lt % 2)
                rv = rinv[lt % 2]; rvk = "rinv%d" % (lt % 2)
                n_c = 8 * t + 7
                ntile = (n_c + 127) // 128
                b0 = 8 * t - 9
                jb, off = b0 // 128, b0 % 128
                for J in range(ntile):
                    rows = min(128, n_c - 128 * J)
                    extra = []
                    if J == jb:
                        extra.append((dwt[:, 128 - off:128 - off + rows], bandt[:, h * 512:(h + 1) * 512], ["dwt", "bandt"]))
                    elif J == jb + 1:
                        extra.append((dwt[:, 256 - off:256 - off + rows], bandt[:, h * 512:(h + 1) * 512], ["dwt", "bandt"]))
                    add_unit(kcT[:, h, J * 128:J * 128 + rows], ["kcT", qmk], rows, qap, extra, (cbias[0:rows, J:J + 1] if J < 2 else None), ["cbias"],
                             (PS[4], PS[5]), ["ps4", "ps5"], vcx[0:rows, J, h, :], ["vcx"], 193, J == 0, J == ntile - 1)

                def post():
                    for half in range(2):
                        pc = PS[4 + half]
                        m.cp("dve", a_[:, 0, half * 2:half * 2 + 2, :], pc[:, 0:386].rearrange("p (g c) -> p g c", g=2)[:, :, 0:65],
                             ["ps%d" % (4 + half)], [ak])
                    m.ts("dve", rv[:, 0:4], a_[:, 0, :, 64], 1e-30, None, ALU.max, None, [ak], [rvk])
                    m.op("dve", lambda e: e.reciprocal(out=rv[:, 0:4], in_=rv[:, 0:4]), [rvk], [rvk])
                    for g in range(4):
                        pc = PS[4 + g // 2]
                        u_ap = pc[:, (g % 2) * 193 + 65:(g % 2) * 193 + 193]
                        if g == 0:
                            m.stt(sc[:], u_ap, rv[:, 0:1], f0[:], ALU.mult, ALU.add, ["ps4", rvk, "f0"], ["sc"])
                        else:
                            m.stt(sc[:], u_ap, rv[:, g:g + 1], sc[:], ALU.mult, ALU.add, ["ps%d" % (4 + g // 2), rvk, "sc"], ["sc"])
                    ncol = 2 * t + 2
                    m.tt("dve", sc[:, 2 * t - 1:2 * t + 2], sc[:, 2 * t - 1:2 * t + 2], b3[:], ALU.add, ["sc", "b3"], ["sc"])
                    m.op("dve", lambda e: e.max(out=mx[:, 0:8], in_=sc[:, 0:ncol]), ["sc"], ["mx"])
                    m.op("dve", lambda e: e.match_replace(out=sc2[:, 0:ncol], in_to_replace=mx[:, 0:8], in_values=sc[:, 0:ncol], imm_value=-1e9),
                         ["sc", "mx"], ["sc2"])
                    m.op("dve", lambda e: e.max(out=mx[:, 8:16], in_=sc2[:, 0:ncol]), ["sc2"], ["mx"])
                    mn = mneg[lt % 2]; mk_ = "mneg%d" % (lt % 2)
                    m.ts("dve", sc2[:, 0:ncol], sc[:, 0:ncol], mx[:, 15:16], -NEG, ALU.is_ge, ALU.mult, ["sc", "mx"], ["sc2"])
                    m.ts("dve", mn[:, 64:64 + ncol], sc2[:, 0:ncol], NEG, None, ALU.add, None, ["sc2"], [mk_])

                    def post_b():
                        for c in range(2):
                            m.mm(PS[4 + c][:, :], mn[:, 64 * c:64 * c + 128], i4[:], True, True, [mk_, "i4"], ["ps%d" % (4 + c)])
                            m.cp("dve", QM[lt % 3][c][64:128, :], PS[4 + c][64:128, :], ["ps%d" % (4 + c)], [qmk])
                    delayed.append([10, post_b])
                add_post(post)

            def emit_win(lt, h=h):
                t = 32 + lt
                qmk = "QM%d" % (lt % 3)
                qap = QM[lt % 3][0][:]
                a_ = acc[lt % 2]; ak = "acc%d" % (lt % 2)
                for j in range(t - 4, t + 1):
                    extra = []
                    if j == t:
                        extra.append((ident_b[:], tabs[:, 0, h * 512:(h + 1) * 512], ["ident_b", "tabs"]))
                    elif j == t - 1:
                        extra.append((ident_b[:], tabs[:, 1, h * 512:(h + 1) * 512], ["ident_b", "tabs"]))
                    elif j == t - 4:
                        extra.append((ident_b[:], anti[:], ["ident_b", "anti"]))
                    jl = j - 28
                    add_unit(kwin[:, jl * 128:(jl + 1) * 128], ["kwin", "kwin_hi", qmk], 128, qap, extra, (kbias[:, j:j + 1] if j < 32 else None), ["kbias"],
                             WACC, ["pb0"], vwin[:, jl, :], ["vwin"], 65, j == t - 4, j == t)
                add_post(lambda: m.cp("dve", a_[:, 2, :, :], WACC[:, 0:260].rearrange("p (g c) -> p g c", g=4), ["pb0"], [ak]))

            def emit_sel(lt, h=h):
                t = 32 + lt
                qmk = "QM%d" % (lt % 3)
                a_ = acc[lt % 2]; ak = "acc%d" % (lt % 2)
                rv = rinv[lt % 2]; rvk = "rinv%d" % (lt % 2)
                if lt == 0:
                    while pend:
                        flush_one()
                    run_delayed()
                for j in range(t + 1):
                    qap = QM[lt % 3][j // 32][:]
                    extra = []
                    if j == t:
                        extra.append((ident_b[:], tabs[:, 0, h * 512:(h + 1) * 512], ["ident_b", "tabs"]))
                    elif j == t - 1:
                        extra.append((ident_b[:], tabs[:, 1, h * 512:(h + 1) * 512], ["ident_b", "tabs"]))
                    add_unit(kslc[:, j * 128:(j + 1) * 128], ["kslc", "kslc_hi", qmk], 128, qap, extra, (kbias[:, j:j + 1] if j < 32 else None), ["kbias"],
                             PS[3], ["ps3"], vslc[:, j, :], ["vslc"], 65, j == 0, j == t)

                def post():
                    m.cp("dve", a_[:, 1, :, :], PS[3][:, 0:260].rearrange("p (g c) -> p g c", g=4), ["ps3"], [ak])
                    m.ts("dve", rv[:, 4:12].rearrange("p (b g) -> p b g", b=2), a_[:, 1:3, :, 64], 1e-30, None, ALU.max, None, [ak], [rvk])
                    m.op("dve", lambda e: e.reciprocal(out=rv[:, 4:12], in_=rv[:, 4:12]), [rvk], [rvk])
                    m.tt("dve", rv[:].rearrange("p (b g) -> p b g", b=3), rv[:].rearrange("p (b g) -> p b g", b=3),
                         gts[:, lt, :].rearrange("p (g b) -> p b g", b=3), ALU.mult, [rvk, "gts"], [rvk])
                    o_ = ob[lt % 2]; ok = "ob%d" % (lt % 2)
                    for g in range(4):
                        m.ts("dve", of[:, g, :], a_[:, 0, g, 0:64], rv[:, g:g + 1], None, ALU.mult, None, [ak, rvk], ["of"])
                        m.stt(of[:, g, :], a_[:, 1, g, 0:64], rv[:, 4 + g:5 + g], of[:, g, :], ALU.mult, ALU.add, [ak, rvk, "of"], ["of"])
                        m.stt(o_[:, g * 64:(g + 1) * 64], a_[:, 2, g, 0:64], rv[:, 8 + g:9 + g], of[:, g, :], ALU.mult, ALU.add, [ak, rvk, "of"], [ok])
                    m.ld("sp", o_d[lt * 128:(lt + 1) * 128, h * 256:(h + 1) * 256], o_[:], [ok], [])
                add_post(post)

            emit_cmp(0)
            for lt in range(NT):
                emit_win(lt)
                if lt + 1 < NT:
                    emit_cmp(lt + 1)
                emit_sel(lt)
            while pend:
                flush_one()
            run_delayed()
        ph.close()

    if "B" in phases:
        phase_BC()

    def phase_D():
        ph = Phase()
        dg = ph.sb("dg", [128, 8, 31, 128], BF16)
        wc = ph.sb("wc", [128, 8, 31], F32)
        cv = ph.sb("cv", [128, 24], F32)
        hc = ph.sb("hc", [128, 8, 128 + OWN], BF16)
        yb = ph.sb("yb", [128, 8, 512], F32)
        ysq = ph.sb("ysq", [128, 512], F32)
        mean = ph.sb("mean", [128, 512], F32); rstd = ph.sb("rstd", [128, 512], F32)
        z = [ph.sb("z%d" % i, [128, 512], F32) for i in range(4)]
        zo = [ph.sb("zo%d" % i, [128, 512], BF16) for i in range(4)]
        m.ld("sp", wc[:].rearrange("p c k -> p (c k)"), dww, [], ["wc"])
        m.ld("sp", cv[:], cvec, [], ["cv"])
        for cc in range(8):
            m.ld("sp", hc[:, cc, :], hconvT_d[cc], [], ["hc"])
            for k in range(31):
                if k % 2:
                    m.ts("dve", dg[:, cc, k, :], ident_f[:], wc[:, cc, k:k + 1], None, ALU.mult, None, ["ident_f", "wc"], ["dg"])
                else:
                    m.act(dg[:, cc, k, :], ident_f[:], AF.Copy, ["ident_f", "wc"], ["dg"], scale=wc[:, cc, k:k + 1])
        for blk in range(8):
            for cc in range(8):
                ps, pk = PS[cc % 4], PSK[cc % 4]
                for k in range(31):
                    c0 = 128 + blk * 512 - 30 + k
                    m.mm(ps[:, :], dg[:, cc, k, :], hc[:, cc, c0:c0 + 512], k == 0, k == 30, ["dg", "hc"], [pk])
                m.act(yb[:, cc, :], ps[:, :], AF.Identity, [pk, "cv"], ["yb%d" % cc], bias=cv[:, cc:cc + 1])
                m.act(ysq[:], yb[:, cc, :], AF.Square, ["yb%d" % cc], ["ysq"])
                m.mm(PS[4][:, :], ones_f[:], yb[:, cc, :], cc == 0, cc == 7, ["ones_f", "yb%d" % cc], ["ps4"])
                m.mm(PS[5][:, :], ones_f[:], ysq[:], cc == 0, cc == 7, ["ones_f", "ysq"], ["ps5"])
            m.act(mean[:], PS[4][:, :], AF.Copy, ["ps4"], ["mean"], scale=1.0 / 1024)
            m.tt("dve", rstd[:], mean[:], mean[:], ALU.mult, ["mean"], ["rstd"])
            m.stt(rstd[:], PS[5][:, :], 1.0 / 1024, rstd[:], ALU.mult, ALU.subtract, ["ps5", "rstd"], ["rstd"])
            m.act(rstd[:], rstd[:], AF.Sqrt, ["rstd"], ["rstd"], bias=EPS)
            m.op("dve", lambda e: e.reciprocal(out=rstd[:], in_=rstd[:]), ["rstd"], ["rstd"])
            for cc in range(8):
                z_ = z[cc % 4]; zk = "z%d" % (cc % 4)
                zo_ = zo[cc % 4]; zok = "zo%d" % (cc % 4)
                m.tt("dve", z_[:], yb[:, cc, :], mean[:], ALU.subtract, ["yb%d" % cc, "mean"], [zk])
                m.tt("pool", z_[:], z_[:], rstd[:], ALU.mult, [zk, "rstd"], [zk])
                m.act(zo_[:], z_[:], AF.Silu, [zk, "cv"], [zok], bias=cv[:, 16 + cc:17 + cc], scale=cv[:, 8 + cc:9 + cc])
                m.ld("sp", oconvT_d[cc, :, blk * 512:(blk + 1) * 512], zo_[:], [zok], [])
        ph.close()

    if "D" in phases:
        phase_D()

    def load_w_bf(ph, name, src, ncols, q="pool"):
        wt = ph.sb(name, [128, KC, ncols], BF16)
        for k4 in range(0, KC, 4):
            m.ld(q, wt[:, k4:k4 + 4, :], src[k4 * 128:(k4 + 4) * 128, :].rearrange("(k p) n -> p k n", p=128), [], [name])
        return wt

    def phase_E1():
        ph = Phase()
        tm = ln_tmps(ph)
        wo_ = load_w_bf(ph, "w_o", w_out, D)
        lnrep2 = ph.sb("lnrep2", [128, 2, D], F32)
        m.ld("sp", lnrep2[:, 0, :], lnv[2:3, :].broadcast_to([128, D]), [], ["lnrep2"])
        m.ld("sp", lnrep2[:, 1, :], lnv[3:4, :].broadcast_to([128, D]), [], ["lnrep2"])
        load_ln(0)
        ot = [ph.sb("ot%d" % i, [128, 1024], BF16) for i in range(2)]
        mixT = [ph.sb("mixT%d" % i, [128, KC, 128], BF16) for i in range(2)]
        xt = [ph.sb("e1x%d" % i, [128, D], F32) for i in range(2)]
        y = [ph.sb("e1y%d" % i, [128, D], F32) for i in range(2)]
        yb_ = [ph.sb("e1yb%d" % i, [128, D], BF16) for i in range(2)]
        xT = [ph.sb("e1xT%d" % i, [128, KC, 128], BF16) for i in range(2)]
        def e1_part2(lt):
            i2 = lt % 2
            transpose_to(xT[i2], "e1xT%d" % i2, yb_[i2], ["e1yb%d" % i2], 0)
            m.ld("pool", x1T_d.rearrange("c p t -> p c t")[:, :, lt * 128:(lt + 1) * 128], xT[i2][:], ["e1xT%d" % i2], [])

        def e1_stage0(lt):
            i2 = lt % 2
            m.ld("sp", ot[i2][:], o_d[lt * 128:(lt + 1) * 128, :], [], ["ot%d" % i2])
            transpose_to(mixT[i2], "mixT%d" % i2, ot[i2], ["ot%d" % i2], 0, nk=8)
            m.ld("sp", mixT[i2][:, 8:16, :], oconvT_d.rearrange("c p t -> p c t")[:, :, lt * 128:(lt + 1) * 128], [], ["mixT%d" % i2])
            m.ld("sp", xt[i2][:], xv[OWN + lt * 128:OWN + (lt + 1) * 128, :], [], ["e1x%d" % i2])
            layer_norm(tm, xt[i2][:], ["e1x%d" % i2], xt[i2][:], ["e1x%d" % i2])

        e1_stage0(0)
        for lt in range(NT):
            i2 = lt % 2
            if lt + 1 < NT:
                e1_stage0(lt + 1)
            for oc in range(4):
                ps, pk = PS[oc], PSK[oc]
                for k in range(KC):
                    m.mm(ps[:, :], mixT[i2][:, k, :], wo_[:, k, oc * 512:(oc + 1) * 512], k == 0, k == KC - 1, ["mixT%d" % i2, "w_o"], [pk])
                m.stt(y[i2][:, oc * 512:(oc + 1) * 512], xt[i2][:, oc * 512:(oc + 1) * 512], ALPHA, ps[:, :], ALU.mult, ALU.add,
                      ["e1x%d" % i2, pk], ["e1y%d" % i2])
            ln_generic(tm, y[i2][:], ["e1y%d" % i2], lnrep2, "lnrep2")
            m.ld("pool", x1_d[lt * 128:(lt + 1) * 128, :], y[i2][:], ["e1y%d" % i2], [])
            m.cp("pool", yb_[i2][:], y[i2][:], ["e1y%d" % i2], ["e1yb%d" % i2])
            if lt > 0:
                e1_part2(lt - 1)
        e1_part2(NT - 1)
        ph.close()

    def ln_generic(tm, src, src_keys, rep, repk, np_=128, last_eng="pool"):
        st, mv, rs, nb = tm
        for c in range(4):
            m.op("dve", lambda e, c=c: e.bn_stats(out=st[:np_, c, :], in_=src[:, c * 512:(c + 1) * 512]), src_keys, ["ln_st"])
        m.op("dve", lambda e: e.bn_aggr(out=mv[:np_, :], in_=st[:np_, :, :]), ["ln_st"], ["ln_mv"])
        m.act(rs[:np_, :], mv[:np_, 1:2], AF.Sqrt, ["ln_mv"], ["ln_rs"], bias=EPS)
        m.op("dve", lambda e: e.reciprocal(out=rs[:np_, :], in_=rs[:np_, :]), ["ln_rs"], ["ln_rs"])
        m.stt(nb[:np_, :], mv[:np_, 0:1], -1.0, rs[:np_, :], ALU.mult, ALU.mult, ["ln_mv", "ln_rs"], ["ln_nb"])
        m.act(src, src, AF.Identity, src_keys + ["ln_rs", "ln_nb"], src_keys, bias=nb[:np_, :], scale=rs[:np_, :])
        m.tt("dve", src, src, rep[:np_, 0, :], ALU.mult, src_keys + [repk], src_keys)
        m.tt(last_eng, src, src, rep[:np_, 1, :], ALU.add, src_keys + [repk], src_keys)

    if "E1" in phases:
        phase_E1()

    def phase_E2():
        ph = Phase()
        tm = ln_tmps(ph)
        KmT = ph.sb("KmT", [128, KC, 256], BF16)
        Vm = ph.sb("Vm", [128, 2, D], BF16)
        load_ln(4)
        pm = Phase()
        mt_ = pm.sb("mt_", [128, D], F32); mb_ = pm.sb("mb_", [128, D], BF16)
        mT = pm.sb("mT", [128, KC, 256], BF16)
        wkv = [pm.sb("wkv%d" % i, [128, KC, 512], BF16) for i in range(2)]
        wkvf = pm.sb("wkvf", [128, KC, 512], F32)
        for ti in range(2):
            m.ld("sp", mt_[:], memb[ti * 128:(ti + 1) * 128, :], [], ["mt_"])
            layer_norm(tm, mt_[:], ["mt_"], mb_[:], ["mb_"])
            transpose_to(mT, "mT", mb_, ["mb_"], ti)
        for cb in range(8):
            w_ = wkv[cb % 2]; wk_ = "wkv%d" % (cb % 2)
            if cb % 2 == 0:
                for k4 in range(0, KC, 4):
                    m.ld("pool", w_[:, k4:k4 + 4, :],
                         xa_wkv[k4 * 128:(k4 + 4) * 128, cb * 512:(cb + 1) * 512].rearrange("(k p) n -> p k n", p=128), [], [wk_])
            else:
                for k4 in range(0, KC, 4):
                    m.ld("sp", wkvf[:, k4:k4 + 4, :],
                         xa_wkv[k4 * 128:(k4 + 4) * 128, cb * 512:(cb + 1) * 512].rearrange("(k p) n -> p k n", p=128), [], ["wkvf%d" % k4])
                    m.cp("dve" if (k4 // 4) % 2 else "act", w_[:, k4:k4 + 4, :], wkvf[:, k4:k4 + 4, :], ["wkvf%d" % k4], [wk_])
            if cb < 4:
                for sub in range(4):
                    ps, pk = PS[sub], PSK[sub]
                    for k in range(KC):
                        m.mm(ps[:, 0:256], w_[:, k, sub * 128:(sub + 1) * 128], mT[:, k, :], k == 0, k == KC - 1, [wk_, "mT"], [pk])
                    m.cp("dve" if sub % 2 else "act", KmT[:, cb * 4 + sub, :], ps[:, 0:256], [pk], ["KmT"])
            else:
                for ti in range(2):
                    ps, pk = PS[ti], PSK[ti]
                    for k in range(KC):
                        m.mm(ps[:, :], mT[:, k, ti * 128:(ti + 1) * 128], w_[:, k, :], k == 0, k == KC - 1, [wk_, "mT"], [pk])
                    m.cp("dve" if ti else "act", Vm[:, ti, (cb - 4) * 512:(cb - 3) * 512], ps[:, :], [pk], ["Vm"])
        pm.close()
        wq_ = load_w_bf(ph, "xwq", xa_wq, D)
        xT = [ph.sb("e2xT%d" % i, [128, KC, 512], BF16) for i in range(2)]
        qx = ph.sb("qx", [128, KC, 512], BF16)
        pT = [ph.sb("e2pT%d" % i, [128, 2, 512], BF16) for i in range(2)]
        rinv = ph.sb("e2rinv", [128, 512], F32)
        ox = [ph.sb("e2ox0", [128, KC, 512], BF16)] * 2
        sc_ = 512 ** -0.5
        for gi in range(8):
            i2 = gi % 2
            xk = "e2xT%d" % i2
            m.ld("sp", xT[i2][:], x1T_d.rearrange("c p t -> p c t")[:, :, gi * 512:(gi + 1) * 512], [], [xk])
            for oc in range(KC):
                ps, pk = PS[oc % 4], PSK[oc % 4]
                for k in range(KC):
                    m.mm(ps[:, :], wq_[:, k, oc * 128:(oc + 1) * 128], xT[i2][:, k, :], k == 0, k == KC - 1, ["xwq", xk], [pk])
                m.cp("dve" if oc % 2 else "act", qx[:, oc, :], ps[:, :], [pk], ["qx%d" % oc])
            for hd in range(4):
                p_ = pT[hd % 2]; ppk = "e2pT%d" % (hd % 2)
                for mt in range(2):
                    ps, pk = PS[mt], PSK[mt]
                    for kk in range(4):
                        m.mm(ps[:, :], KmT[:, hd * 4 + kk, mt * 128:(mt + 1) * 128], qx[:, hd * 4 + kk, :], kk == 0, kk == 3,
                             ["KmT", "qx%d" % (hd * 4 + kk)], [pk])
                    m.act(p_[:, mt, :], ps[:, :], AF.Exp, [pk], [ppk], scale=sc_)
                for mt in range(2):
                    m.mm(PS[2][:, :], ones_b[:], p_[:, mt, :], mt == 0, mt == 1, ["ones_b", ppk], ["ps2"])
                m.op("dve", lambda e: e.reciprocal(out=rinv[:], in_=PS[2][:, :]), ["ps2"], ["e2rinv"])
                for dc in range(4):
                    ps, pk = PS[3 + dc % 3], PSK[3 + dc % 3]
                    for mt in range(2):
                        m.mm(ps[:, :], Vm[:, mt, (hd * 4 + dc) * 128:(hd * 4 + dc + 1) * 128], p_[:, mt, :], mt == 0, mt == 1, ["Vm", ppk], [pk])
                    m.tt("dve", ox[i2][:, hd * 4 + dc, :], ps[:, :], rinv[:], ALU.mult, [pk, "e2rinv"], ["e2ox0"])
            m.ld("pool", oxT_d.rearrange("c p t -> p c t")[:, :, gi * 512:(gi + 1) * 512], ox[i2][:], ["e2ox0"], [])
        ph.close()

    if "E2" in phases:
        phase_E2()

    def phase_E3():
        ph = Phase()
        tm = ln_tmps(ph)
        wo_ = load_w_bf(ph, "xwo", xa_wo, D)
        load_ln(6)
        rws = ph.sb("rws", [128, KC, 36], F32)
        rbs = ph.sb("rbs", [128, 36], F32)
        ut = ph.sb("ut", [128, 128], BF16)
        ecs = ph.sb("ecs", [128, 32], F32)
        carry = ph.sb("carry", [128, 32], F32)
        m.ld("sp", rws[:], rw.rearrange("(c p) j -> p c j", p=128), [], ["rws"])
        m.ld("sp", rbs[:], rb.broadcast_to([128, 36]), [], ["rbs"])
        m.ld("pool", ut[:], ut_c, [], ["ut"])
        m.ld("sp", ecs[:], ec_c, [], ["ecs"])
        m.op("dve", lambda e: e.memset(carry[:], 0.0), [], ["carry"])
        oT = [ph.sb("e3oT%d" % i, [128, KC, 128], BF16) for i in range(2)]
        x1 = [ph.sb("e3x1%d" % i, [128, D], F32) for i in range(2)]
        y = [ph.sb("e3y%d" % i, [128, D], F32) for i in range(2)]
        yb_ = [ph.sb("e3yb%d" % i, [128, D], BF16) for i in range(2)]
        yT = ph.sb("e3yT", [128, KC, 128], F32)
        lg = ph.sb("lg", [128, 36], F32)
        sm = ph.sb("sm", [128, 16], F32)
        oh = ph.sb("oh", [128, 4, 32], F32)
        ohb = ph.sb("ohb", [128, 32], BF16)
        lem = ph.sb("lem", [128, 32], F32)
        mx8 = ph.sb("mx8", [128, 8], F32)
        rk = ph.sb("rk", [128, 32], F32)
        rto = [ph.sb("rto%d" % i, [128, 4], F32) for i in range(2)]
        dsti = [ph.sb("dsti%d" % i, [128, 2], I32) for i in range(2)]
        def e3_part2a(lt):
            i2 = lt % 2
            yk = "e3y%d" % i2
            for k0 in range(0, KC, 4):
                ps, pk = PS[4 + (k0 // 4) % 2], PSK[4 + (k0 // 4) % 2]
                for kk in range(4):
                    m.tr(ps[:, kk * 128:(kk + 1) * 128], y[i2][:, (k0 + kk) * 128:(k0 + kk + 1) * 128], ident_f[:], [yk, "ident_f"], [pk])
                m.cp("act", yT[:, k0:k0 + 4, :], ps[:, :].rearrange("p (k t) -> p k t", k=4), [pk], ["e3yT"])

        def e3_part2(lt):
            i2 = lt % 2
            yk = "e3y%d" % i2
            for k in range(KC):
                m.mm(PS[4][:, 0:36], yT[:, k, :], rws[:, k, :], k == 0, k == KC - 1, ["e3yT", "rws"], ["ps4"])
            m.tt("dve", lg[:], PS[4][:, 0:36], rbs[:], ALU.add, ["ps4", "rbs"], ["lg"])
            m.op("dve", lambda e: e.tensor_reduce(out=sm[:, 0:1], in_=lg[:, 0:4], axis=AX.X, op=ALU.max), ["lg"], ["sm"])
            m.ts("dve", sm[:, 1:2], sm[:, 0:1], -1.0, None, ALU.mult, None, ["sm"], ["sm"])
            m.act(sm[:, 4:8], lg[:, 0:4], AF.Exp, ["lg", "sm"], ["sm"], bias=sm[:, 1:2], accum=sm[:, 2:3])
            m.op("dve", lambda e: e.reciprocal(out=sm[:, 3:4], in_=sm[:, 2:3]), ["sm"], ["sm"])
            m.ts("dve", sm[:, 8:12], lg[:, 0:4], sm[:, 0:1], None, ALU.is_ge, None, ["lg", "sm"], ["sm"])
            m.ts("dve", oh[:, 0, :].rearrange("p (g e) -> p g e", g=4), sm[:, 8:12].unsqueeze(2).broadcast_to([128, 4, 8]), 1.0, 1e4,
                 ALU.subtract, ALU.mult, ["sm"], ["oh"])
            m.tt("dve", lem[:], lg[:, 4:36], oh[:, 0, :], ALU.add, ["lg", "oh"], ["lem"])
            m.op("dve", lambda e: e.max(out=mx8[:], in_=lem[:]), ["lem"], ["mx8"])
            m.ts("dve", oh[:, 1, :], lem[:], mx8[:, 0:1], None, ALU.is_ge, None, ["lem", "mx8"], ["oh"])
            m.ts("dve", oh[:, 3, :], lem[:], mx8[:, 1:2], None, ALU.is_ge, None, ["lem", "mx8"], ["oh"])
            m.tt("dve", oh[:, 2, :], oh[:, 3, :], oh[:, 1, :], ALU.subtract, ["oh"], ["oh"])
            m.cp("dve", ohb[:], oh[:, 3, :], ["oh"], ["ohb"])
            m.tt("dve", sm[:, 12:13], mx8[:, 1:2], mx8[:, 0:1], ALU.subtract, ["mx8"], ["sm"])
            m.act(sm[:, 13:14], sm[:, 12:13], AF.Exp, ["sm"], ["sm"])
            m.ts("dve", sm[:, 14:15], sm[:, 13:14], 1.0, None, ALU.add, None, ["sm"], ["sm"])
            m.op("dve", lambda e: e.reciprocal(out=sm[:, 14:15], in_=sm[:, 14:15]), ["sm"], ["sm"])
            r_ = rto[i2]; rk_ = "rto%d" % i2
            m.tt("dve", r_[:, 2:3], sm[:, 14:15], sm[:, 3:4], ALU.mult, ["sm"], [rk_])
            m.tt("dve", r_[:, 3:4], sm[:, 13:14], r_[:, 2:3], ALU.mult, ["sm", rk_], [rk_])
            m.mm(PS[5][:, 0:32], ut[:], ohb[:], True, True, ["ut", "ohb"], ["ps5"])
            m.tt("dve", rk[:], PS[5][:, 0:32], carry[:], ALU.add, ["ps5", "carry"], ["rk"])
            m.tt("dve", rk[:], rk[:], ecs[:], ALU.add, ["rk", "ecs"], ["rk"])
            m.mm(PS[5][:, 32:64], ones_b[:], ohb[:], True, True, ["ones_b", "ohb"], ["ps5"])
            m.tt("dve", carry[:], carry[:], PS[5][:, 32:64], ALU.add, ["ps5", "carry"], ["carry"])
            for kq in range(2):
                m.tt("dve", oh[:, 0, :], oh[:, 1 + kq, :], rk[:], ALU.mult, ["oh", "rk"], ["oh"])
                m.op("dve", lambda e, kq=kq, r_=r_: e.tensor_reduce(out=r_[:, kq:kq + 1], in_=oh[:, 0, :], axis=AX.X, op=ALU.add), ["oh"], [rk_])
            d_ = dsti[i2]; dk = "dsti%d" % i2
            m.cp("dve", d_[:], r_[:, 0:2], [rk_], [dk])
            m.ld("pool", rt_d[lt * 128:(lt + 1) * 128, :], r_[:], [rk_], [])
            for kq in range(2):
                m.dma("pool", lambda e, kq=kq, d_=d_, yy=yb_[i2]: e.indirect_dma_start(
                    out=xs_d, out_offset=bass.IndirectOffsetOnAxis(ap=d_[:, kq:kq + 1], axis=0), in_=yy[:], in_offset=None),
                    ["e3yb%d" % i2, dk], ["xs_d"])

        for lt in range(NT):
            i2 = lt % 2
            m.ld("sp", oT[i2][:], oxT_d.rearrange("c p t -> p c t")[:, :, lt * 128:(lt + 1) * 128], [], ["e3oT%d" % i2])
            m.ld("sp", x1[i2][:], x1_d[lt * 128:(lt + 1) * 128, :], [], ["e3x1%d" % i2])
            if lt > 0:
                e3_part2a(lt - 1)
            for oc in range(4):
                ps, pk = PS[oc], PSK[oc]
                for k in range(KC):
                    m.mm(ps[:, :], oT[i2][:, k, :], wo_[:, k, oc * 512:(oc + 1) * 512], k == 0, k == KC - 1, ["e3oT%d" % i2, "xwo"], [pk])
                m.stt(y[i2][:, oc * 512:(oc + 1) * 512], x1[i2][:, oc * 512:(oc + 1) * 512], ALPHA, ps[:, :], ALU.mult, ALU.add,
                      ["e3x1%d" % i2, pk], ["e3y%d" % i2])
            yk = "e3y%d" % i2
            ln_generic(tm, y[i2][:], [yk], lnrep, "lnrep", last_eng="dve")
            m.ld("pool", x2_d[lt * 128:(lt + 1) * 128, :], y[i2][:], [yk], [])
            m.cp("dve", yb_[i2][:], y[i2][:], [yk], ["e3yb%d" % i2])
            if lt > 0:
                e3_part2(lt - 1)
        e3_part2a(NT - 1)
        e3_part2(NT - 1)
        ph.close()

    if "E3" in phases:
        phase_E3()

    def phase_F():
        ph = Phase()
        wg_ = [ph.sb("fwg%d" % i, [128, KC, 512], BF16) for i in range(2)]
        wu_ = [ph.sb("fwu%d" % i, [128, KC, 512], BF16) for i in range(2)]
        wd_ = [ph.sb("fwd%d" % i, [128, 4, D], BF16) for i in range(2)]
        xr = [ph.sb("fxr%d" % i, [128, D], BF16) for i in range(2)]
        xsT = [ph.sb("fxsT%d" % i, [128, KC, CAP], BF16) for i in range(2)]
        hT = ph.sb("fhT", [128, 4, CAP], BF16)
        sg = [ph.sb("fsg%d" % i, [128, CAP], F32) for i in range(2)]
        yo = [ph.sb("fyo%d" % i, [128, D], F32) for i in range(2)]
        wdf = ph.sb("fwdf", [128, 2, D], F32)
        cnt = {"i": 0}

        def f_loadw(e_):
            i2 = e_ % 2
            for k8 in range(0, KC, 8):
                m.ld("pool", wg_[i2][:, k8:k8 + 8, :], mwg[e_, k8 * 128:(k8 + 8) * 128, :].rearrange("(k p) n -> p k n", p=128), [], ["fwg%d" % i2])
            for k8 in range(0, KC, 8):
                m.ld("pool", wu_[i2][:, k8:k8 + 8, :], mwu[e_, k8 * 128:(k8 + 8) * 128, :].rearrange("(k p) n -> p k n", p=128), [], ["fwu%d" % i2])
            for k2 in range(0, 4, 2):
                for kk in range(2):
                    m.ld("sp", wdf[:, kk, :], mwd[e_, (k2 + kk) * 128:(k2 + kk + 1) * 128, :], [], ["fwdf%d" % kk])
                    m.cp("pool", wd_[i2][:, k2 + kk, :], wdf[:, kk, :], ["fwdf%d" % kk], ["fwd%d" % i2])

        def f_xsT(e_):
            i2 = e_ % 2
            for rt in range(4):
                x_ = xr[rt % 2]; xk = "fxr%d" % (rt % 2)
                m.ld("sp", x_[:], xs_d[e_ * CAP + rt * 128:e_ * CAP + (rt + 1) * 128, :], ["xs_d"], [xk])
                transpose_to(xsT[i2], "fxsT%d" % i2, x_, [xk], rt, only="dve")

        f_loadw(0)
        f_xsT(0)
        for e_ in range(NE):
            i2 = e_ % 2
            if e_ + 1 < NE:
                f_loadw(e_ + 1)
            for hc in range(4):
                pg, pgk = PS[(hc % 2) * 2], PSK[(hc % 2) * 2]
                pu, puk = PS[(hc % 2) * 2 + 1], PSK[(hc % 2) * 2 + 1]
                for k in range(KC):
                    m.mm(pg[:, :], wg_[i2][:, k, hc * 128:(hc + 1) * 128], xsT[i2][:, k, :], k == 0, k == KC - 1, ["fwg%d" % i2, "fxsT%d" % i2], [pgk])
                for k in range(KC):
                    m.mm(pu[:, :], wu_[i2][:, k, hc * 128:(hc + 1) * 128], xsT[i2][:, k, :], k == 0, k == KC - 1, ["fwu%d" % i2, "fxsT%d" % i2], [puk])
                s_ = sg[hc % 2]; sk = "fsg%d" % (hc % 2)
                m.act(s_[:], pg[:, :], AF.Silu, [pgk], [sk])
                m.tt("dve", hT[:, hc, :], pu[:, :], s_[:], ALU.mult, [puk, sk], ["fhT%d" % hc])
            if e_ + 1 < NE:
                f_xsT(e_ + 1)
            for rt in range(4):
                y_ = yo[rt % 2]; yk = "fyo%d" % (rt % 2)
                for oc in range(4):
                    pi = 4 + cnt["i"] % 2; cnt["i"] += 1
                    ps, pk = PS[pi], PSK[pi]
                    for hc in range(4):
                        m.mm(ps[:, :], hT[:, hc, rt * 128:(rt + 1) * 128], wd_[i2][:, hc, oc * 512:(oc + 1) * 512], hc == 0, hc == 3,
                             ["fhT%d" % hc, "fwd%d" % i2], [pk])
                    m.cp("dve", y_[:, oc * 512:(oc + 1) * 512], ps[:, :], [pk], [yk])
                r0 = e_ * CAP + rt * 128
                m.ld("act", ys_d[r0:r0 + 128, :], y_[:], [yk], ["ys_d"])
        ph.close()

    if "F" in phases:
        phase_F()

    def phase_G():
        ph = Phase()
        tm = ln_tmps(ph)
        load_ln(8)
        NB = 4
        x2 = [ph.sb("gx%d" % i, [128, D], F32) for i in range(NB)]
        y1 = [ph.sb("gy1%d" % i, [128, D], F32) for i in range(NB)]
        y2 = [ph.sb("gy2%d" % i, [128, D], F32) for i in range(NB)]
        rto = [ph.sb("grt%d" % i, [128, 4], F32) for i in range(NB)]
        dsti = [ph.sb("gds%d" % i, [128, 2], I32) for i in range(NB)]

        def issue(lt):
            i2 = lt % NB
            m.ld("sp", x2[i2][:], x2_d[lt * 128:(lt + 1) * 128, :], [], ["gx%d" % i2])
            m.ld("sp", rto[i2][:], rt_d[lt * 128:(lt + 1) * 128, :], [], ["grt%d" % i2])
            m.cp("dve", dsti[i2][:], rto[i2][:, 0:2], ["grt%d" % i2], ["gds%d" % i2])
            for kq, yy in enumerate((y1[i2], y2[i2])):
                yk = "gy%d%d" % (kq + 1, i2)
                m.dma("pool", lambda e, kq=kq, yy=yy, d_=dsti[i2]: e.indirect_dma_start(
                    out=yy[:], out_offset=None, in_=ys_d, in_offset=bass.IndirectOffsetOnAxis(ap=d_[:, kq:kq + 1], axis=0)),
                    ["ys_d", "gds%d" % i2], [yk])

        issue(0)
        issue(1)
        for lt in range(NT):
            i2 = lt % NB
            if lt + 2 < NT:
                issue(lt + 2)
            xk = "gx%d" % i2
            m.act(x2[i2][:], x2[i2][:], AF.Copy, [xk], [xk], scale=ALPHA)
            m.stt(x2[i2][:], y1[i2][:], rto[i2][:, 2:3], x2[i2][:], ALU.mult, ALU.add, ["gy1%d" % i2, "grt%d" % i2, xk], [xk])
            m.stt(x2[i2][:], y2[i2][:], rto[i2][:, 3:4], x2[i2][:], ALU.mult, ALU.add, ["gy2%d" % i2, "grt%d" % i2, xk], [xk])
            ln_generic(tm, x2[i2][:], [xk], lnrep, "lnrep", last_eng="dve")
            m.ld("sp", out[lt * 128:(lt + 1) * 128, :], x2[i2][:], [xk], [])
        ph.close()

    if "G" in phases:
        phase_G()

    m.finish()
    return nc, m


def _t5_bucket(dist):
    n = np.maximum(dist, 0)
    nf = np.maximum(n, 1).astype(np.float32)
    large = 16 + (np.log(nf / np.float32(16)) / np.float32(math.log(8.0)) * np.float32(16)).astype(np.int32)
    large = np.minimum(large, 31)
    return np.where(n < 16, n, large).astype(np.int64)


def _prep(inputs):
    f = lambda a: np.ascontiguousarray(np.asarray(a, dtype=np.float32))
    x = f(inputs["x"]); mem = f(inputs["mem"])
    w_in = f(inputs["w_in"])[0]
    sizes = [1024] + [256] * 6 + [48, 2048]
    cuts = np.cumsum([0] + sizes)
    sec = [w_in[:, cuts[i]:cuts[i + 1]] for i in range(9)]
    q, k_c, v_c, k_s, v_s, k_w, v_w, g, u = sec
    w_inp = np.ascontiguousarray(np.concatenate([k_c, v_c, k_s, k_w, v_s, v_w, q, g, u], axis=1))
    lnv = np.stack([f(inputs["ln_in_g"]), f(inputs["ln_in_b"]), f(inputs["ln1_g"])[0], f(inputs["ln1_b"])[0],
                    f(inputs["mem_ln_g"])[0], f(inputs["mem_ln_b"])[0], f(inputs["ln2_g"])[0], f(inputs["ln2_b"])[0],
                    f(inputs["ln3_g"])[0], f(inputs["ln3_b"])[0]])

    def w1l(w):
        return np.ascontiguousarray(f(w)[0].reshape(32, 64, 256).transpose(1, 0, 2).reshape(64, 32 * 256))

    rel = f(inputs["rel_table"])
    kk = np.arange(128)[:, None]; qq = np.arange(128)[None, :]
    d_diag = qq - kk
    diag_raw = rel[_t5_bucket(d_diag)]
    diag_raw = np.ascontiguousarray(diag_raw.transpose(0, 2, 1).reshape(128, 16 * 128))
    diag_mask = np.where(d_diag >= 0, 0.0, NEG).astype(np.float32)
    sub1_raw = np.ascontiguousarray(rel[_t5_bucket(128 + qq - kk)].transpose(0, 2, 1).reshape(128, 16 * 128))
    ii = np.arange(16)[:, None]
    d_band = qq - 16 * ii + 113
    band_raw = np.ascontiguousarray(rel[_t5_bucket(d_band)].transpose(0, 2, 1).reshape(16, 16 * 128))
    band_mask = np.where(d_band >= 0, 0.0, NEG).astype(np.float32)
    anti_mask = np.where(kk > qq, 0.0, NEG).astype(np.float32)
    r31 = np.ascontiguousarray(np.broadcast_to(rel[31][None, :], (128, 16)))
    dw_c = np.zeros((16, 272), np.float32)
    dw_c[np.arange(16), 128 + np.arange(16)] = 1.0
    ident = np.eye(128, dtype=np.float32)
    ut = np.triu(np.ones((128, 128), np.float32), 1)
    cs = np.arange(512) * 16; ce = cs + 32
    ss = np.arange(128) * 64; se = ss + 64
    ov = (np.clip(np.minimum(ce[:, None], se[None, :]) - np.maximum(cs[:, None], ss[None, :]), 0, None) / 32.0).astype(np.float32)
    ov[511] = 0.0
    ec = np.ascontiguousarray(np.broadcast_to((np.arange(32) * CAP).astype(np.float32)[None, :], (128, 32)))
    asel = np.zeros((64, SV), np.float32)
    kcol = np.arange(SV)
    asel[(2 * (kcol // 128) + (kcol % 128) // 64) % 64, kcol] = 1.0
    band3 = np.zeros((128, 3), np.float32)
    band3[:64, 0] = 100.0; band3[:, 1] = 100.0; band3[64:, 2] = 100.0; band3[:64, 2] = -100.0
    dww = np.ascontiguousarray(f(inputs["conv_dw_w"])[0][:, 0, :].reshape(31, 8, 128).transpose(2, 1, 0).reshape(128, 8 * 31))
    cvec = np.concatenate([f(inputs["conv_dw_b"])[0].reshape(8, 128).T, f(inputs["conv_ln_g"])[0].reshape(8, 128).T,
                           f(inputs["conv_ln_b"])[0].reshape(8, 128).T], axis=1)
    cvec = np.ascontiguousarray(cvec)
    rwm = np.ascontiguousarray(np.concatenate([f(inputs["router_group_w"])[0], f(inputs["router_expert_w"])[0]], axis=1))
    rbm = np.concatenate([f(inputs["router_group_b"])[0], f(inputs["router_expert_b"])[0]])[None, :]
    common = dict(
        lnv=np.ascontiguousarray(lnv), w_inp=w_inp,
        w1k=w1l(inputs["cmp_w1_k"]), w1v=w1l(inputs["cmp_w1_v"]),
        pek=np.ascontiguousarray(f(inputs["cmp_pe_k"])[0].T), pev=np.ascontiguousarray(f(inputs["cmp_pe_v"])[0].T),
        w2k=f(inputs["cmp_w2_k"])[0], w2v=f(inputs["cmp_w2_v"])[0],
        diag_raw=diag_raw, sub1_raw=sub1_raw, band_raw=band_raw, r31=r31,
        diag_mask=diag_mask, anti_mask=anti_mask, band_mask=band_mask,
        dw_c=dw_c, asel_c=asel, ident_c=ident, ut_c=ut, ov_c=ov, ec_c=ec, band3_c=band3,
        dww=dww, cvec=cvec,
        w_out=f(inputs["w_out"])[0], xa_wq=f(inputs["xa_wq"])[0], xa_wkv=f(inputs["xa_wkv"])[0], xa_wo=f(inputs["xa_wo"])[0],
        rw=rwm, rb=np.ascontiguousarray(rbm),
        mwg=f(inputs["moe_w_gate"])[0], mwu=f(inputs["moe_w_up"])[0], mwd=f(inputs["moe_w_down"])[0],
    )
    in_maps = []
    for c in range(8):
        b, s = c // 2, c % 2
        if s == 1:
            xvv = x[b]
        else:
            xvv = np.ascontiguousarray(np.concatenate([x[b, OWN:], x[b, :OWN]], axis=0))
        keyb = np.zeros((128, 64), np.float32)
        cmpb = np.zeros((128, 4), np.float32)
        f0 = np.zeros((128, 128), np.float32)
        if s == 0:
            keyb[:, :32] = NEG
            cmpb[:, :2] = NEG
            f0[:, 64] = 100.0
        else:
            f0[:, 0] = 100.0
        cmpb[127, 3] = NEG
        halo = np.full((128, 1), float(s), np.float32)
        d = dict(common)
        d.update(xv=xvv, memb=mem[b], keybias_c=keyb, cmpbias_c=cmpb, f0_c=f0, halo_c=halo)
        in_maps.append(d)
    return in_maps


_CACHE = {}


def kernel(**inputs):
    in_maps = _prep(inputs)
    if "nc" not in _CACHE:
        _CACHE["nc"] = build()[0]
    res = run_bass_kernel_spmd(_CACHE["nc"], in_maps, core_ids=list(range(8)))
    outp = np.zeros((4, SV, D), np.float32)
    for c in range(8):
        b, s = c // 2, c % 2
        outp[b, s * OWN:(s + 1) * OWN] = res.results[c]["out"]
    return outp
```
